# Optimizing a Trainium2 kernel written in Bass

```python
import math
import jax
import jax.numpy as jnp
from jax import lax
import numpy as np

D_MODEL = 1024
BATCH = 16
SEQ = 2048
DEPTH = 2

GRID_W = 64
CTX_LEN = 256
HEAD_DIM = 64
ROPE_THETA = 10000.0
NA_HEADS = 8
NA_WIN_H = 8
NA_WIN_W = 16
NA_COL_BLOCK = 16
NA_COL_BAND = NA_WIN_W + NA_COL_BLOCK
DIFF_HEADS = 4
DIFF_VDIM = 2 * HEAD_DIM
GQA_Q_HEADS = 16
GQA_KV_HEADS = 4
GQA_GROUP = GQA_Q_HEADS // GQA_KV_HEADS
Q_BLOCK = 128
FFN_DIM = 2816
N_EXPERTS = 8
TOP_K = 2
EXPERT_DIM = 3584
MOE_BLOCK = 256
DEEPNORM_ALPHA = (2 * DEPTH) ** 0.25
DEEPNORM_BETA = (8 * DEPTH) ** -0.25
LN_EPS = 1e-5
RMS_EPS = 1e-6
NEG_INF = -1e30
NA_W = NA_HEADS * HEAD_DIM
DIFF_QK_W = DIFF_HEADS * 2 * HEAD_DIM
DIFF_V_W = DIFF_HEADS * DIFF_VDIM
EVEN_IN = 3 * NA_W + 2 * DIFF_QK_W + DIFF_V_W
MIX_W_EVEN = NA_W + DIFF_V_W
GQA_Q_W = GQA_Q_HEADS * HEAD_DIM
GQA_KV_W = GQA_KV_HEADS * HEAD_DIM
ODD_IN = GQA_Q_W + 2 * GQA_KV_W

kernel_name = 'hybrid_natten_diffattn_gqa_moe_dit'


def layer_norm(x, g, b):
    xf = x.astype(jnp.float32)
    mu = jnp.mean(xf, axis=-1, keepdims=True)
    var = jnp.mean(jnp.square(xf - mu), axis=-1, keepdims=True)
    return ((xf - mu) * lax.rsqrt(var + LN_EPS)).astype(x.dtype) * g + b


def rms_norm(x, g):
    xf = x.astype(jnp.float32)
    return (xf * lax.rsqrt(jnp.mean(xf * xf, axis=-1, keepdims=True) + RMS_EPS)).astype(x.dtype) * g


def ada_mods(cond, w, b):
    return jnp.split(jax.nn.silu(cond) @ w + b, 6, axis=-1)


def modulate(h, shift, scale):
    return h * (1 + scale) + shift


def swiglu(h, w13, w2):
    a, g = jnp.split(h @ w13, 2, axis=-1)
    return (jax.nn.silu(a) * g) @ w2


def axial_rope_tables(n_tok, dtype):
    t = jnp.arange(n_tok, dtype=jnp.int32)
    n_freq = HEAD_DIM // 4
    inv_freq = ROPE_THETA ** (-jnp.arange(n_freq, dtype=jnp.float32) / n_freq)
    ang = jnp.concatenate([(t // GRID_W).astype(jnp.float32)[:, None] * inv_freq,
                           (t % GRID_W).astype(jnp.float32)[:, None] * inv_freq], axis=-1)
    return jnp.cos(ang).astype(dtype), jnp.sin(ang).astype(dtype)


def apply_rope(x, cos, sin):
    shape = (cos.shape[0],) + (1,) * (x.ndim - 3) + (cos.shape[1],)
    c = cos.reshape(shape)
    s = sin.reshape(shape)
    x1, x2 = jnp.split(x, 2, axis=-1)
    return jnp.concatenate([x1 * c - x2 * s, x2 * c + x1 * s], axis=-1)


def sweep_query_blocks(fn, q, k, v):
    bsz, n_tok = q.shape[:2]
    qb = jnp.moveaxis(q.reshape((bsz, n_tok // Q_BLOCK, Q_BLOCK) + q.shape[2:]), 1, 0)
    out = lax.map(lambda qi: fn(qi, k, v), qb)
    return jnp.moveaxis(out, 0, 1).reshape((bsz, n_tok) + out.shape[3:])


def softmax_attention(q, k, v):
    s = jnp.einsum('bqhd,bkhd->bhqk', q, k, preferred_element_type=jnp.float32) * (q.shape[-1] ** -0.5)
    p = jax.nn.softmax(s, axis=-1).astype(v.dtype)
    return jnp.einsum('bhqk,bkhd->bqhd', p, v)


def diff_attend(q, k, v, lam):
    s = jnp.einsum('bqhcd,bkhcd->bhcqk', q, k, preferred_element_type=jnp.float32) * (q.shape[-1] ** -0.5)
    p = jax.nn.softmax(s, axis=-1)
    a = (p[:, :, 0] - lam * p[:, :, 1]).astype(v.dtype)
    return jnp.einsum('bhqk,bkhe->bqhe', a, v)


def gqa_attend(q, k, v):
    s = jnp.einsum('bqgrd,bkgd->bgrqk', q, k, preferred_element_type=jnp.float32) * (q.shape[-1] ** -0.5)
    p = jax.nn.softmax(s, axis=-1).astype(v.dtype)
    return jnp.einsum('bgrqk,bkgd->bqgrd', p, v)


def neighbourhood_attention(q, k, v, kc, vc, rpb):
    bsz, n_tok, n_h, dh = q.shape
    rows = n_tok // GRID_W
    kh = min(NA_WIN_H, rows)
    n_cb = GRID_W // NA_COL_BLOCK
    row_start = np.clip(np.arange(rows) - kh // 2, 0, rows - kh)
    dr_idx = row_start[:, None] + np.arange(kh)[None, :] - np.arange(rows)[:, None] + NA_WIN_H - 1
    q_cols = np.arange(GRID_W).reshape(n_cb, NA_COL_BLOCK)
    band_start = np.clip(q_cols[:, 0] - NA_WIN_W // 2, 0, GRID_W - NA_COL_BAND)
    band_cols = band_start[:, None] + np.arange(NA_COL_BAND)[None, :]
    win_start = np.clip(q_cols - NA_WIN_W // 2, 0, GRID_W - NA_WIN_W)
    bc = band_cols[:, None, :]
    in_win = (bc >= win_start[..., None]) & (bc < win_start[..., None] + NA_WIN_W)
    dc_idx = np.clip(bc - q_cols[..., None] + NA_WIN_W - 1, 0, 2 * NA_WIN_W - 2)
    bias = rpb.astype(jnp.float32)[:, dr_idx][..., dc_idx]
    bias = jnp.where(in_win, bias, NEG_INF).transpose(1, 0, 3, 4, 2, 5)
    scale = dh ** -0.5
    n_loc = kh * NA_COL_BAND
    qg = jnp.moveaxis(q.reshape(bsz, rows, n_cb, NA_COL_BLOCK, n_h, dh), 1, 0)
    kg = k.reshape(bsz, rows, GRID_W, n_h, dh)
    vg = v.reshape(bsz, rows, GRID_W, n_h, dh)

    def one_row(args):
        q_r, r0, bias_r = args
        k_band = lax.dynamic_slice_in_dim(kg, r0, kh, axis=1)[:, :, band_cols]
        v_band = lax.dynamic_slice_in_dim(vg, r0, kh, axis=1)[:, :, band_cols]
        s_loc = jnp.einsum('bnqhd,binjhd->bhnqij', q_r, k_band, preferred_element_type=jnp.float32) * scale + bias_r
        s_ctx = jnp.einsum('bnqhd,blhd->bhnql', q_r, kc, preferred_element_type=jnp.float32) * scale
        s = jnp.concatenate([s_loc.reshape(s_loc.shape[:4] + (n_loc,)), s_ctx], axis=-1)
        p = jax.nn.softmax(s, axis=-1).astype(v.dtype)
        p_loc = p[..., :n_loc].reshape(s_loc.shape)
        p_ctx = p[..., n_loc:]
        return (jnp.einsum('bhnqij,binjhd->bnqhd', p_loc, v_band)
                + jnp.einsum('bhnql,blhd->bnqhd', p_ctx, vc))

    out = lax.map(one_row, (qg, jnp.asarray(row_start, dtype=jnp.int32), bias))
    return jnp.moveaxis(out, 0, 1).reshape(bsz, n_tok, n_h * dh)


def moe_swiglu(h, router_w, w13, w2):
    hf = h.reshape(-1, h.shape[-1])
    n_tok = hf.shape[0]
    logits = jnp.matmul(hf, router_w, preferred_element_type=jnp.float32)
    top_v, top_i = lax.top_k(logits, TOP_K)
    gates = jax.nn.softmax(top_v, axis=-1)
    n_assign = n_tok * TOP_K
    flat_e = top_i.reshape(-1)
    flat_tok = jnp.repeat(jnp.arange(n_tok, dtype=jnp.int32), TOP_K)
    flat_g = gates.reshape(-1)
    order = jnp.argsort(flat_e)
    e_sorted = flat_e[order]
    counts = jnp.bincount(flat_e, length=N_EXPERTS)
    padded = (counts + MOE_BLOCK - 1) // MOE_BLOCK * MOE_BLOCK
    pad_end = jnp.cumsum(padded)
    pad_start = pad_end - padded
    start = jnp.cumsum(counts) - counts
    dest = pad_start[e_sorted] + jnp.arange(n_assign, dtype=jnp.int32) - start[e_sorted]
    n_blocks = -(-(n_assign + N_EXPERTS * (MOE_BLOCK - 1)) // MOE_BLOCK)
    n_slots = n_blocks * MOE_BLOCK
    slot_tok = jnp.zeros((n_slots,), jnp.int32).at[dest].set(flat_tok[order])
    slot_g = jnp.zeros((n_slots,), jnp.float32).at[dest].set(flat_g[order])
    block_e = jnp.minimum(jnp.searchsorted(pad_end, jnp.arange(n_blocks, dtype=jnp.int32) * MOE_BLOCK, side='right'),
                          N_EXPERTS - 1)

    def expert_block(args):
        tok, g, e = args
        return swiglu(hf[tok], w13[e], w2[e]) * g[:, None].astype(hf.dtype)

    y = lax.map(expert_block, (slot_tok.reshape(n_blocks, MOE_BLOCK), slot_g.reshape(n_blocks, MOE_BLOCK), block_e))
    out = jnp.zeros_like(hf).at[slot_tok].add(y.reshape(n_slots, hf.shape[-1]))
    return out.reshape(h.shape)


def split_even(p):
    lead = p.shape[:2]
    cuts = np.cumsum([NA_W, NA_W, NA_W, DIFF_QK_W, DIFF_QK_W]).tolist()
    qa, ka, va, qb, kb, vb = jnp.split(p, cuts, axis=-1)
    return (qa.reshape(lead + (NA_HEADS, HEAD_DIM)), ka.reshape(lead + (NA_HEADS, HEAD_DIM)),
            va.reshape(lead + (NA_HEADS, HEAD_DIM)), qb.reshape(lead + (DIFF_HEADS, 2, HEAD_DIM)),
            kb.reshape(lead + (DIFF_HEADS, 2, HEAD_DIM)), vb.reshape(lead + (DIFF_HEADS, DIFF_VDIM)))


def split_odd(p):
    lead = p.shape[:2]
    q, k, v = jnp.split(p, [GQA_Q_W, GQA_Q_W + GQA_KV_W], axis=-1)
    return (q.reshape(lead + (GQA_Q_HEADS, HEAD_DIM)), k.reshape(lead + (GQA_KV_HEADS, HEAD_DIM)),
            v.reshape(lead + (GQA_KV_HEADS, HEAD_DIM)))


def even_layer(x, ctx, c, c_ctx, params, layer_idx, last):
    (ada_w, ada_b, w_in, rpb, lambda_qk, subln_g, w_out,
     ln1_g, ln1_b, ffn_w13, ffn_w2, ln2_g, ln2_b) = params
    bsz, n_tok, _ = x.shape
    n_ctx = ctx.shape[1]
    sh1, sc1, g1, sh2, sc2, g2 = [m[:, None, :] for m in ada_mods(c, ada_w, ada_b)]
    csh1, csc1, cg1, csh2, csc2, cg2 = ada_mods(c_ctx, ada_w, ada_b)
    qa, ka, va, qb, kb, vb = split_even(modulate(x, sh1, sc1) @ w_in)
    qa_c, ka_c, va_c, qb_c, kb_c, vb_c = split_even(modulate(ctx, csh1, csc1) @ w_in)
    cos, sin = axial_rope_tables(n_tok, x.dtype)
    y_a = neighbourhood_attention(qa, ka, va, ka_c, va_c, rpb)
    lam_init = 0.8 - 0.6 * math.exp(-0.3 * layer_idx)
    lq = lambda_qk.astype(jnp.float32)
    lam = jnp.exp(jnp.sum(lq[0] * lq[1])) - jnp.exp(jnp.sum(lq[2] * lq[3])) + lam_init
    kb_all = jnp.concatenate([apply_rope(kb, cos, sin), kb_c], axis=1)
    vb_all = jnp.concatenate([vb, vb_c], axis=1)
    y_b = sweep_query_blocks(lambda qi, kk, vv: diff_attend(qi, kk, vv, lam), apply_rope(qb, cos, sin), kb_all, vb_all)
    y_b = rms_norm(y_b, subln_g) * (1.0 - lam_init)
    y = jnp.concatenate([y_a, y_b.reshape(bsz, n_tok, DIFF_V_W)], axis=-1) @ w_out
    x_new = layer_norm(DEEPNORM_ALPHA * x + g1 * y, ln1_g, ln1_b)
    x_new = layer_norm(DEEPNORM_ALPHA * x_new + g2 * swiglu(modulate(x_new, sh2, sc2), ffn_w13, ffn_w2), ln2_g, ln2_b)
    if not last:
        ya_c = softmax_attention(qa_c, ka_c, va_c).reshape(bsz, n_ctx, NA_W)
        yb_c = rms_norm(diff_attend(qb_c, kb_c, vb_c, lam), subln_g) * (1.0 - lam_init)
        yc = jnp.concatenate([ya_c, yb_c.reshape(bsz, n_ctx, DIFF_V_W)], axis=-1) @ w_out
        ctx = layer_norm(DEEPNORM_ALPHA * ctx + cg1 * yc, ln1_g, ln1_b)
        ctx = layer_norm(DEEPNORM_ALPHA * ctx + cg2 * swiglu(modulate(ctx, csh2, csc2), ffn_w13, ffn_w2), ln2_g, ln2_b)
    return x_new, ctx


def odd_layer(x, ctx, c, c_ctx, params, layer_idx, last):
    (ada_w, ada_b, w_in, q_norm_g, k_norm_g, w_out, ln1_g, ln1_b,
     router_w, moe_w13, moe_w2, ln2_g, ln2_b) = params
    bsz, n_tok, _ = x.shape
    n_ctx = ctx.shape[1]
    sh1, sc1, g1, sh2, sc2, g2 = [m[:, None, :] for m in ada_mods(c, ada_w, ada_b)]
    csh1, csc1, cg1, csh2, csc2, cg2 = ada_mods(c_ctx, ada_w, ada_b)
    q, k, v = split_odd(modulate(x, sh1, sc1) @ w_in)
    q_c, k_c, v_c = split_odd(modulate(ctx, csh1, csc1) @ w_in)
    cos, sin = axial_rope_tables(n_tok, x.dtype)
    q = apply_rope(rms_norm(q, q_norm_g), cos, sin)
    k = apply_rope(rms_norm(k, k_norm_g), cos, sin)
    k_c = rms_norm(k_c, k_norm_g)
    k_all = jnp.concatenate([k, k_c], axis=1)
    v_all = jnp.concatenate([v, v_c], axis=1)
    qg = q.reshape(bsz, n_tok, GQA_KV_HEADS, GQA_GROUP, HEAD_DIM)
    y = sweep_query_blocks(gqa_attend, qg, k_all, v_all).reshape(bsz, n_tok, GQA_Q_W) @ w_out
    x_new = layer_norm(DEEPNORM_ALPHA * x + g1 * y, ln1_g, ln1_b)
    x_new = layer_norm(DEEPNORM_ALPHA * x_new + g2 * moe_swiglu(modulate(x_new, sh2, sc2), router_w, moe_w13, moe_w2),
                       ln2_g, ln2_b)
    if not last:
        qc = rms_norm(q_c, q_norm_g).reshape(bsz, n_ctx, GQA_KV_HEADS, GQA_GROUP, HEAD_DIM)
        yc = gqa_attend(qc, k_c, v_c).reshape(bsz, n_ctx, GQA_Q_W) @ w_out
        ctx = layer_norm(DEEPNORM_ALPHA * ctx + cg1 * yc, ln1_g, ln1_b)
        ctx = layer_norm(DEEPNORM_ALPHA * ctx + cg2 * moe_swiglu(modulate(ctx, csh2, csc2), router_w, moe_w13, moe_w2),
                         ln2_g, ln2_b)
    return x_new, ctx


def setup_inputs(seed: int = 0) -> dict:
    key = jax.random.key(seed)
    keys = iter(jax.random.split(key, 64))

    def nrm(shape, std):
        return jax.random.normal(next(keys), shape, jnp.float32) * std

    def gain(n):
        return 1.0 + nrm((n,), 0.05)

    D = D_MODEL
    s_in = D ** -0.5
    return {
        'x': nrm((BATCH, SEQ, D), 1.0),
        'c': nrm((BATCH, D), 1.0),
        'ctx': nrm((BATCH, CTX_LEN, D), 1.0),
        'c_ctx': nrm((D,), 1.0),
        'l0_ada_w': nrm((D, 6 * D), 0.5 * s_in),
        'l0_ada_b': nrm((6 * D,), 0.02),
        'l0_w_in': nrm((D, EVEN_IN), s_in),
        'l0_rpb': nrm((NA_HEADS, 2 * NA_WIN_H - 1, 2 * NA_WIN_W - 1), 0.1),
        'l0_lambda_qk': nrm((4, HEAD_DIM), 0.1),
        'l0_subln_g': gain(DIFF_VDIM),
        'l0_w_out': nrm((MIX_W_EVEN, D), MIX_W_EVEN ** -0.5 * DEEPNORM_BETA),
        'l0_ln1_g': gain(D),
        'l0_ln1_b': nrm((D,), 0.02),
        'l0_ffn_w13': nrm((D, 2 * FFN_DIM), s_in),
        'l0_ffn_w2': nrm((FFN_DIM, D), FFN_DIM ** -0.5 * DEEPNORM_BETA),
        'l0_ln2_g': gain(D),
        'l0_ln2_b': nrm((D,), 0.02),
        'l1_ada_w': nrm((D, 6 * D), 0.5 * s_in),
        'l1_ada_b': nrm((6 * D,), 0.02),
        'l1_w_in': nrm((D, ODD_IN), s_in),
        'l1_q_norm_g': gain(HEAD_DIM),
        'l1_k_norm_g': gain(HEAD_DIM),
        'l1_w_out': nrm((GQA_Q_W, D), GQA_Q_W ** -0.5 * DEEPNORM_BETA),
        'l1_ln1_g': gain(D),
        'l1_ln1_b': nrm((D,), 0.02),
        'l1_router_w': nrm((D, N_EXPERTS), s_in),
        'l1_moe_w13': nrm((N_EXPERTS, D, 2 * EXPERT_DIM), s_in),
        'l1_moe_w2': nrm((N_EXPERTS, EXPERT_DIM, D), EXPERT_DIM ** -0.5 * DEEPNORM_BETA),
        'l1_ln2_g': gain(D),
        'l1_ln2_b': nrm((D,), 0.02),
    }


def reference(x, c, ctx, c_ctx,
              l0_ada_w, l0_ada_b, l0_w_in, l0_rpb, l0_lambda_qk, l0_subln_g, l0_w_out,
              l0_ln1_g, l0_ln1_b, l0_ffn_w13, l0_ffn_w2, l0_ln2_g, l0_ln2_b,
              l1_ada_w, l1_ada_b, l1_w_in, l1_q_norm_g, l1_k_norm_g, l1_w_out,
              l1_ln1_g, l1_ln1_b, l1_router_w, l1_moe_w13, l1_moe_w2, l1_ln2_g, l1_ln2_b):
    layer_params = (
        (l0_ada_w, l0_ada_b, l0_w_in, l0_rpb, l0_lambda_qk, l0_subln_g, l0_w_out,
         l0_ln1_g, l0_ln1_b, l0_ffn_w13, l0_ffn_w2, l0_ln2_g, l0_ln2_b),
        (l1_ada_w, l1_ada_b, l1_w_in, l1_q_norm_g, l1_k_norm_g, l1_w_out,
         l1_ln1_g, l1_ln1_b, l1_router_w, l1_moe_w13, l1_moe_w2, l1_ln2_g, l1_ln2_b),
    )
    for layer in range(DEPTH):
        last = layer == DEPTH - 1
        if layer % 2 == 0:
            x, ctx = even_layer(x, ctx, c, c_ctx, layer_params[layer], layer, last)
        else:
            x, ctx = odd_layer(x, ctx, c, c_ctx, layer_params[layer], layer, last)
    return x
```

```python
import math
from contextlib import ExitStack
import numpy as np
import concourse.bass as bass
import concourse.mybir as mybir
from concourse.bass_utils import run_bass_kernel_spmd

F32 = mybir.dt.float32
BF16 = mybir.dt.bfloat16
AF = mybir.ActivationFunctionType
ALU = mybir.AluOpType
AX = mybir.AxisListType

NCORES = 8
D = 1024
SEQ = 2048
NCTX = 256
T = SEQ + NCTX
ALPHA = 4.0 ** 0.25
LN_EPS = 1e-5
RMS_EPS = 1e-6
FFN = 2816
EDIM = 3584
NEXP = 8
NEG = -30000.0
LAM_INIT0 = 0.8 - 0.6 * math.exp(-0.3 * 0)

ENGS = ("pe", "act", "dve", "pool", "sp")
SELF_SYNC = ("act", "dve", "pool")


class Op:
    __slots__ = ("eng", "fn", "deps", "idx", "is_dma", "waits", "signal", "sem", "target", "dma_prev")

    def __init__(self, eng, fn, is_dma):
        self.eng = eng
        self.fn = fn
        self.deps = []
        self.is_dma = is_dma
        self.waits = []
        self.signal = False
        self.sem = None
        self.target = None
        self.dma_prev = None


class Prog:
    def __init__(self, nc, n_rot=8, n_dma_sems=12):
        self.nc = nc
        self.ops = {e: [] for e in ENGS}
        self.last_writer = {}
        self.readers = {}
        self.n_rot = n_rot
        self.n_dma_sems = n_dma_sems
        self.dmas_live = []

    def _add(self, eng, fn, reads, writes, is_dma, extra=()):
        psr = tuple(k for k in reads if isinstance(k, str) and k.startswith("ps") and k[2:].isdigit())
        if psr:
            reads = tuple(k for k in reads if k not in psr)
            writes = tuple(writes) + tuple(k for k in psr if k not in writes)
        op = Op(eng, fn, is_dma)
        op.idx = len(self.ops[eng])
        deps = list(extra)
        for k in reads:
            w = self.last_writer.get(k)
            if w is not None:
                deps.append(w)
        for k in writes:
            w = self.last_writer.get(k)
            if w is not None:
                deps.append(w)
            deps.extend(self.readers.get(k, ()))
        op.deps = deps
        for k in writes:
            self.last_writer[k] = op
            self.readers[k] = []
        for k in reads:
            if k not in writes:
                self.readers.setdefault(k, []).append(op)
        if is_dma:
            self.dmas_live.append(op)
        self.ops[eng].append(op)
        return op

    def op(self, eng, fn, reads=(), writes=()):
        return self._add(eng, fn, tuple(reads), tuple(writes), False)

    def dma(self, eng, out, in_, reads=(), writes=()):
        return self._add(eng, lambda e: e.dma_start(out=out, in_=in_), tuple(reads), tuple(writes), True)

    def barrier(self):
        lasts = [self.ops[e][-1] for e in ENGS if self.ops[e]]
        deps = lasts + self.dmas_live
        for e in ENGS:
            self._add(e, lambda eng: eng.nop(), (), (), False, extra=deps)
        self.last_writer.clear()
        self.readers.clear()
        self.dmas_live = []

    def resolve(self):
        R, K = self.n_rot, self.n_dma_sems
        for e in ENGS:
            dmas = [o for o in self.ops[e] if o.is_dma]
            for i, o in enumerate(dmas):
                o.sem = ("dma", e, i % K)
                o.target = 16 * (i // K + 1)
                o.signal = True
                o.dma_prev = dmas[i - K] if i >= K else None
        for e in ENGS:
            seen_eng = {x: -1 for x in ENGS}
            seen_dma = {}
            for o in self.ops[e]:
                deps = o.deps
                if o.is_dma and o.dma_prev is not None:
                    deps = deps + [o.dma_prev]
                best = {}
                for d in deps:
                    if d.is_dma:
                        if seen_dma.get(d.sem, 0) >= d.target:
                            continue
                        seen_dma[d.sem] = d.target
                        best[d.sem] = d
                    else:
                        if d.eng == e and e not in SELF_SYNC:
                            continue
                        if seen_eng[d.eng] >= d.idx:
                            continue
                        seen_eng[d.eng] = d.idx
                        best[("e", d.eng)] = d
                o.waits = list(best.values())
                for d in o.waits:
                    d.signal = True
                o.deps = None
        for e in ENGS:
            s = 0
            for o in self.ops[e]:
                if o.is_dma:
                    continue
                if o.signal:
                    o.sem = ("cmp", e, s % R)
                    o.target = s // R + 1
                    s += 1

    def emit(self):
        nc = self.nc
        self.resolve()
        names = set()
        for e in ENGS:
            for o in self.ops[e]:
                if o.signal:
                    names.add(o.sem)
        with ExitStack() as st:
            sems = {n: st.enter_context(nc.semaphore("_".join(map(str, n)))) for n in sorted(names)}
            block = st.enter_context(nc.Block())

            def run(e):
                def body(eng):
                    for o in self.ops[e]:
                        ws = o.waits
                        attach = None
                        if ws and not o.is_dma:
                            attach = ws[-1]
                            ws = ws[:-1]
                        for d in ws:
                            eng.wait_ge(sems[d.sem], d.target)
                        ins = o.fn(eng)
                        if attach is not None:
                            ins._wait_ge(sems[attach.sem], attach.target)
                        if o.signal:
                            ins.then_inc(sems[o.sem], 16 if o.is_dma else 1)
                return body

            block.tensor(run("pe"))
            block.scalar(run("act"))
            block.vector(run("dve"))
            block.gpsimd(run("pool"))
            block.sync(run("sp"))


class Arena:
    def __init__(self, ap, nwords):
        self.ap = ap
        self.n = nwords
        self.off = 0
        self.base = 0

    def _take(self, words):
        words = (words + 7) // 8 * 8
        a = self.off
        self.off += words
        assert self.off <= self.n, f"arena overflow {self.off} > {self.n}"
        return self.ap[:, a:a + words], words

    @staticmethod
    def _shape(v, free):
        if len(free) == 1:
            return v
        if len(free) == 2:
            return v.rearrange("p (a b) -> p a b", a=free[0])
        if len(free) == 3:
            return v.rearrange("p (a b c) -> p a b c", a=free[0], b=free[1])
        raise ValueError

    def f32(self, *free):
        n = int(np.prod(free))
        v, w = self._take(n)
        return self._shape(v[:, 0:n], free)

    def bf(self, *free):
        n = int(np.prod(free))
        v, w = self._take((n + 1) // 2)
        return self._shape(v.bitcast(BF16)[:, 0:n], free)

    def mark(self):
        self.base = self.off

    def reset(self):
        self.off = self.base


def build_program(debug=False, nphases=10**9):
    nc = bass.Bass("TRN2", target_bir_lowering=False)

    def din(name, shape, dt=F32):
        return nc.dram_tensor(name, list(shape), dt, kind="ExternalInput").ap()

    def dscr(name, shape, dt):
        if debug:
            return nc.dram_tensor(name, list(shape), dt, kind="ExternalOutput").ap()
        return nc.dram_tensor(name, list(shape), dt).ap()

    x_in = din("x", [2, SEQ, D])
    ctx_in = din("ctx", [2, NCTX, D])
    cT_d = din("cT", [128, 8, 3])
    ident_d = din("ident", [128, 128])
    blk_d = din("blk", [128, 128])
    ropeC_d = din("ropeC", [128, T])
    ropeS_d = din("ropeS", [128, T])
    L = [dict(), dict()]
    for l in range(2):
        L[l]["ada_w"] = din(f"l{l}_ada_w", [D, 6 * D])
        L[l]["ada_bT"] = din(f"l{l}_ada_bT", [128, 48])
        L[l]["ada_b"] = din(f"l{l}_ada_b", [1, 6 * D])
        L[l]["w_out"] = din(f"l{l}_w_out", [D, D])
        L[l]["ln"] = din(f"l{l}_ln", [4, D])
    L[0]["wfm"] = din("l0_wfm", [D, 2048])
    L[0]["wfmp"] = din("l0_wfmp", [D, 1024])
    L[0]["wv"] = din("l0_wv", [D, 1024])
    nabias_d = din("l0_nabias", [5, 128, 8, 832])
    lq_d = din("l0_lq", [1, 256])
    subg_d = din("l0_subg", [1, 128])
    subgc_d = din("l0_subgc", [128, 1])
    w13_0 = din("l0_w13", [D, 2 * FFN])
    w2_0 = din("l0_w2", [FFN, D])
    L[1]["wfm"] = din("l1_wfm", [D, 1536])
    L[1]["wfmp"] = din("l1_wfmp", [D, 1536])
    L[1]["wv"] = din("l1_wv", [D, 256])
    gcols_d = din("l1_gcols", [128, 4])
    grow_d = din("l1_grow", [1, 128])
    router_d = din("l1_router", [D, NEXP])
    w13_1 = din("l1_w13", [NEXP, D, 2 * EDIM])
    w2_1 = din("l1_w2", [NEXP, EDIM, D])
    out_d = nc.dram_tensor("out", [2, SEQ, D], F32, kind="ExternalOutput").ap()

    qkT = [dscr("qkT0", [2, 16, 128, T], BF16), dscr("qkT1", [2, 12, 128, T], BF16)]
    vsc = [dscr("v0", [2, T, 1024], BF16), dscr("v1", [2, T, 256], BF16)]
    att = dscr("att", [2, T, D], BF16)
    attT = dscr("attT", [2, 8, 128, T], BF16)
    X1 = dscr("X1", [2, T, D], F32)
    X2 = dscr("X2", [2, T, D], F32)
    X3 = dscr("X3", [2, SEQ, D], F32)
    gates_d = dscr("gates", [2, 2, 3, 128, D], F32)

    P = Prog(nc)
    out_keys = []

    with ExitStack() as st:
        NW = 52000
        arena_t = st.enter_context(nc.sbuf_tensor("arena", [128, NW], F32))
        psa = st.enter_context(nc.psum_tensor("psa", [128, 4096], F32))
        A = Arena(arena_t, NW)

        def bank(i, n=512):
            return psa[:, 512 * i:512 * i + n]

        ident = A.f32(128)
        identb = A.bf(128)
        blk = A.f32(128)
        ones = A.f32(128)
        cst = A.f32(8)
        modsT = [A.f32(48, 3), A.f32(48, 3)]
        opsc = [[A.f32(8, 3), A.f32(8, 3)], [A.f32(8, 3), A.f32(8, 3)]]
        lamt = A.f32(8)
        subg = A.f32(128)
        gcols = A.f32(4)
        A.mark()

        P.dma("sp", ident, ident_d, writes=["ident"])
        P.dma("sp", blk, blk_d, writes=["blk"])
        P.dma("sp", gcols, gcols_d, writes=["gcols"])
        P.op("dve", lambda e: e.tensor_copy(identb, ident), reads=["ident"], writes=["identb"])
        P.op("dve", lambda e: e.memset(ones, 1.0), writes=["ones"])
        P.op("dve", lambda e: e.memset(cst[:, 0:1], LN_EPS), writes=["cst0"])
        P.op("dve", lambda e: e.memset(cst[:, 1:2], RMS_EPS), writes=["cst1"])
        P.op("dve", lambda e: e.memset(cst[:, 2:3], 0.0), writes=["cst2"])
        lqb = A.f32(256)
        lqp = A.f32(128)
        P.dma("sp", lqb, lq_d.partition_broadcast(128), writes=["lqb"])
        P.dma("sp", subg, subg_d.partition_broadcast(128), writes=["subg"])
        P.op("dve", lambda e: e.tensor_scalar(subg, subg, 1.0 - LAM_INIT0, None, ALU.mult), reads=["subg"], writes=["subg"])
        P.op("dve", lambda e: e.tensor_tensor(lqp[:, 0:64], lqb[:, 0:64], lqb[:, 64:128], ALU.mult), reads=["lqb"], writes=["lqp0"])
        P.op("dve", lambda e: e.tensor_tensor(lqp[:, 64:128], lqb[:, 128:192], lqb[:, 192:256], ALU.mult), reads=["lqb"], writes=["lqp1"])
        P.op("dve", lambda e: e.reduce_sum(lamt[:, 2:3], lqp[:, 0:64], axis=AX.X), reads=["lqp0"], writes=["lam2"])
        P.op("dve", lambda e: e.reduce_sum(lamt[:, 3:4], lqp[:, 64:128], axis=AX.X), reads=["lqp1"], writes=["lam3"])
        P.op("act", lambda e: e.activation(lamt[:, 4:6], lamt[:, 2:4], AF.Exp), reads=["lam2", "lam3"], writes=["lam45"])
        P.op("dve", lambda e: e.tensor_tensor(lamt[:, 0:1], lamt[:, 4:5], lamt[:, 5:6], ALU.subtract), reads=["lam45"], writes=["lam0a"])
        P.op("dve", lambda e: e.tensor_scalar(lamt[:, 0:1], lamt[:, 0:1], LAM_INIT0, None, ALU.add), reads=["lam0a"], writes=["lam0"])
        P.barrier()
        A.reset()

        def phase_mods(l):
            A.reset()
            cT = A.f32(8, 3)
            scT = A.f32(8, 3)
            abT = A.f32(48)
            wch = [A.f32(8, 1024), A.f32(8, 1024)]
            bbc = [A.f32(512), A.f32(512)]
            gst = [A.f32(512), A.f32(512)]
            pm = bank(0, 144)
            P.dma("sp", cT, cT_d, writes=["cT"])
            P.dma("sp", abT, L[l]["ada_bT"], writes=["abT"])
            P.op("act", lambda e: e.activation(scT, cT, AF.Silu), reads=["cT"], writes=["scT"])
            gi = 0
            for ty in range(6):
                wb = wch[ty % 2]
                wk = f"wch{ty % 2}"
                P.dma("sp", wb, L[l]["ada_w"][:, ty * 1024:(ty + 1) * 1024].rearrange("(k p) n -> p k n", p=128), writes=[wk])
                for jc in range(8):
                    j = ty * 8 + jc
                    for k in range(8):
                        P.op("pe", lambda e, wb=wb, jc=jc, k=k, j=j: e.matmul(pm[:, 3 * j:3 * j + 3], wb[:, k, jc * 128:(jc + 1) * 128], scT[:, k, :],
                                                                       start=(k == 0), stop=(k == 7)),
                             reads=[wk, "scT"], writes=["ps0"])
            pm3 = pm.rearrange("p (j m) -> p j m", m=3)
            for m in range(3):
                P.op("dve", lambda e, m=m: e.tensor_tensor(modsT[l][:, :, m], pm3[:, :, m], abT, ALU.add), reads=["ps0", "abT"], writes=[f"modsT{m}"])
            Rg = [A.f32(1024), A.f32(1024)]
            gi = 0
            for gsel, ty in ((0, 2), (1, 5)):
                for m in range(3):
                    R = Rg[gi % 2]
                    for k in range(8):
                        P.op("dve", lambda e, R=R, k=k, ty=ty, m=m: e.tensor_scalar(R[:, k * 128:(k + 1) * 128], ident, modsT[l][:, ty * 8 + k, m:m + 1], None, ALU.mult),
                             reads=[f"modsT{m}", "ident"], writes=[f"Rg{gi % 2}"])
                    for half in range(2):
                        pb = bank(1 + half)
                        P.op("pe", lambda e, pb=pb, R=R, half=half: e.matmul(pb, ones, R[:, half * 512:(half + 1) * 512], start=True, stop=True),
                             reads=[f"Rg{gi % 2}", "ones"], writes=[f"ps{1 + half}"])
                        gs = gst[half]
                        P.op("act", lambda e, gs=gs, pb=pb: e.copy(gs, pb), reads=[f"ps{1 + half}"], writes=[f"gst{half}"])
                        P.dma("sp", gates_d[l, gsel, m, :, half * 512:(half + 1) * 512], gs, reads=[f"gst{half}"])
                    gi += 1
            P.op("dve", lambda e: e.tensor_scalar(opsc[l][0], modsT[l][:, 8:16, :], 1.0, None, ALU.add), reads=["modsT0", "modsT1", "modsT2"], writes=["opsc0"])
            P.op("dve", lambda e: e.tensor_scalar(opsc[l][1], modsT[l][:, 32:40, :], 1.0, None, ALU.add), reads=["modsT0", "modsT1", "modsT2"], writes=["opsc1"])
            P.barrier()

        def tiles_of(Xsrc, b, nlat=16, nctx=2, l0=False):
            tl = []
            for t in range(nlat):
                if l0:
                    tl.append((x_in[b, t * 128:(t + 1) * 128, :], b))
                else:
                    tl.append((Xsrc[b, t * 128:(t + 1) * 128, :], b))
            for t in range(nctx):
                if l0:
                    tl.append((ctx_in[b, t * 128:(t + 1) * 128, :], 2))
                else:
                    tl.append((Xsrc[b, SEQ + t * 128:SEQ + (t + 1) * 128, :], 2))
            return tl

        def emit_hT(l, sub, xt, xk, m, hT, hk, col0, pbanks, pkeys, cnt, hTf=None, hfk=None):
            shift_base = 0 if sub == 0 else 24
            for half in range(2):
                pb = pbanks[cnt[0] % 2]
                pk = pkeys[cnt[0] % 2]
                cnt[0] += 1
                for kk in range(4):
                    k = half * 4 + kk
                    P.op("pe", lambda e, pb=pb, kk=kk, k=k: e.transpose(pb[:, kk * 128:(kk + 1) * 128], xt[:, k * 128:(k + 1) * 128], ident),
                         reads=[xk, "ident"], writes=[pk])
                for kk in range(4):
                    k = half * 4 + kk
                    sc = opsc[l][sub][:, k, m:m + 1]
                    sh = modsT[l][:, shift_base + k, m:m + 1]
                    if hTf is not None:
                        P.op("act", lambda e, pb=pb, kk=kk, k=k, sc=sc, sh=sh: e.activation(hTf[:, k, :], pb[:, kk * 128:(kk + 1) * 128], AF.Identity, bias=sh, scale=sc),
                             reads=[pk], writes=[hfk])
                    elif pk == pkeys[0]:
                        P.op("act", lambda e, pb=pb, kk=kk, k=k, sc=sc, sh=sh: e.activation(hT[:, k, col0:col0 + 128], pb[:, kk * 128:(kk + 1) * 128], AF.Identity, bias=sh, scale=sc),
                             reads=[pk], writes=[hk])
                    else:
                        P.op("dve", lambda e, pb=pb, kk=kk, k=k, sc=sc, sh=sh: e.tensor_scalar(hT[:, k, col0:col0 + 128], pb[:, kk * 128:(kk + 1) * 128], sc, sh, ALU.mult, ALU.add),
                             reads=[pk], writes=[hk])
            if hTf is not None:
                P.op("dve", lambda e: e.tensor_copy(hT[:, :, col0:col0 + 128], hTf), reads=[hfk], writes=[hk])

        def phase_proj(l, b, Xsrc):
            A.reset()
            nfm = 16 if l == 0 else 12
            nperm = 8 if l == 0 else 12
            perm_first = 8 if l == 0 else 0
            nv = 1024 if l == 0 else 256
            wfm = A.bf(8, nfm * 128)
            wfmp = A.bf(8, nperm * 128)
            wv = A.bf(8, nv)
            xt = [A.f32(4, 1024), A.f32(4, 1024)]
            hT = [A.bf(8, 512), A.bf(8, 512)]
            if l == 0:
                tabs = [(A.f32(T), A.f32(T))]
            else:
                tabs = [(A.f32(T), A.f32(T)), (A.f32(T), A.f32(T))]
            stg = [A.bf(512) for _ in range(4)]
            t1 = [A.f32(512), A.f32(512)]
            t2 = [A.f32(512), A.f32(512)]
            sq = [A.f32(512), A.f32(512)]
            rs = [A.f32(512), A.f32(512)]
            for c0 in range(0, nfm * 128, 512):
                P.dma("pool", wfm[:, :, c0:c0 + 512], L[l]["wfm"][:, c0:c0 + 512].rearrange("(k p) n -> p k n", p=128), writes=["wfm"])
            for c0 in range(0, nperm * 128, 512):
                P.dma("pool", wfmp[:, :, c0:c0 + 512], L[l]["wfmp"][:, c0:c0 + 512].rearrange("(k p) n -> p k n", p=128), writes=["wfmp"])
            for c0 in range(0, nv, 512):
                n = min(512, nv - c0)
                P.dma("pool", wv[:, :, c0:c0 + n], L[l]["wv"][:, c0:c0 + n].rearrange("(k p) n -> p k n", p=128), writes=["wv"])
            for ti, (tc, ts) in enumerate(tabs):
                P.dma("sp", tc, ropeC_d, writes=[f"tc{ti}"])
                P.dma("sp", ts, ropeS_d, writes=[f"ts{ti}"])
                if l == 1:
                    P.op("pool", lambda e, tc=tc, ti=ti: e.tensor_scalar(tc, tc, gcols[:, 2 * ti:2 * ti + 1], None, ALU.mult), reads=[f"tc{ti}", "gcols"], writes=[f"tc{ti}"])
                    P.op("pool", lambda e, ts=ts, ti=ti: e.tensor_scalar(ts, ts, gcols[:, 2 * ti + 1:2 * ti + 2], None, ALU.mult), reads=[f"ts{ti}", "gcols"], writes=[f"ts{ti}"])
            tl = tiles_of(Xsrc, b, l0=(l == 0))
            groups = [tl[0:4], tl[4:8], tl[8:12], tl[12:16], tl[16:18]]
            cnt = [0]
            si = 0
            ci = 0
            vi = 0
            tails = []
            def load_grp(g):
                for ti, (src, m) in enumerate(groups[g]):
                    P.dma("sp", xt[g % 2][:, ti, :], src, writes=[f"xt{g % 2}_{ti}"])
            def build_hT(g):
                gb_ = g % 2
                for ti, (src, m) in enumerate(groups[g]):
                    emit_hT(l, 0, xt[gb_][:, ti, :], f"xt{gb_}_{ti}", m, hT[gb_], f"hT{gb_}", ti * 128, [bank(0), bank(1)], ["ps0", "ps1"], cnt)

            load_grp(0)
            build_hT(0)
            load_grp(1)
            for g, grp in enumerate(groups):
                gb = g % 2
                ntok = 128 * len(grp)
                tok0 = g * 512
                for c in range(nfm):
                    if c == nfm // 2:
                        if g + 1 < len(groups):
                            build_hT(g + 1)
                        if g + 2 < len(groups):
                            load_grp(g + 2)
                    pa = bank(2 + (ci % 2) * 2)
                    pka = f"ps{2 + (ci % 2) * 2}"
                    pbk = bank(3 + (ci % 2) * 2)
                    pkb = f"ps{3 + (ci % 2) * 2}"
                    roped = c >= perm_first
                    for k in range(8):
                        P.op("pe", lambda e, pa=pa, k=k, c=c, gb=gb, ntok=ntok: e.matmul(pa[:, 0:ntok], wfm[:, k, c * 128:(c + 1) * 128], hT[gb][:, k, 0:ntok], start=(k == 0), stop=(k == 7)),
                             reads=["wfm", f"hT{gb}"], writes=[pka])
                    sg = stg[si % 4]
                    sk = f"stg{si % 4}"
                    si += 1
                    if not roped:
                        if c % 2 == 0:
                            P.op("act", lambda e, sg=sg, pa=pa, ntok=ntok: e.copy(sg[:, 0:ntok], pa[:, 0:ntok]), reads=[pka], writes=[sk])
                        else:
                            P.op("dve", lambda e, sg=sg, pa=pa, ntok=ntok: e.tensor_copy(sg[:, 0:ntok], pa[:, 0:ntok]), reads=[pka], writes=[sk])
                    else:
                        cp = c - perm_first
                        for k in range(8):
                            P.op("pe", lambda e, pbk=pbk, k=k, cp=cp, gb=gb, ntok=ntok: e.matmul(pbk[:, 0:ntok], wfmp[:, k, cp * 128:(cp + 1) * 128], hT[gb][:, k, 0:ntok], start=(k == 0), stop=(k == 7)),
                                 reads=["wfmp", f"hT{gb}"], writes=[pkb])
                        tsel = 0 if (l == 0 or c < 8) else 1
                        tc, ts = tabs[tsel]
                        u = ci % 2
                        a1, a2 = t1[u], t2[u]
                        P.op("dve", lambda e, a1=a1, pa=pa, tc=tc, ntok=ntok, tok0=tok0: e.tensor_tensor(a1[:, 0:ntok], pa[:, 0:ntok], tc[:, tok0:tok0 + ntok], ALU.mult),
                             reads=[pka, f"tc{tsel}"], writes=[f"t1{u}"])
                        P.op("dve", lambda e, a2=a2, pbk=pbk, ts=ts, ntok=ntok, tok0=tok0: e.tensor_tensor(a2[:, 0:ntok], pbk[:, 0:ntok], ts[:, tok0:tok0 + ntok], ALU.mult),
                             reads=[pkb, f"ts{tsel}"], writes=[f"t2{u}"])
                        if l == 0:
                            P.op("pool", lambda e, sg=sg, a1=a1, a2=a2, ntok=ntok: e.tensor_tensor(sg[:, 0:ntok], a1[:, 0:ntok], a2[:, 0:ntok], ALU.add),
                                 reads=[f"t1{u}", f"t2{u}"], writes=[sk])
                        else:
                            s_q, r_s = sq[u], rs[u]
                            pc = bank(6)
                            P.op("act", lambda e, s_q=s_q, pa=pa, ntok=ntok: e.activation(s_q[:, 0:ntok], pa[:, 0:ntok], AF.Square), reads=[pka], writes=[f"sq{u}"])
                            P.op("dve", lambda e, a1=a1, a2=a2, ntok=ntok: e.tensor_tensor(a1[:, 0:ntok], a1[:, 0:ntok], a2[:, 0:ntok], ALU.add),
                                 reads=[f"t1{u}", f"t2{u}"], writes=[f"t1{u}"])

                            def tail(pc=pc, s_q=s_q, r_s=r_s, a1=a1, sg=sg, sk=sk, u=u, ntok=ntok, c=c, tok0=tok0):
                                P.op("pe", lambda e: e.matmul(pc[:, 0:ntok], blk, s_q[:, 0:ntok], start=True, stop=True),
                                     reads=[f"sq{u}", "blk"], writes=["ps6"])
                                P.op("act", lambda e: e.activation(r_s[:, 0:ntok], pc[:, 0:ntok], AF.Ln, bias=cst[:, 1:2], scale=1.0),
                                     reads=["ps6", "cst1"], writes=[f"rs{u}"])
                                P.op("act", lambda e: e.activation(r_s[:, 0:ntok], r_s[:, 0:ntok], AF.Exp, bias=cst[:, 2:3], scale=-0.5), reads=[f"rs{u}"], writes=[f"rs{u}"])
                                P.op("pool", lambda e: e.tensor_tensor(sg[:, 0:ntok], a1[:, 0:ntok], r_s[:, 0:ntok], ALU.mult),
                                     reads=[f"t1{u}", f"rs{u}"], writes=[sk])
                                P.dma("sp", qkT[l][b, c, :, tok0:tok0 + ntok], sg[:, 0:ntok], reads=[sk])
                            if tails:
                                tails.pop(0)()
                            tails.append(tail)
                            ci += 1
                            continue
                    P.dma("sp", qkT[l][b, c, :, tok0:tok0 + ntok], sg[:, 0:ntok], reads=[sk])
                    ci += 1
                while tails:
                    tails.pop(0)()
                for ti, (src, m) in enumerate(grp):
                    row0 = tok0 + ti * 128
                    for c0 in range(0, nv, 512):
                        n = min(512, nv - c0)
                        pv = bank(7)
                        for k in range(8):
                            P.op("pe", lambda e, pv=pv, k=k, gb=gb, ti=ti, c0=c0, n=n: e.matmul(pv[:, 0:n], hT[gb][:, k, ti * 128:(ti + 1) * 128], wv[:, k, c0:c0 + n], start=(k == 0), stop=(k == 7)),
                                 reads=["wv", f"hT{gb}"], writes=["ps7"])
                        sg = stg[si % 4]
                        sk = f"stg{si % 4}"
                        si += 1
                        if vi % 2 == 0:
                            P.op("act", lambda e, sg=sg, pv=pv, n=n: e.copy(sg[:, 0:n], pv[:, 0:n]), reads=["ps7"], writes=[sk])
                        else:
                            P.op("dve", lambda e, sg=sg, pv=pv, n=n: e.tensor_copy(sg[:, 0:n], pv[:, 0:n]), reads=["ps7"], writes=[sk])
                        vi += 1
                        P.dma("sp", vsc[l][b, row0:row0 + 128, c0:c0 + n], sg[:, 0:n], reads=[sk])
            P.barrier()

        PTv = psa[:, 2560:3712].bitcast(BF16)
        O_ps = [psa[:, 3840:3968], psa[:, 3968:4096]]

        def pt_keys(c0, c1):
            ks = set()
            for c in range(c0, c1):
                ks.add("ps5" if c < 8 else ("ps6" if c < 16 else "ps7"))
            return sorted(ks)

        def softmax_pv(S_ap, s_keys, nk, scale, chunks, Pb, pkey, PT, ptkey, st, stkey, o_idx, uid):
            P.op("dve", lambda e: e.reduce_max(st[:, 0:1], S_ap, axis=AX.X), reads=s_keys, writes=[stkey + "m"])
            P.op("dve", lambda e: e.tensor_scalar(st[:, 1:2], st[:, 0:1], -scale, None, ALU.mult), reads=[stkey + "m"], writes=[stkey + "n"])
            P.op("act", lambda e: e.activation(Pb[:, 0:nk], S_ap, AF.Exp, bias=st[:, 1:2], scale=scale, accum_out=st[:, 2:3]),
                 reads=list(s_keys) + [stkey + "n"], writes=[pkey, stkey + "s"])
            nch = len(chunks)
            for j, (c0, w, v_ap, vk) in enumerate(chunks):
                P.op("pe", lambda e, j=j, c0=c0, w=w: e.transpose(PTv[0:w, j * 128:(j + 1) * 128], Pb[:, c0:c0 + w], identb),
                     reads=[pkey, "identb"], writes=pt_keys(j, j + 1))
            segs = [(0, min(nch, 8)), (8, min(nch, 16)), (16, nch)]
            for si, (a, bnd) in enumerate(segs):
                if bnd <= a:
                    continue
                eng = "act" if (uid + si) % 2 == 0 else "dve"
                if eng == "act":
                    P.op("act", lambda e, a=a, bnd=bnd: e.copy(PT[:, a * 128:bnd * 128], PTv[:, a * 128:bnd * 128]), reads=pt_keys(a, bnd), writes=[ptkey + str(si)])
                else:
                    P.op("dve", lambda e, a=a, bnd=bnd: e.tensor_copy(PT[:, a * 128:bnd * 128], PTv[:, a * 128:bnd * 128]), reads=pt_keys(a, bnd), writes=[ptkey + str(si)])
            dv = chunks[0][2].shape[-1]
            Oa = O_ps[o_idx][:, 0:dv]
            for j, (c0, w, v_ap, vk) in enumerate(chunks):
                P.op("pe", lambda e, j=j, w=w, v_ap=v_ap: e.matmul(Oa, PT[0:w, j * 128:(j + 1) * 128], v_ap, start=(j == 0), stop=(j == nch - 1)),
                     reads=[ptkey + str(j // 8), vk], writes=["ps7"])
            P.op("dve", lambda e: e.reciprocal(st[:, 2:3], st[:, 2:3]), reads=[stkey + "s"], writes=[stkey + "s"])
            return Oa

        def phase_na(b):
            A.reset()
            kctx = A.bf(4, 256)
            vctx = A.bf(2, 512)
            bias = A.f32(8, 832)
            qz = [[A.bf(4, 128), A.bf(4, 128)] for _ in range(2)]
            kb = [A.bf(4, 576), A.bf(4, 576)]
            vb = [A.bf(5, 512), A.bf(5, 512)]
            Tt = [A.f32(832) for _ in range(3)]
            Pb = [A.bf(896) for _ in range(3)]
            PT = [A.bf(7 * 128), A.bf(7 * 128)]
            sts = [A.f32(8) for _ in range(6)]
            onat = [A.bf(512), A.bf(512)]
            PTp = [bank(5).bitcast(BF16), bank(6).bitcast(BF16)]
            Obk = [(bank(7), "ps7"), (bank(4), "ps4")]
            for u in range(2):
                P.op("pool", lambda e, u=u: e.memset(qz[u][0][64:128], 0.0), writes=[f"qzz{u}0"])
                P.op("pool", lambda e, u=u: e.memset(qz[u][1][0:64], 0.0), writes=[f"qzz{u}1"])
                P.op("pool", lambda e, u=u: e.memset(vb[u][64:128, 4, :], 0.0), writes=[f"vbz{u}"])
            P.dma("sp", kctx, qkT[0][b, 4:8, :, SEQ:T].rearrange("c p t -> p c t"), writes=["kctx"])
            P.dma("sp", vctx, vsc[0][b, SEQ:T, 0:512].rearrange("(j p) c -> p j c", p=128), writes=["vctx"])

            def load_tile(i):
                u = i % 2
                P.dma("sp", qz[u][0][0:64], qkT[0][b, 0:4, 0:64, i * 128:(i + 1) * 128].rearrange("c p t -> p c t"), writes=[f"qz{u}0"])
                P.dma("sp", qz[u][1][64:128], qkT[0][b, 0:4, 64:128, i * 128:(i + 1) * 128].rearrange("c p t -> p c t"), writes=[f"qz{u}1"])
                if i < 16:
                    K0 = min(max(2 * i - 4, 0), 23)
                    r0 = K0 * 64
                    P.dma("sp", kb[u], qkT[0][b, 4:8, :, r0:r0 + 576].rearrange("c p t -> p c t"), writes=[f"kb{u}"])
                    P.dma("sp", vb[u][:, 0:4, :], vsc[0][b, r0:r0 + 512, 0:512].rearrange("(j p) c -> p j c", p=128), writes=[f"vb{u}"])
                    P.dma("sp", vb[u][0:64, 4, :], vsc[0][b, r0 + 512:r0 + 576, 0:512], writes=[f"vb4{u}"])

            units = [(i, h) for i in range(18) for h in range(8)]
            pid_of = lambda i: {0: 0, 1: 1, 14: 3, 15: 4}.get(i, 2)
            state = {"pid": -1}

            def stage1(n):
                i, h = units[n]
                u = i % 2
                cb, var = h // 2, h % 2
                if h == 2 and i + 1 < 18:
                    load_tile(i + 1)
                if h == 0:
                    if i < 16 and pid_of(i) != state["pid"]:
                        P.dma("sp", bias, nabias_d[pid_of(i)], writes=["bias"])
                        state["pid"] = pid_of(i)
                su = n % 2
                sbank = psa[:, su * 1024:su * 1024 + 832]
                skeys = [f"ps{2 * su}", f"ps{2 * su + 1}"]
                st_ = sts[n % 6]
                stk = f"st{n % 6}"
                qap = qz[u][var][:, cb, :]
                qk = [f"qz{u}{var}", f"qzz{u}{var}"]
                pb = Pb[n % 3]
                if i < 16:
                    for (c0, w) in ((0, 512), (512, 64)):
                        P.op("pe", lambda e, sbank=sbank, qap=qap, u=u, cb=cb, c0=c0, w=w: e.matmul(sbank[:, c0:c0 + w], qap, kb[u][:, cb, c0:c0 + w], start=True, stop=True),
                             reads=qk + [f"kb{u}"], writes=skeys)
                    P.op("pe", lambda e, sbank=sbank, qap=qap, cb=cb: e.matmul(sbank[:, 576:832], qap, kctx[:, cb, :], start=True, stop=True),
                         reads=qk + ["kctx"], writes=skeys)
                    Tb = Tt[n % 3]
                    tk = f"T{n % 3}"
                    P.op("dve", lambda e, Tb=Tb, sbank=sbank, h=h: e.scalar_tensor_tensor(Tb, sbank, 0.125, bias[:, h, :], ALU.mult, ALU.add),
                         reads=skeys + ["bias"], writes=[tk])
                    src, srck, nk, scale = Tb, [tk], 832, 1.0
                else:
                    P.op("pe", lambda e, sbank=sbank, qap=qap, cb=cb: e.matmul(sbank[:, 0:256], qap, kctx[:, cb, :], start=True, stop=True),
                         reads=qk + ["kctx"], writes=skeys)
                    src, srck, nk, scale = sbank[:, 0:256], skeys, 256, 0.125
                P.op("dve", lambda e, st_=st_, src=src: e.reduce_max(st_[:, 0:1], src, axis=AX.X), reads=srck, writes=[stk + "m"])
                P.op("dve", lambda e, st_=st_, scale=scale: e.tensor_scalar(st_[:, 1:2], st_[:, 0:1], -scale, None, ALU.mult), reads=[stk + "m"], writes=[stk + "n"])
                P.op("act", lambda e, pb=pb, src=src, nk=nk, scale=scale, st_=st_: e.activation(pb[:, 0:nk], src, AF.Exp, bias=st_[:, 1:2], scale=scale, accum_out=st_[:, 2:3]),
                     reads=list(srck) + [stk + "n"], writes=[f"P{n % 3}", stk + "s"])

            def stage2(n):
                i, h = units[n]
                u = i % 2
                st_ = sts[n % 6]
                stk = f"st{n % 6}"
                pb = Pb[n % 3]
                ptp = PTp[n % 2]
                ptk = f"ps{5 + n % 2}"
                pts = PT[n % 2]
                ob, obk = Obk[n % 2]
                if i < 16:
                    chunks = [(j * 128, vb[u][:, j, h * 64:(h + 1) * 64], [f"vb{u}"]) for j in range(4)]
                    chunks.append((512, vb[u][:, 4, h * 64:(h + 1) * 64], [f"vb4{u}", f"vbz{u}"]))
                    chunks += [(576 + j * 128, vctx[:, j, h * 64:(h + 1) * 64], ["vctx"]) for j in range(2)]
                else:
                    chunks = [(j * 128, vctx[:, j, h * 64:(h + 1) * 64], ["vctx"]) for j in range(2)]
                nch = len(chunks)
                for j, (c0, v_ap, vk) in enumerate(chunks):
                    P.op("pe", lambda e, j=j, c0=c0, ptp=ptp, pb=pb: e.transpose(ptp[:, j * 128:(j + 1) * 128], pb[:, c0:c0 + 128], identb),
                         reads=[f"P{n % 3}", "identb"], writes=[ptk])
                P.op("dve", lambda e, pts=pts, ptp=ptp, nch=nch: e.tensor_copy(pts[:, 0:nch * 128], ptp[:, 0:nch * 128]), reads=[ptk], writes=[f"PT{n % 2}"])
                Oa = ob[:, 0:64]
                for j, (c0, v_ap, vk) in enumerate(chunks):
                    P.op("pe", lambda e, j=j, Oa=Oa, pts=pts, v_ap=v_ap, nch=nch: e.matmul(Oa, pts[:, j * 128:(j + 1) * 128], v_ap, start=(j == 0), stop=(j == nch - 1)),
                         reads=[f"PT{n % 2}"] + vk, writes=[obk])
                P.op("dve", lambda e, st_=st_: e.reciprocal(st_[:, 2:3], st_[:, 2:3]), reads=[stk + "s"], writes=[stk + "s"])
                P.op("dve", lambda e, Oa=Oa, u=u, h=h, st_=st_: e.tensor_scalar(onat[u][:, h * 64:(h + 1) * 64], Oa, st_[:, 2:3], None, ALU.mult),
                     reads=[obk, stk + "s"], writes=[f"onat{u}"])
                if h == 7:
                    P.dma("sp", att[b, i * 128:(i + 1) * 128, 0:512], onat[u], reads=[f"onat{u}"])

            load_tile(0)
            N = len(units)
            stage1(0)
            stage1(1)
            for n in range(N):
                stage2(n)
                if n + 2 < N:
                    stage1(n + 2)
            P.barrier()

        def phase_full_attn(l, b):
            A.reset()
            dv = 128 if l == 0 else 64
            nsub = 2 if l == 0 else 4
            kT = [A.bf(T), A.bf(T)]
            qTa = [A.bf(2, T) if l == 1 else A.bf(1, T) for _ in range(2)]
            Vt = [A.bf(18, dv + (1 if l == 1 else 0)) for _ in range(2)]
            nqc, nqt = (2, SEQ) if l == 1 else (1, T)
            qz = [[A.bf(nqc, nqt), A.bf(nqc, nqt)] for _ in range(2)]
            Pc = [A.bf(2, 512) for _ in range(3)]
            onesb = A.bf(128)
            onesf = A.f32(128)
            rrow = [A.f32(512), A.f32(512)]
            bcs = [A.f32(512), A.f32(512)]
            outb = [A.bf(512), A.bf(512)]
            negB = [A.f32(8), A.f32(8)]
            kst = A.f32(32)
            if l == 0:
                ksq = A.bf(T)
                qsq = A.bf(T)
                sel = [A.bf(128), A.bf(128)]
                i0 = [A.f32(512), A.f32(512)]
                aa = A.f32(512)
                cc_ = A.f32(512)
                oo = [A.f32(512), A.f32(512)]
                sq = [A.f32(512), A.f32(512)]
                lnv = [A.f32(512), A.f32(512)]
                subgc = A.f32(8)
            else:
                grow = A.f32(128)
            Sb = [bank(0), bank(1), bank(2)]
            Sb2 = [psa[:, 0:1024].rearrange("p (a n) -> p a n", a=2), psa[:, 1024:2048].rearrange("p (a n) -> p a n", a=2)]
            Ob = [bank(4), bank(5)]
            Rb = [bank(6), bank(7)]
            P.op("dve", lambda e: e.memset(onesb, 1.0), writes=["onesb"])
            P.op("dve", lambda e: e.memset(onesf, 1.0), writes=["onesf"])
            for gu_ in range(2):
                P.op("pool", lambda e, gu_=gu_: e.memset(qz[gu_][0][64:128], 0.0), writes=[f"qzz{gu_}0"])
                P.op("pool", lambda e, gu_=gu_: e.memset(qz[gu_][1][0:64], 0.0), writes=[f"qzz{gu_}1"])
            if l == 0:
                for r in range(2):
                    P.op("dve", lambda e, r=r: e.memset(sel[r], 0.0), writes=[f"sel{r}"])
                    P.op("dve", lambda e, r=r: e.memset(sel[r][r * 64:(r + 1) * 64, :], 1.0), writes=[f"sel{r}"])
                P.dma("sp", subgc[:, 0:1], subgc_d, writes=["subgc"])
                P.op("dve", lambda e: e.tensor_scalar(subgc[:, 0:1], subgc[:, 0:1], 1.0 - LAM_INIT0, None, ALU.mult), reads=["subgc"], writes=["subgc"])
                P.op("dve", lambda e: e.tensor_scalar(subgc[:, 1:2], lamt[:, 0:1], -1.0, None, ALU.mult), writes=["neglam"])
            else:
                P.dma("sp", grow, grow_d.partition_broadcast(128), writes=["grow"])
                P.op("dve", lambda e: e.reduce_max(kst[:, 0:1], grow[:, 0:64], axis=AX.X, apply_absolute_value=True), reads=["grow"], writes=["kst0"])
                P.op("dve", lambda e: e.reduce_max(kst[:, 1:2], grow[:, 64:128], axis=AX.X, apply_absolute_value=True), reads=["grow"], writes=["kst1"])
                for gu in range(2):
                    P.op("dve", lambda e, gu=gu: e.scalar_tensor_tensor(negB[gu][:, 0:1], kst[:, 0:1], -8.0, kst[:, 1:2], ALU.mult, ALU.mult), reads=["kst0", "kst1"], writes=[f"negB{gu}_0"])

            def load_g(g):
                gu = g % 2
                if l == 0:
                    P.dma("sp", kT[gu], qkT[0][b, 12 + g], writes=[f"kT{gu}"])
                    P.dma("sp", qTa[gu][:, 0, :], qkT[0][b, 8 + g], writes=[f"qTa{gu}"])
                    P.dma("sp", qz[gu][0][0:64, 0, :], qkT[0][b, 8 + g, 0:64, :], writes=[f"qz{gu}0"])
                    P.dma("sp", qz[gu][1][64:128, 0, :], qkT[0][b, 8 + g, 64:128, :], writes=[f"qz{gu}1"])
                    P.dma("sp", Vt[gu], vsc[0][b, :, 512 + 128 * g:512 + 128 * (g + 1)].rearrange("(j p) c -> p j c", p=128), writes=[f"V{gu}"])
                else:
                    P.dma("sp", kT[gu], qkT[1][b, 8 + g], writes=[f"kT{gu}"])
                    P.dma("sp", qz[gu][0][0:64], qkT[1][b, 2 * g:2 * g + 2, 0:64, 0:SEQ].rearrange("c p t -> p c t"), writes=[f"qz{gu}0"])
                    P.dma("sp", qz[gu][1][64:128], qkT[1][b, 2 * g:2 * g + 2, 64:128, 0:SEQ].rearrange("c p t -> p c t"), writes=[f"qz{gu}1"])
                    P.dma("sp", Vt[gu][:, :, 0:64], vsc[1][b, :, 64 * g:64 * (g + 1)].rearrange("(j p) c -> p j c", p=128), writes=[f"V{gu}"])
                    P.op("pool", lambda e, gu=gu: e.memset(Vt[gu][:, :, 64:65], 1.0), writes=[f"V{gu}"])

            def group_prologue(g):
                gu = g % 2
                if l != 0:
                    return
                P.op("pool", lambda e, gu=gu: e.tensor_tensor(ksq, kT[gu], kT[gu], ALU.mult), reads=[f"kT{gu}"], writes=["ksq"])
                P.op("pool", lambda e, gu=gu: e.tensor_tensor(qsq, qTa[gu][:, 0, :], qTa[gu][:, 0, :], ALU.mult), reads=[f"qTa{gu}"], writes=["qsq"])
                for r in range(2):
                    for si, (src, sk_) in enumerate(((ksq, "ksq"), (qsq, "qsq"))):
                        for cc in range(5):
                            w = min(512, T - cc * 512)
                            bi = (si * 5 + cc) % 3
                            pb = Sb[bi]
                            P.op("pe", lambda e, pb=pb, r=r, cc=cc, w=w, src=src: e.matmul(pb[:, 0:w], sel[r], src[:, cc * 512:cc * 512 + w], start=True, stop=True),
                                 reads=[sk_, f"sel{r}"], writes=[f"ps{bi}"])
                            col = 8 + si * 5 + cc
                            P.op("dve", lambda e, pb=pb, col=col, w=w: e.reduce_max(kst[:, col:col + 1], pb[:, 0:w], axis=AX.X), reads=[f"ps{bi}"], writes=[f"kstc{col}"])
                    P.op("dve", lambda e, r=r: e.reduce_max(kst[:, 4:5], kst[:, 8:13], axis=AX.X), reads=[f"kstc{c}" for c in range(8, 13)], writes=["kstk"])
                    P.op("dve", lambda e, r=r: e.reduce_max(kst[:, 5:6], kst[:, 13:18], axis=AX.X), reads=[f"kstc{c}" for c in range(13, 18)], writes=["kstq"])
                    P.op("dve", lambda e: e.tensor_tensor(kst[:, 6:7], kst[:, 4:5], kst[:, 5:6], ALU.add), reads=["kstk", "kstq"], writes=["kstsum"])
                    P.op("dve", lambda e, r=r, gu=gu: e.tensor_scalar(negB[gu][:, r:r + 1], kst[:, 6:7], -0.0625, None, ALU.mult), reads=["kstsum"], writes=[f"negB{gu}_{r}"])

            units = []
            for g in range(4):
                for G in range(5 if l == 0 else 4):
                    for r in range(nsub):
                        units.append((g, G, r))
            items = []
            for ui, (g, G, r) in enumerate(units):
                kcs = list(range(16, 18)) if G == 4 else list(range(18))
                for ki in range(0, len(kcs), 2):
                    items.append((ui, ki // 2, kcs[ki], len(kcs) // 2))
            n = len(items)
            state = {"oi": 0}
            pending = []

            def qcols(G):
                return (SEQ, NCTX) if G == 4 else (G * 512, 512)

            def stage_S(k):
                ui, ki, kc, nkc = items[k]
                g, G, r = units[ui]
                gu = g % 2
                if ki == 0 and G == 0 and r == 0:
                    group_prologue(g)
                if ki == 0 and G == 1 and r == 0 and g + 1 < 4:
                    load_g(g + 1)
                q0, nq = qcols(G)
                if l == 0:
                    var = r
                    qap = qz[gu][var][:, 0, q0:q0 + nq]
                    nbc = r
                else:
                    var = (4 * g + r) % 2
                    qap = qz[gu][var][:, r // 2, q0:q0 + nq]
                    nbc = 0
                sb = Sb2[k % 2]
                skeys = [f"ps{2 * (k % 2)}", f"ps{2 * (k % 2) + 1}"]
                for a_ in range(2):
                    P.op("pe", lambda e, sb=sb, qap=qap, gu=gu, kc=kc, nq=nq, a_=a_: e.matmul(sb[:, a_, 0:nq], kT[gu][:, (kc + a_) * 128:(kc + a_ + 1) * 128], qap, start=True, stop=True),
                         reads=[f"qz{gu}{var}", f"qzz{gu}{var}", f"kT{gu}"], writes=[skeys[a_]])
                pc = Pc[k % 3]
                P.op("act", lambda e, pc=pc, sb=sb, nq=nq, gu=gu, nbc=nbc: e.activation(pc[:, :, 0:nq], sb[:, :, 0:nq], AF.Exp, bias=negB[gu][:, nbc:nbc + 1], scale=0.125),
                     reads=skeys + [f"negB{gu}_{nbc}"], writes=[f"Pc{k % 3}"])

            def stage_V(k):
                ui, ki, kc, nkc = items[k]
                g, G, r = units[ui]
                gu = g % 2
                q0, nq = qcols(G)
                pc = Pc[k % 3]
                ob = Ob[ui % 2]
                rb = Rb[ui % 2]
                okey, rkey = f"ps{4 + ui % 2}", f"ps{6 + ui % 2}"
                M = dv + (1 if l == 1 else 0)
                for a_ in range(2):
                    P.op("pe", lambda e, ob=ob, pc=pc, gu=gu, kc=kc, nq=nq, M=M, ki=ki, nkc=nkc, a_=a_: e.matmul(ob[0:M, 0:nq], Vt[gu][:, kc + a_, 0:M], pc[:, a_, 0:nq], start=(ki == 0 and a_ == 0), stop=(ki == nkc - 1 and a_ == 1)),
                         reads=[f"Pc{k % 3}", f"V{gu}"], writes=[okey])
                if l == 0:
                    for a_ in range(2):
                        P.op("pe", lambda e, rb=rb, pc=pc, nq=nq, ki=ki, nkc=nkc, a_=a_: e.matmul(rb[:, 0:nq], onesb, pc[:, a_, 0:nq], start=(ki == 0 and a_ == 0), stop=(ki == nkc - 1 and a_ == 1)),
                             reads=[f"Pc{k % 3}", "onesb"], writes=[rkey])
                if ki != nkc - 1:
                    return
                if l == 1:
                    oi = state["oi"]
                    state["oi"] += 1
                    u2 = oi % 2
                    P.op("dve", lambda e, u2=u2, ob=ob, nq=nq: e.reciprocal(rrow[u2][64:65, 0:nq], ob[64:65, 0:nq]), reads=[okey], writes=[f"rrow{u2}"])

                    def part_b(u2=u2, ob=ob, rb=rb, nq=nq, q0=q0, g=g, r=r, okey=okey, rkey=rkey):
                        P.op("pe", lambda e: e.matmul(rb[0:64, 0:nq], onesf[64:65, 0:64], rrow[u2][64:65, 0:nq], start=True, stop=True),
                             reads=[f"rrow{u2}", "onesf"], writes=[rkey])
                        P.op("dve", lambda e: e.tensor_copy(bcs[u2][0:64, 0:nq], rb[0:64, 0:nq]), reads=[rkey], writes=[f"bcs{u2}"])
                        P.op("dve", lambda e: e.tensor_tensor(outb[u2][0:64, 0:nq], ob[0:64, 0:nq], bcs[u2][0:64, 0:nq], ALU.mult), reads=[okey, f"bcs{u2}"], writes=[f"outb{u2}"])
                        hq = 4 * g + r
                        pb0 = (hq % 2) * 64
                        P.dma("sp", attT[b, hq // 2, pb0:pb0 + 64, q0:q0 + nq], outb[u2][0:64, 0:nq], reads=[f"outb{u2}"])
                    pending.append((k + 2, part_b))
                else:
                    if r == 0:
                        P.op("dve", lambda e, rb=rb, nq=nq: e.reciprocal(i0[0][:, 0:nq], rb[:, 0:nq]), reads=[rkey], writes=["i0a"])
                        P.op("dve", lambda e, ob=ob, nq=nq: e.tensor_tensor(aa[:, 0:nq], ob[:, 0:nq], i0[0][:, 0:nq], ALU.mult), reads=[okey, "i0a"], writes=["aa"])
                        return
                    oi = state["oi"]
                    state["oi"] += 1
                    u2 = oi % 2
                    P.op("dve", lambda e, rb=rb, nq=nq: e.reciprocal(i0[1][:, 0:nq], rb[:, 0:nq]), reads=[rkey], writes=["i0b"])
                    P.op("dve", lambda e, ob=ob, nq=nq: e.tensor_tensor(cc_[:, 0:nq], ob[:, 0:nq], i0[1][:, 0:nq], ALU.mult), reads=[okey, "i0b"], writes=["cc"])
                    P.op("dve", lambda e, u2=u2, nq=nq: e.scalar_tensor_tensor(oo[u2][:, 0:nq], cc_[:, 0:nq], subgc[:, 1:2], aa[:, 0:nq], ALU.mult, ALU.add), reads=["cc", "aa", "neglam"], writes=[f"oo{u2}"])
                    P.op("pool", lambda e, u2=u2, nq=nq: e.tensor_tensor(sq[u2][:, 0:nq], oo[u2][:, 0:nq], oo[u2][:, 0:nq], ALU.mult), reads=[f"oo{u2}"], writes=[f"sq{u2}"])

                    def part_b0(u2=u2, nq=nq, rb=rb, rkey=rkey, q0=q0, g=g):
                        P.op("pe", lambda e: e.matmul(rb[:, 0:nq], onesf, sq[u2][:, 0:nq], start=True, stop=True), reads=[f"sq{u2}", "onesf"], writes=[rkey])
                        P.op("act", lambda e: e.activation(lnv[u2][:, 0:nq], rb[:, 0:nq], AF.Ln, bias=cst[:, 1:2], scale=1.0 / 128.0), reads=[rkey], writes=[f"lnv{u2}"])
                        P.op("act", lambda e: e.activation(lnv[u2][:, 0:nq], lnv[u2][:, 0:nq], AF.Exp, bias=cst[:, 2:3], scale=-0.5), reads=[f"lnv{u2}"], writes=[f"lnv{u2}"])
                        P.op("dve", lambda e: e.scalar_tensor_tensor(outb[u2][:, 0:nq], oo[u2][:, 0:nq], subgc[:, 0:1], lnv[u2][:, 0:nq], ALU.mult, ALU.mult), reads=[f"oo{u2}", f"lnv{u2}", "subgc"], writes=[f"outb{u2}"])
                        P.dma("sp", attT[b, 4 + g, :, q0:q0 + nq], outb[u2][:, 0:nq], reads=[f"outb{u2}"])
                    pending.append((k + 2, part_b0))

            load_g(0)
            for k in range(-1, n):
                if k + 1 < n:
                    stage_S(k + 1)
                if k >= 0:
                    stage_V(k)
                while pending and pending[0][0] <= k:
                    pending.pop(0)[1]()
            while pending:
                pending.pop(0)[1]()
            P.barrier()

        def ln_ops(z, zk, o, okey, lng, lnb, st, stk, bn, n_t, nk):
            return [
                lambda: P.op("dve", lambda e: e.bn_stats(bn[:, 0, :], z[:, 0:512]), reads=[zk], writes=[stk + "b0"]),
                lambda: P.op("dve", lambda e: e.bn_stats(bn[:, 1, :], z[:, 512:1024]), reads=[zk], writes=[stk + "b1"]),
                lambda: P.op("dve", lambda e: e.bn_aggr(st[:, 0:2], bn.rearrange("p a b -> p (a b)")), reads=[stk + "b0", stk + "b1"], writes=[stk + "mv"]),
                lambda: P.op("act", lambda e: e.activation(st[:, 2:3], st[:, 1:2], AF.Sqrt, bias=cst[:, 0:1], scale=1.0), reads=[stk + "mv"], writes=[stk + "sd"]),
                lambda: P.op("dve", lambda e: e.reciprocal(st[:, 2:3], st[:, 2:3]), reads=[stk + "sd"], writes=[stk + "sd"]),
                lambda: P.op("dve", lambda e: e.scalar_tensor_tensor(st[:, 3:4], st[:, 0:1], -1.0, st[:, 2:3], ALU.mult, ALU.mult), reads=[stk + "mv", stk + "sd"], writes=[stk + "nb"]),
                lambda: P.op("act", lambda e: e.activation(n_t, z, AF.Identity, bias=st[:, 3:4], scale=st[:, 2:3]), reads=[zk, stk + "nb", stk + "sd"], writes=[nk]),
                lambda: P.op("pool", lambda e: e.tensor_tensor(n_t, n_t, lng, ALU.mult), reads=[nk, "lng"], writes=[nk]),
                lambda: P.op("dve", lambda e: e.tensor_tensor(o, n_t, lnb, ALU.add), reads=[nk, "lnb"], writes=[okey]),
            ]

        def zip_run(lists):
            n_ = max(len(x) for x in lists)
            for j in range(n_):
                for x in lists:
                    if j < len(x):
                        x[j]()

        def phase_outproj(l, b, Xsrc, Xdst, ntl):
            A.reset()
            wo = A.bf(8, 1024)
            gbc = [A.f32(1024), A.f32(1024)]
            lng = A.f32(1024)
            lnb = A.f32(1024)
            at = [A.bf(1024), A.bf(1024)]
            aT = [A.bf(8, 128), A.bf(8, 128)]
            xt = [A.f32(1024), A.f32(1024)]
            z = [A.f32(1024), A.f32(1024)]
            n_t = [A.f32(1024), A.f32(1024)]
            o = [A.f32(1024), A.f32(1024)]
            bn = [A.f32(2, 6), A.f32(2, 6)]
            sts = [A.f32(8), A.f32(8)]
            for c0 in (0, 512):
                P.dma("pool", wo[:, :, c0:c0 + 512], L[l]["w_out"][:, c0:c0 + 512].rearrange("(k p) n -> p k n", p=128), writes=["wo"])
            P.dma("sp", gbc[0], gates_d[l, 0, b], writes=["gbc0"])
            P.dma("sp", gbc[1], gates_d[l, 0, 2], writes=["gbc1"])
            P.dma("sp", lng, L[l]["ln"][0:1, :].partition_broadcast(128), writes=["lng"])
            P.dma("sp", lnb, L[l]["ln"][1:2, :].partition_broadcast(128), writes=["lnb"])
            tl = tiles_of(Xsrc, b, nlat=16, nctx=ntl - 16, l0=(l == 0))
            def load_t(i):
                uu = i % 2
                if l == 0:
                    P.dma("sp", at[uu][:, 0:512], att[b, i * 128:(i + 1) * 128, 0:512], writes=[f"at{uu}"])
                    P.dma("sp", aT[uu][:, 4:8, :], attT[b, 4:8, :, i * 128:(i + 1) * 128].rearrange("c p t -> p c t"), writes=[f"aTd{uu}"])
                else:
                    P.dma("sp", aT[uu], attT[b, :, :, i * 128:(i + 1) * 128].rearrange("c p t -> p c t"), writes=[f"aTd{uu}"])
                P.dma("sp", xt[uu], tl[i][0], writes=[f"xt{uu}"])

            def stage_A(i):
                u = i % 2
                src, m = tl[i]
                akeys = [f"aTd{u}"]
                if l == 0:
                    ptb = bank(u).bitcast(BF16)
                    for k in range(4):
                        P.op("pe", lambda e, ptb=ptb, k=k, u=u: e.transpose(ptb[:, k * 128:(k + 1) * 128], at[u][:, k * 128:(k + 1) * 128], identb),
                             reads=[f"at{u}", "identb"], writes=[f"ps{u}"])
                    P.op("dve", lambda e, u=u, ptb=ptb: e.tensor_copy(aT[u][:, 0:4, :].rearrange("p k t -> p (k t)"), ptb[:, 0:512]), reads=[f"ps{u}"], writes=[f"aT{u}"])
                    akeys.append(f"aT{u}")
                yb = psa[:, 1024 + u * 1024:2048 + u * 1024]
                for half in range(2):
                    for k in range(8):
                        P.op("pe", lambda e, yb=yb, half=half, k=k, u=u: e.matmul(yb[:, half * 512:(half + 1) * 512], aT[u][:, k, :], wo[:, k, half * 512:(half + 1) * 512], start=(k == 0), stop=(k == 7)),
                             reads=akeys + ["wo"], writes=[f"ps{2 + 2 * u + half}"])
                gsel = 0 if m < 2 else 1
                P.op("dve", lambda e, u=u, yb=yb, gsel=gsel: e.tensor_tensor(z[u], yb, gbc[gsel], ALU.mult), reads=[f"ps{2 + 2 * u}", f"ps{3 + 2 * u}", f"gbc{gsel}"], writes=[f"z{u}"])
                P.op("dve", lambda e, u=u: e.scalar_tensor_tensor(z[u], xt[u], ALPHA, z[u], ALU.mult, ALU.add), reads=[f"xt{u}", f"z{u}"], writes=[f"z{u}"])

            def stage_B_ops(i):
                u = i % 2
                return ln_ops(z[u], f"z{u}", o[u], f"o{u}", lng, lnb, sts[u], f"st{u}", bn[u], n_t[u], f"n{u}") + \
                    [lambda: P.dma("sp", Xdst[b, i * 128:(i + 1) * 128, :], o[u], reads=[f"o{u}"])]

            load_t(0)
            load_t(1)
            for i in range(0, len(tl), 2):
                stage_A(i)
                stage_A(i + 1)
                if i + 2 < len(tl):
                    load_t(i + 2)
                    load_t(i + 3)
                zip_run([stage_B_ops(i), stage_B_ops(i + 1)])
            P.barrier()


        def phase_ffn(l, tl, dsts, final):
            A.reset()
            NT = len(tl)
            moe = (l == 1)
            FD = EDIM if moe else FFN
            nexp = NEXP if moe else 1
            hT = A.bf(8, NT * 128)
            yacc = A.f32(NT, 1024)
            G = A.f32(NT, 8)
            mark = A.off
            xt = [A.f32(1024), A.f32(1024)]
            hTf = [A.f32(8, 128), A.f32(8, 128)]
            rw = A.f32(8, 8)
            lg = [A.f32(8), A.f32(8)]
            ex = [A.f32(8), A.f32(8)]
            sts = [A.f32(16), A.f32(16)]
            if moe:
                P.dma("sp", rw, router_d.rearrange("(k p) n -> p k n", p=128), writes=["rw"])
            cnt = [0]
            P.dma("sp", xt[0], tl[0][0], writes=["xt0"])
            for i, (src, m) in enumerate(tl):
                u = i % 2
                if i + 1 < NT:
                    P.dma("sp", xt[(i + 1) % 2], tl[i + 1][0], writes=[f"xt{(i + 1) % 2}"])
                if moe:
                    emit_hT(l, 1, xt[u], f"xt{u}", m, hT, "hT", i * 128, [bank(0), bank(1)], ["ps0", "ps1"], cnt, hTf=hTf[u], hfk=f"hTf{u}")
                    pl = bank(2 + u, 8)
                    for k in range(8):
                        P.op("pe", lambda e, pl=pl, k=k, u=u: e.matmul(pl, hTf[u][:, k, :], rw[:, k, :], start=(k == 0), stop=(k == 7)),
                             reads=[f"hTf{u}", "rw"], writes=[f"ps{2 + u}"])
                    s_ = sts[u]
                    sk = f"rst{u}"
                    P.op("dve", lambda e, u=u, pl=pl: e.tensor_copy(lg[u], pl), reads=[f"ps{2 + u}"], writes=[f"lg{u}"])
                    P.op("dve", lambda e, u=u, s_=s_: e.max(s_[:, 0:8], lg[u]), reads=[f"lg{u}"], writes=[sk + "a"])
                    P.op("dve", lambda e, s_=s_: e.tensor_scalar(s_[:, 8:9], s_[:, 0:1], -1.0, None, ALU.mult), reads=[sk + "a"], writes=[sk + "b"])
                    P.op("act", lambda e, u=u, s_=s_: e.activation(ex[u], lg[u], AF.Exp, bias=s_[:, 8:9], scale=1.0), reads=[f"lg{u}", sk + "b"], writes=[f"ex{u}"])
                    P.op("dve", lambda e, u=u, s_=s_: e.tensor_scalar(lg[u], lg[u], s_[:, 1:2], None, ALU.is_ge), reads=[f"lg{u}", sk + "a", f"ex{u}"], writes=[f"lg{u}"])
                    P.op("dve", lambda e, u=u: e.tensor_tensor(ex[u], ex[u], lg[u], ALU.mult), reads=[f"lg{u}", f"ex{u}"], writes=[f"ex{u}"])
                    P.op("dve", lambda e, u=u, s_=s_: e.reduce_sum(s_[:, 9:10], ex[u], axis=AX.X), reads=[f"ex{u}"], writes=[sk + "c"])
                    P.op("dve", lambda e, s_=s_: e.reciprocal(s_[:, 9:10], s_[:, 9:10]), reads=[sk + "c"], writes=[sk + "c"])
                    P.op("dve", lambda e, u=u, i=i, s_=s_: e.tensor_scalar(G[:, i, :], ex[u], s_[:, 9:10], None, ALU.mult), reads=[f"ex{u}", sk + "c"], writes=["G"])
                else:
                    emit_hT(l, 1, xt[u], f"xt{u}", m, hT, "hT", i * 128, [bank(0), bank(1)], ["ps0", "ps1"], cnt)
            P.barrier()
            A.off = mark
            wa = [A.bf(8, 512), A.bf(8, 512)]
            wg = [A.bf(8, 512), A.bf(8, 512)]
            w2 = [A.bf(4, 1024), A.bf(4, 1024)]
            uT = [A.bf(4, 512), A.bf(4, 512)]
            sa = [A.f32(512), A.f32(512)]
            ev = [A.f32(1024), A.f32(1024)]
            fchunks = []
            f0 = 0
            while f0 < FD:
                fw = min(512, FD - f0)
                fchunks.append((f0, fw))
                f0 += fw
            tgs = [(t0, min(4, NT - t0)) for t0 in range(0, NT, 4)]
            wlist = [(ex_i, f0, fw) for ex_i in range(nexp) for (f0, fw) in fchunks]
            items = []
            for wi, (ex_i, f0, fw) in enumerate(wlist):
                for (t0, nt) in tgs:
                    items.append((ex_i, f0, fw, t0, nt, wi))
            loaded = set()

            def load_w(wi_):
                if wi_ in loaded or wi_ >= len(wlist):
                    return
                loaded.add(wi_)
                ex_i, f0, fw = wlist[wi_]
                wu = wi_ % 2
                if moe:
                    w13s, w2s = w13_1[ex_i], w2_1[ex_i]
                else:
                    w13s, w2s = w13_0, w2_0
                P.dma("pool", wa[wu][:, :, 0:fw], w13s[:, f0:f0 + fw].rearrange("(k p) n -> p k n", p=128), writes=[f"wa{wu}"])
                P.dma("pool", wg[wu][:, :, 0:fw], w13s[:, FD + f0:FD + f0 + fw].rearrange("(k p) n -> p k n", p=128), writes=[f"wg{wu}"])
                P.dma("pool", w2[wu][:, 0:fw // 128, :], w2s[f0:f0 + fw, :].rearrange("(j p) n -> p j n", p=128), writes=[f"w2{wu}"])

            upc = [0]

            def emit_up(it, n):
                ex_i, f0, fw, t0, nt, wi_ = it
                wu = wi_ % 2
                uu = n % 2
                ntok = nt * 128
                for fs in range(fw // 128):
                    q = upc[0] % 2
                    upc[0] += 1
                    pa, pg = bank(q), bank(2 + q)
                    for k in range(8):
                        P.op("pe", lambda e, pa=pa, k=k, fs=fs, wu=wu, t0=t0, ntok=ntok: e.matmul(pa[:, 0:ntok], wa[wu][:, k, fs * 128:(fs + 1) * 128], hT[:, k, t0 * 128:t0 * 128 + ntok], start=(k == 0), stop=(k == 7)),
                             reads=[f"wa{wu}"], writes=[f"ps{q}"])
                    for k in range(8):
                        P.op("pe", lambda e, pg=pg, k=k, fs=fs, wu=wu, t0=t0, ntok=ntok: e.matmul(pg[:, 0:ntok], wg[wu][:, k, fs * 128:(fs + 1) * 128], hT[:, k, t0 * 128:t0 * 128 + ntok], start=(k == 0), stop=(k == 7)),
                             reads=[f"wg{wu}"], writes=[f"ps{2 + q}"])
                    P.op("act", lambda e, q=q, pa=pa, ntok=ntok: e.activation(sa[q][:, 0:ntok], pa[:, 0:ntok], AF.Silu), reads=[f"ps{q}"], writes=[f"sa{q}"])
                    P.op("dve", lambda e, q=q, pg=pg, uu=uu, fs=fs, ntok=ntok: e.tensor_tensor(uT[uu][:, fs, 0:ntok], sa[q][:, 0:ntok], pg[:, 0:ntok], ALU.mult),
                         reads=[f"sa{q}", f"ps{2 + q}"], writes=[f"uT{uu}"])

            dnc = [0]
            first_done = set()

            def emit_down(it, n):
                ex_i, f0, fw, t0, nt, wi_ = it
                wu = wi_ % 2
                uu = n % 2
                nfs = fw // 128
                for tt in range(nt):
                    ti = t0 + tt
                    q = dnc[0] % 2
                    dnc[0] += 1
                    pd = psa[:, 2048 + q * 1024:3072 + q * 1024]
                    for oh in range(2):
                        for fs in range(nfs):
                            P.op("pe", lambda e, pd=pd, oh=oh, fs=fs, uu=uu, tt=tt, wu=wu, nfs=nfs: e.matmul(pd[:, oh * 512:(oh + 1) * 512], uT[uu][:, fs, tt * 128:(tt + 1) * 128], w2[wu][:, fs, oh * 512:(oh + 1) * 512], start=(fs == 0), stop=(fs == nfs - 1)),
                                 reads=[f"uT{uu}", f"w2{wu}"], writes=[f"ps{4 + 2 * q + oh}"])
                    yk = f"y{ti}"
                    first = ti not in first_done
                    first_done.add(ti)
                    if moe:
                        gsc = G[:, ti, ex_i:ex_i + 1]
                        if first:
                            P.op("act", lambda e, pd=pd, ti=ti, gsc=gsc: e.activation(yacc[:, ti, :], pd, AF.Identity, bias=cst[:, 2:3], scale=gsc), reads=[f"ps{4 + 2 * q}", f"ps{5 + 2 * q}"], writes=[yk])
                        else:
                            P.op("act", lambda e, pd=pd, q=q, gsc=gsc: e.activation(ev[q], pd, AF.Identity, bias=cst[:, 2:3], scale=gsc), reads=[f"ps{4 + 2 * q}", f"ps{5 + 2 * q}"], writes=[f"ev{q}"])
                            eng = "pool" if ti % 2 == 0 else "dve"
                            P.op(eng, lambda e, ti=ti, q=q: e.tensor_tensor(yacc[:, ti, :], yacc[:, ti, :], ev[q], ALU.add), reads=[f"ev{q}", yk], writes=[yk])
                    else:
                        if first:
                            P.op("act", lambda e, pd=pd, ti=ti: e.copy(yacc[:, ti, :], pd), reads=[f"ps{4 + 2 * q}", f"ps{5 + 2 * q}"], writes=[yk])
                        else:
                            P.op("dve", lambda e, pd=pd, ti=ti: e.tensor_tensor(yacc[:, ti, :], yacc[:, ti, :], pd, ALU.add), reads=[f"ps{4 + 2 * q}", f"ps{5 + 2 * q}", yk], writes=[yk])

            load_w(0)
            for n, it in enumerate(items):
                if n == 0 or items[n - 1][5] != it[5]:
                    load_w(it[5] + 1)
                if n == 0:
                    emit_up(it, 0)
                if n + 1 < len(items):
                    emit_up(items[n + 1], n + 1)
                emit_down(it, n)
            P.barrier()
            A.off = mark
            xt = [A.f32(1024), A.f32(1024)]
            gbm = [A.f32(1024), A.f32(1024), A.f32(1024)]
            lng = A.f32(1024)
            lnb = A.f32(1024)
            z = [A.f32(1024), A.f32(1024)]
            n_t = [A.f32(1024), A.f32(1024)]
            o = [A.f32(1024), A.f32(1024)]
            bn = [A.f32(2, 6), A.f32(2, 6)]
            st2 = [A.f32(8), A.f32(8)]
            ms = sorted(set(m for _, m in tl))
            for m in ms:
                P.dma("sp", gbm[m], gates_d[l, 1, m], writes=[f"gbm{m}"])
            P.dma("sp", lng, L[l]["ln"][2:3, :].partition_broadcast(128), writes=["lng"])
            P.dma("sp", lnb, L[l]["ln"][3:4, :].partition_broadcast(128), writes=["lnb"])
            def ld3(i):
                P.dma("sp", xt[i % 2], tl[i][0], writes=[f"xt{i % 2}"])

            def stA(i):
                u = i % 2
                m = tl[i][1]
                P.op("dve", lambda e, u=u, i=i, m=m: e.tensor_tensor(z[u], yacc[:, i, :], gbm[m], ALU.mult), reads=[f"gbm{m}"], writes=[f"z{u}"])
                P.op("dve", lambda e, u=u: e.scalar_tensor_tensor(z[u], xt[u], ALPHA, z[u], ALU.mult, ALU.add), reads=[f"xt{u}", f"z{u}"], writes=[f"z{u}"])

            def stB_ops(i):
                u = i % 2
                return ln_ops(z[u], f"z{u}", o[u], f"o{u}", lng, lnb, st2[u], f"lst{u}", bn[u], n_t[u], f"n{u}") + \
                    [lambda: P.dma("sp", dsts[i], o[u], reads=[f"o{u}"])]

            ld3(0)
            ld3(1)
            for i in range(0, NT, 2):
                stA(i)
                stA(i + 1)
                if i + 2 < NT:
                    ld3(i + 2)
                    ld3(i + 3)
                zip_run([stB_ops(i), stB_ops(i + 1)])
            P.barrier()

        plist = []
        plist.append(lambda: phase_mods(0))
        for b in range(2):
            plist.append(lambda b=b: phase_proj(0, b, None))
            plist.append(lambda b=b: phase_na(b))
            plist.append(lambda b=b: phase_full_attn(0, b))
            plist.append(lambda b=b: phase_outproj(0, b, None, X1, 18))
        for b in range(2):
            tl = [(X1[b, t * 128:(t + 1) * 128, :], b) for t in range(16)]
            ds = [X2[b, t * 128:(t + 1) * 128, :] for t in range(16)]
            plist.append(lambda tl=tl, ds=ds: phase_ffn(0, tl, ds, False))
        tl = [(X1[b, SEQ + t * 128:SEQ + (t + 1) * 128, :], 2) for b in range(2) for t in range(2)]
        ds = [X2[b, SEQ + t * 128:SEQ + (t + 1) * 128, :] for b in range(2) for t in range(2)]
        plist.append(lambda tl=tl, ds=ds: phase_ffn(0, tl, ds, False))
        plist.append(lambda: phase_mods(1))
        for b in range(2):
            plist.append(lambda b=b: phase_proj(1, b, X2))
            plist.append(lambda b=b: phase_full_attn(1, b))
            plist.append(lambda b=b: phase_outproj(1, b, X2, X3, 16))
            tl = [(X3[b, t * 128:(t + 1) * 128, :], b) for t in range(16)]
            ds = [out_d[b, t * 128:(t + 1) * 128, :] for t in range(16)]
            plist.append(lambda tl=tl, ds=ds: phase_ffn(1, tl, ds, True))
        for pf in plist[:nphases]:
            pf()
        P.emit()
    return nc


def _perm64(ncols):
    idx = np.arange(ncols)
    blk = idx // 64
    r = idx % 64
    return blk * 64 + (r + 32) % 64


def _na_bias_tables(rpb):
    out = np.zeros((5, 128, 8, 832), np.float32)
    reps = [0, 1, 2, 14, 15]
    qi = np.arange(128)
    rr, c = qi // 64, qi % 64
    kk = np.arange(576)
    kr, kc = kk // 64, kk % 64
    for pid, i in enumerate(reps):
        K0 = min(max(2 * i - 4, 0), 23)
        r = 2 * i + rr
        r0 = np.clip(r - 4, 0, 24)
        ws = np.clip(c - 8, 0, 48)
        krow = K0 + kr
        vrow = (krow[None, :] >= r0[:, None]) & (krow[None, :] < r0[:, None] + 8)
        vcol = (kc[None, :] >= ws[:, None]) & (kc[None, :] < ws[:, None] + 16)
        valid = vrow & vcol
        dr = np.clip(krow[None, :] - r[:, None] + 7, 0, 14)
        dc = np.clip(kc[None, :] - c[:, None] + 15, 0, 30)
        g = rpb[:, dr, dc]
        g = np.where(valid[None], g, np.float32(NEG))
        out[pid, :, :, 0:576] = g.transpose(1, 0, 2)
    return out


def _rope_tables():
    t = np.arange(SEQ)
    inv = (np.float32(10000.0) ** (-np.arange(16, dtype=np.float32) / np.float32(16))).astype(np.float32)
    ang = np.concatenate([(t // 64).astype(np.float32)[:, None] * inv, (t % 64).astype(np.float32)[:, None] * inv], axis=-1)
    cos = np.cos(ang).astype(np.float32).T
    sin = np.sin(ang).astype(np.float32).T
    C = np.ones((128, T), np.float32)
    S = np.zeros((128, T), np.float32)
    for p in range(128):
        C[p, :SEQ] = cos[p % 32]
        S[p, :SEQ] = sin[p % 32] if (p % 64) >= 32 else -sin[p % 32]
    return C, S


_CACHE = {}


def _prep(inp):
    f = lambda a: np.ascontiguousarray(np.asarray(a, dtype=np.float32))
    C, S = _rope_tables()
    blk = np.zeros((128, 128), np.float32)
    blk[:64, :64] = 1.0 / 64
    blk[64:, 64:] = 1.0 / 64
    shared = {"ident": np.eye(128, dtype=np.float32), "blk": blk, "ropeC": C, "ropeS": S}
    for l in range(2):
        shared[f"l{l}_ada_w"] = f(inp[f"l{l}_ada_w"])
        ab = f(inp[f"l{l}_ada_b"])
        shared[f"l{l}_ada_bT"] = np.ascontiguousarray(ab.reshape(48, 128).T)
        shared[f"l{l}_ada_b"] = ab.reshape(1, 6 * D)
        shared[f"l{l}_w_out"] = f(inp[f"l{l}_w_out"])
        shared[f"l{l}_ln"] = np.stack([f(inp[f"l{l}_ln1_g"]), f(inp[f"l{l}_ln1_b"]), f(inp[f"l{l}_ln2_g"]), f(inp[f"l{l}_ln2_b"])])
    w0 = f(inp["l0_w_in"])
    qa, ka, va, qb, kb, vb = w0[:, 0:512], w0[:, 512:1024], w0[:, 1024:1536], w0[:, 1536:2048], w0[:, 2048:2560], w0[:, 2560:3072]
    shared["l0_wfm"] = np.ascontiguousarray(np.concatenate([qa, ka, qb, kb], axis=1))
    p512 = _perm64(512)
    shared["l0_wfmp"] = np.ascontiguousarray(np.concatenate([qb[:, p512], kb[:, p512]], axis=1))
    shared["l0_wv"] = np.ascontiguousarray(np.concatenate([va, vb], axis=1))
    shared["l0_nabias"] = _na_bias_tables(f(inp["l0_rpb"]))
    shared["l0_lq"] = f(inp["l0_lambda_qk"]).reshape(1, 256)
    shared["l0_subg"] = f(inp["l0_subln_g"]).reshape(1, 128)
    shared["l0_subgc"] = f(inp["l0_subln_g"]).reshape(128, 1)
    shared["l0_w13"] = f(inp["l0_ffn_w13"])
    shared["l0_w2"] = f(inp["l0_ffn_w2"])
    w1 = f(inp["l1_w_in"])
    q1, k1, v1 = w1[:, 0:1024], w1[:, 1024:1280], w1[:, 1280:1536]
    kdup = np.concatenate([np.concatenate([k1[:, 64 * g:64 * (g + 1)]] * 2, axis=1) for g in range(4)], axis=1)
    wfm1 = np.concatenate([q1, kdup], axis=1)
    shared["l1_wfm"] = np.ascontiguousarray(wfm1)
    shared["l1_wfmp"] = np.ascontiguousarray(wfm1[:, _perm64(1536)])
    shared["l1_wv"] = np.ascontiguousarray(v1)
    gq, gk = f(inp["l1_q_norm_g"]), f(inp["l1_k_norm_g"])
    p64 = _perm64(64)
    shared["l1_gcols"] = np.ascontiguousarray(np.stack([np.tile(gq, 2), np.tile(gq[p64], 2), np.tile(gk, 2), np.tile(gk[p64], 2)], axis=1))
    shared["l1_grow"] = np.ascontiguousarray(np.concatenate([gq, gk]).reshape(1, 128))
    shared["l1_router"] = f(inp["l1_router_w"])
    shared["l1_w13"] = f(inp["l1_moe_w13"])
    shared["l1_w2"] = f(inp["l1_moe_w2"])
    x, ctx, c, c_ctx = f(inp["x"]), f(inp["ctx"]), f(inp["c"]), f(inp["c_ctx"])
    in_maps = []
    for i in range(NCORES):
        d = dict(shared)
        d["x"] = x[2 * i:2 * i + 2]
        d["ctx"] = ctx[2 * i:2 * i + 2]
        c3 = np.stack([c[2 * i], c[2 * i + 1], c_ctx])
        d["cT"] = np.ascontiguousarray(c3.reshape(3, 8, 128).transpose(2, 1, 0))
        in_maps.append(d)
    return in_maps


def kernel(**inp):
    if "nc" not in _CACHE:
        _CACHE["nc"] = build_program()
    nc = _CACHE["nc"]
    in_maps = _prep(inp)
    res = run_bass_kernel_spmd(nc, in_maps, core_ids=list(range(NCORES)))
    return np.concatenate([r["out"] for r in res.results], axis=0).astype(np.float32)
```

```python
import math
from contextlib import ExitStack
import numpy as np
import concourse.bass as bass
import concourse.mybir as mybir
from concourse.bass_utils import run_bass_kernel_spmd

F32 = mybir.dt.float32
BF16 = mybir.dt.bfloat16
AF = mybir.ActivationFunctionType
ALU = mybir.AluOpType
AX = mybir.AxisListType

NCORES = 8
D = 1024
SEQ = 2048
NCTX = 256
T = SEQ + NCTX
ALPHA = 4.0 ** 0.25
LN_EPS = 1e-5
RMS_EPS = 1e-6
FFN = 2816
EDIM = 3584
NEXP = 8
NEG = -30000.0
LAM_INIT0 = 0.8 - 0.6 * math.exp(-0.3 * 0)

ENGS = ("pe", "act", "dve", "pool", "sp")
SELF_SYNC = ("act", "dve", "pool")


class Op:
    __slots__ = ("eng", "fn", "deps", "idx", "is_dma", "waits", "signal", "sem", "target", "dma_prev")

    def __init__(self, eng, fn, is_dma):
        self.eng = eng
        self.fn = fn
        self.deps = []
        self.is_dma = is_dma
        self.waits = []
        self.signal = False
        self.sem = None
        self.target = None
        self.dma_prev = None


class Prog:
    def __init__(self, nc, n_rot=8, n_dma_sems=12):
        self.nc = nc
        self.ops = {e: [] for e in ENGS}
        self.last_writer = {}
        self.readers = {}
        self.n_rot = n_rot
        self.n_dma_sems = n_dma_sems
        self.dmas_live = []

    def _add(self, eng, fn, reads, writes, is_dma, extra=()):
        psr = tuple(k for k in reads if isinstance(k, str) and k.startswith("ps") and k[2:].isdigit())
        if psr:
            reads = tuple(k for k in reads if k not in psr)
            writes = tuple(writes) + tuple(k for k in psr if k not in writes)
        op = Op(eng, fn, is_dma)
        op.idx = len(self.ops[eng])
        deps = list(extra)
        for k in reads:
            w = self.last_writer.get(k)
            if w is not None:
                deps.append(w)
        for k in writes:
            w = self.last_writer.get(k)
            if w is not None:
                deps.append(w)
            deps.extend(self.readers.get(k, ()))
        op.deps = deps
        for k in writes:
            self.last_writer[k] = op
            self.readers[k] = []
        for k in reads:
            if k not in writes:
                self.readers.setdefault(k, []).append(op)
        if is_dma:
            self.dmas_live.append(op)
        self.ops[eng].append(op)
        return op

    def op(self, eng, fn, reads=(), writes=()):
        return self._add(eng, fn, tuple(reads), tuple(writes), False)

    def dma(self, eng, out, in_, reads=(), writes=()):
        return self._add(eng, lambda e: e.dma_start(out=out, in_=in_), tuple(reads), tuple(writes), True)

    def barrier(self):
        lasts = [self.ops[e][-1] for e in ENGS if self.ops[e]]
        deps = lasts + self.dmas_live
        for e in ENGS:
            self._add(e, lambda eng: eng.nop(), (), (), False, extra=deps)
        self.last_writer.clear()
        self.readers.clear()
        self.dmas_live = []

    def resolve(self):
        R, K = self.n_rot, self.n_dma_sems
        for e in ENGS:
            dmas = [o for o in self.ops[e] if o.is_dma]
            for i, o in enumerate(dmas):
                o.sem = ("dma", e, i % K)
                o.target = 16 * (i // K + 1)
                o.signal = True
                o.dma_prev = dmas[i - K] if i >= K else None
        for e in ENGS:
            seen_eng = {x: -1 for x in ENGS}
            seen_dma = {}
            for o in self.ops[e]:
                deps = o.deps
                if o.is_dma and o.dma_prev is not None:
                    deps = deps + [o.dma_prev]
                best = {}
                for d in deps:
                    if d.is_dma:
                        if seen_dma.get(d.sem, 0) >= d.target:
                            continue
                        seen_dma[d.sem] = d.target
                        best[d.sem] = d
                    else:
                        if d.eng == e and e not in SELF_SYNC:
                            continue
                        if seen_eng[d.eng] >= d.idx:
                            continue
                        seen_eng[d.eng] = d.idx
                        best[("e", d.eng)] = d
                o.waits = list(best.values())
                for d in o.waits:
                    d.signal = True
                o.deps = None
        for e in ENGS:
            s = 0
            for o in self.ops[e]:
                if o.is_dma:
                    continue
                if o.signal:
                    o.sem = ("cmp", e, s % R)
                    o.target = s // R + 1
                    s += 1

    def emit(self):
        nc = self.nc
        self.resolve()
        names = set()
        for e in ENGS:
            for o in self.ops[e]:
                if o.signal:
                    names.add(o.sem)
        with ExitStack() as st:
            sems = {n: st.enter_context(nc.semaphore("_".join(map(str, n)))) for n in sorted(names)}
            block = st.enter_context(nc.Block())

            def run(e):
                def body(eng):
                    for o in self.ops[e]:
                        ws = o.waits
                        attach = None
                        if ws and not o.is_dma:
                            attach = ws[-1]
                            ws = ws[:-1]
                        for d in ws:
                            eng.wait_ge(sems[d.sem], d.target)
                        ins = o.fn(eng)
                        if attach is not None:
                            ins._wait_ge(sems[attach.sem], attach.target)
                        if o.signal:
                            ins.then_inc(sems[o.sem], 16 if o.is_dma else 1)
                return body

            block.tensor(run("pe"))
            block.scalar(run("act"))
            block.vector(run("dve"))
            block.gpsimd(run("pool"))
            block.sync(run("sp"))


class Arena:
    def __init__(self, ap, nwords):
        self.ap = ap
        self.n = nwords
        self.off = 0
        self.base = 0

    def _take(self, words):
        words = (words + 7) // 8 * 8
        a = self.off
        self.off += words
        assert self.off <= self.n, f"arena overflow {self.off} > {self.n}"
        return self.ap[:, a:a + words], words

    @staticmethod
    def _shape(v, free):
        if len(free) == 1:
            return v
        if len(free) == 2:
            return v.rearrange("p (a b) -> p a b", a=free[0])
        if len(free) == 3:
            return v.rearrange("p (a b c) -> p a b c", a=free[0], b=free[1])
        raise ValueError

    def f32(self, *free):
        n = int(np.prod(free))
        v, w = self._take(n)
        return self._shape(v[:, 0:n], free)

    def bf(self, *free):
        n = int(np.prod(free))
        v, w = self._take((n + 1) // 2)
        return self._shape(v.bitcast(BF16)[:, 0:n], free)

    def mark(self):
        self.base = self.off

    def reset(self):
        self.off = self.base


def build_program(debug=False, nphases=10**9):
    nc = bass.Bass("TRN2", target_bir_lowering=False)

    def din(name, shape, dt=F32):
        return nc.dram_tensor(name, list(shape), dt, kind="ExternalInput").ap()

    def dscr(name, shape, dt):
        if debug:
            return nc.dram_tensor(name, list(shape), dt, kind="ExternalOutput").ap()
        return nc.dram_tensor(name, list(shape), dt).ap()

    x_in = din("x", [2, SEQ, D])
    ctx_in = din("ctx", [2, NCTX, D])
    cT_d = din("cT", [128, 8, 3])
    ident_d = din("ident", [128, 128])
    blk_d = din("blk", [128, 128])
    ropeC_d = din("ropeC", [128, T])
    ropeS_d = din("ropeS", [128, T])
    L = [dict(), dict()]
    for l in range(2):
        L[l]["ada_w"] = din(f"l{l}_ada_w", [D, 6 * D])
        L[l]["ada_bT"] = din(f"l{l}_ada_bT", [128, 48])
        L[l]["ada_b"] = din(f"l{l}_ada_b", [1, 6 * D])
        L[l]["w_out"] = din(f"l{l}_w_out", [D, D])
        L[l]["ln"] = din(f"l{l}_ln", [4, D])
    L[0]["wfm"] = din("l0_wfm", [D, 2048])
    L[0]["wfmp"] = din("l0_wfmp", [D, 1024])
    L[0]["wv"] = din("l0_wv", [D, 1024])
    nabias_d = din("l0_nabias", [5, 128, 8, 832])
    lq_d = din("l0_lq", [1, 256])
    subg_d = din("l0_subg", [1, 128])
    subgc_d = din("l0_subgc", [128, 1])
    w13_0 = din("l0_w13", [D, 2 * FFN])
    w2_0 = din("l0_w2", [FFN, D])
    L[1]["wfm"] = din("l1_wfm", [D, 1536])
    L[1]["wfmp"] = din("l1_wfmp", [D, 1536])
    L[1]["wv"] = din("l1_wv", [D, 256])
    gcols_d = din("l1_gcols", [128, 4])
    grow_d = din("l1_grow", [1, 128])
    router_d = din("l1_router", [D, NEXP])
    w13_1 = din("l1_w13", [NEXP, D, 2 * EDIM])
    w2_1 = din("l1_w2", [NEXP, EDIM, D])
    out_d = nc.dram_tensor("out", [2, SEQ, D], F32, kind="ExternalOutput").ap()

    qkT = [dscr("qkT0", [2, 16, 128, T], BF16), dscr("qkT1", [2, 12, 128, T], BF16)]
    vsc = [dscr("v0", [2, T, 1024], BF16), dscr("v1", [2, T, 256], BF16)]
    att = dscr("att", [2, T, D], BF16)
    attT = dscr("attT", [2, 8, 128, T], BF16)
    X1 = dscr("X1", [2, T, D], F32)
    X2 = dscr("X2", [2, T, D], F32)
    X3 = dscr("X3", [2, SEQ, D], F32)
    gates_d = dscr("gates", [2, 2, 3, 128, D], F32)

    P = Prog(nc)
    out_keys = []

    with ExitStack() as st:
        NW = 52000
        arena_t = st.enter_context(nc.sbuf_tensor("arena", [128, NW], F32))
        psa = st.enter_context(nc.psum_tensor("psa", [128, 4096], F32))
        A = Arena(arena_t, NW)

        def bank(i, n=512):
            return psa[:, 512 * i:512 * i + n]

        ident = A.f32(128)
        identb = A.bf(128)
        blk = A.f32(128)
        ones = A.f32(128)
        cst = A.f32(8)
        modsT = [A.f32(48, 3), A.f32(48, 3)]
        opsc = [[A.f32(8, 3), A.f32(8, 3)], [A.f32(8, 3), A.f32(8, 3)]]
        lamt = A.f32(8)
        subg = A.f32(128)
        gcols = A.f32(4)
        A.mark()

        P.dma("sp", ident, ident_d, writes=["ident"])
        P.dma("sp", blk, blk_d, writes=["blk"])
        P.dma("sp", gcols, gcols_d, writes=["gcols"])
        P.op("dve", lambda e: e.tensor_copy(identb, ident), reads=["ident"], writes=["identb"])
        P.op("dve", lambda e: e.memset(ones, 1.0), writes=["ones"])
        P.op("dve", lambda e: e.memset(cst[:, 0:1], LN_EPS), writes=["cst0"])
        P.op("dve", lambda e: e.memset(cst[:, 1:2], RMS_EPS), writes=["cst1"])
        P.op("dve", lambda e: e.memset(cst[:, 2:3], 0.0), writes=["cst2"])
        lqb = A.f32(256)
        lqp = A.f32(128)
        P.dma("sp", lqb, lq_d.partition_broadcast(128), writes=["lqb"])
        P.dma("sp", subg, subg_d.partition_broadcast(128), writes=["subg"])
        P.op("dve", lambda e: e.tensor_scalar(subg, subg, 1.0 - LAM_INIT0, None, ALU.mult), reads=["subg"], writes=["subg"])
        P.op("dve", lambda e: e.tensor_tensor(lqp[:, 0:64], lqb[:, 0:64], lqb[:, 64:128], ALU.mult), reads=["lqb"], writes=["lqp0"])
        P.op("dve", lambda e: e.tensor_tensor(lqp[:, 64:128], lqb[:, 128:192], lqb[:, 192:256], ALU.mult), reads=["lqb"], writes=["lqp1"])
        P.op("dve", lambda e: e.reduce_sum(lamt[:, 2:3], lqp[:, 0:64], axis=AX.X), reads=["lqp0"], writes=["lam2"])
        P.op("dve", lambda e: e.reduce_sum(lamt[:, 3:4], lqp[:, 64:128], axis=AX.X), reads=["lqp1"], writes=["lam3"])
        P.op("act", lambda e: e.activation(lamt[:, 4:6], lamt[:, 2:4], AF.Exp), reads=["lam2", "lam3"], writes=["lam45"])
        P.op("dve", lambda e: e.tensor_tensor(lamt[:, 0:1], lamt[:, 4:5], lamt[:, 5:6], ALU.subtract), reads=["lam45"], writes=["lam0a"])
        P.op("dve", lambda e: e.tensor_scalar(lamt[:, 0:1], lamt[:, 0:1], LAM_INIT0, None, ALU.add), reads=["lam0a"], writes=["lam0"])
        P.barrier()
        A.reset()

        def phase_mods(l):
            A.reset()
            cT = A.f32(8, 3)
            scT = A.f32(8, 3)
            abT = A.f32(48)
            wch = [A.f32(8, 1024), A.f32(8, 1024)]
            bbc = [A.f32(512), A.f32(512)]
            gst = [A.f32(512), A.f32(512)]
            pm = bank(0, 144)
            P.dma("sp", cT, cT_d, writes=["cT"])
            P.dma("sp", abT, L[l]["ada_bT"], writes=["abT"])
            P.op("act", lambda e: e.activation(scT, cT, AF.Silu), reads=["cT"], writes=["scT"])
            gi = 0
            for ty in range(6):
                wb = wch[ty % 2]
                wk = f"wch{ty % 2}"
                P.dma("sp", wb, L[l]["ada_w"][:, ty * 1024:(ty + 1) * 1024].rearrange("(k p) n -> p k n", p=128), writes=[wk])
                for jc in range(8):
                    j = ty * 8 + jc
                    for k in range(8):
                        P.op("pe", lambda e, wb=wb, jc=jc, k=k, j=j: e.matmul(pm[:, 3 * j:3 * j + 3], wb[:, k, jc * 128:(jc + 1) * 128], scT[:, k, :],
                                                                       start=(k == 0), stop=(k == 7)),
                             reads=[wk, "scT"], writes=["ps0"])
            pm3 = pm.rearrange("p (j m) -> p j m", m=3)
            for m in range(3):
                P.op("dve", lambda e, m=m: e.tensor_tensor(modsT[l][:, :, m], pm3[:, :, m], abT, ALU.add), reads=["ps0", "abT"], writes=[f"modsT{m}"])
            Rg = [A.f32(1024), A.f32(1024)]
            gi = 0
            for gsel, ty in ((0, 2), (1, 5)):
                for m in range(3):
                    R = Rg[gi % 2]
                    for k in range(8):
                        P.op("dve", lambda e, R=R, k=k, ty=ty, m=m: e.tensor_scalar(R[:, k * 128:(k + 1) * 128], ident, modsT[l][:, ty * 8 + k, m:m + 1], None, ALU.mult),
                             reads=[f"modsT{m}", "ident"], writes=[f"Rg{gi % 2}"])
                    for half in range(2):
                        pb = bank(1 + half)
                        P.op("pe", lambda e, pb=pb, R=R, half=half: e.matmul(pb, ones, R[:, half * 512:(half + 1) * 512], start=True, stop=True),
                             reads=[f"Rg{gi % 2}", "ones"], writes=[f"ps{1 + half}"])
                        gs = gst[half]
                        P.op("act", lambda e, gs=gs, pb=pb: e.copy(gs, pb), reads=[f"ps{1 + half}"], writes=[f"gst{half}"])
                        P.dma("sp", gates_d[l, gsel, m, :, half * 512:(half + 1) * 512], gs, reads=[f"gst{half}"])
                    gi += 1
            P.op("dve", lambda e: e.tensor_scalar(opsc[l][0], modsT[l][:, 8:16, :], 1.0, None, ALU.add), reads=["modsT0", "modsT1", "modsT2"], writes=["opsc0"])
            P.op("dve", lambda e: e.tensor_scalar(opsc[l][1], modsT[l][:, 32:40, :], 1.0, None, ALU.add), reads=["modsT0", "modsT1", "modsT2"], writes=["opsc1"])
            P.barrier()

        def tiles_of(Xsrc, b, nlat=16, nctx=2, l0=False):
            tl = []
            for t in range(nlat):
                if l0:
                    tl.append((x_in[b, t * 128:(t + 1) * 128, :], b))
                else:
                    tl.append((Xsrc[b, t * 128:(t + 1) * 128, :], b))
            for t in range(nctx):
                if l0:
                    tl.append((ctx_in[b, t * 128:(t + 1) * 128, :], 2))
                else:
                    tl.append((Xsrc[b, SEQ + t * 128:SEQ + (t + 1) * 128, :], 2))
            return tl

        def emit_hT(l, sub, xt, xk, m, hT, hk, col0, pbanks, pkeys, cnt, hTf=None, hfk=None):
            shift_base = 0 if sub == 0 else 24
            for half in range(2):
                pb = pbanks[cnt[0] % 2]
                pk = pkeys[cnt[0] % 2]
                cnt[0] += 1
                for kk in range(4):
                    k = half * 4 + kk
                    P.op("pe", lambda e, pb=pb, kk=kk, k=k: e.transpose(pb[:, kk * 128:(kk + 1) * 128], xt[:, k * 128:(k + 1) * 128], ident),
                         reads=[xk, "ident"], writes=[pk])
                for kk in range(4):
                    k = half * 4 + kk
                    sc = opsc[l][sub][:, k, m:m + 1]
                    sh = modsT[l][:, shift_base + k, m:m + 1]
                    if hTf is not None:
                        P.op("act", lambda e, pb=pb, kk=kk, k=k, sc=sc, sh=sh: e.activation(hTf[:, k, :], pb[:, kk * 128:(kk + 1) * 128], AF.Identity, bias=sh, scale=sc),
                             reads=[pk], writes=[hfk])
                    elif pk == pkeys[0]:
                        P.op("act", lambda e, pb=pb, kk=kk, k=k, sc=sc, sh=sh: e.activation(hT[:, k, col0:col0 + 128], pb[:, kk * 128:(kk + 1) * 128], AF.Identity, bias=sh, scale=sc),
                             reads=[pk], writes=[hk])
                    else:
                        P.op("dve", lambda e, pb=pb, kk=kk, k=k, sc=sc, sh=sh: e.tensor_scalar(hT[:, k, col0:col0 + 128], pb[:, kk * 128:(kk + 1) * 128], sc, sh, ALU.mult, ALU.add),
                             reads=[pk], writes=[hk])
            if hTf is not None:
                P.op("dve", lambda e: e.tensor_copy(hT[:, :, col0:col0 + 128], hTf), reads=[hfk], writes=[hk])

        def phase_proj(l, b, Xsrc):
            A.reset()
            nfm = 16 if l == 0 else 12
            nperm = 8 if l == 0 else 12
            perm_first = 8 if l == 0 else 0
            nv = 1024 if l == 0 else 256
            wfm = A.bf(8, nfm * 128)
            wfmp = A.bf(8, nperm * 128)
            wv = A.bf(8, nv)
            xt = [A.f32(4, 1024), A.f32(4, 1024)]
            hT = [A.bf(8, 512), A.bf(8, 512)]
            if l == 0:
                tabs = [(A.f32(T), A.f32(T))]
            else:
                tabs = [(A.f32(T), A.f32(T)), (A.f32(T), A.f32(T))]
            stg = [A.bf(512) for _ in range(4)]
            t1 = [A.f32(512), A.f32(512)]
            t2 = [A.f32(512), A.f32(512)]
            sq = [A.f32(512), A.f32(512)]
            rs = [A.f32(512), A.f32(512)]
            wl = [("wfm", wfm, c0) for c0 in range(0, nfm * 128, 512)]
            pl_ = [("wfmp", wfmp, c0) for c0 in range(0, nperm * 128, 512)]
            if l == 1:
                order = [x for pair in zip(wl, pl_) for x in pair]
            else:
                order = wl + pl_
            for nm, wt, c0 in order:
                P.dma("pool", wt[:, :, c0:c0 + 512], L[l][nm][:, c0:c0 + 512].rearrange("(k p) n -> p k n", p=128), writes=[nm + str(c0)])
            for c0 in range(0, nv, 512):
                n = min(512, nv - c0)
                P.dma("pool", wv[:, :, c0:c0 + n], L[l]["wv"][:, c0:c0 + n].rearrange("(k p) n -> p k n", p=128), writes=["wv"])
            for ti, (tc, ts) in enumerate(tabs):
                P.dma("sp", tc, ropeC_d, writes=[f"tc{ti}"])
                P.dma("sp", ts, ropeS_d, writes=[f"ts{ti}"])
                if l == 1:
                    P.op("dve", lambda e, tc=tc, ti=ti: e.tensor_scalar(tc, tc, gcols[:, 2 * ti:2 * ti + 1], None, ALU.mult), reads=[f"tc{ti}", "gcols"], writes=[f"tc{ti}"])
                    P.op("dve", lambda e, ts=ts, ti=ti: e.tensor_scalar(ts, ts, gcols[:, 2 * ti + 1:2 * ti + 2], None, ALU.mult), reads=[f"ts{ti}", "gcols"], writes=[f"ts{ti}"])
            tl = tiles_of(Xsrc, b, l0=(l == 0))
            groups = [tl[0:4], tl[4:8], tl[8:12], tl[12:16], tl[16:18]]
            cnt = [0]
            si = 0
            ci = 0
            vi = 0
            tails = []
            def load_grp(g):
                for ti, (src, m) in enumerate(groups[g]):
                    P.dma("sp", xt[g % 2][:, ti, :], src, writes=[f"xt{g % 2}_{ti}"])
            def build_hT(g):
                gb_ = g % 2
                for ti, (src, m) in enumerate(groups[g]):
                    emit_hT(l, 0, xt[gb_][:, ti, :], f"xt{gb_}_{ti}", m, hT[gb_], f"hT{gb_}", ti * 128, [bank(0), bank(1)], ["ps0", "ps1"], cnt)

            load_grp(0)
            build_hT(0)
            load_grp(1)
            for g, grp in enumerate(groups):
                gb = g % 2
                ntok = 128 * len(grp)
                tok0 = g * 512
                for c in range(nfm):
                    if c == nfm // 2:
                        if g + 1 < len(groups):
                            build_hT(g + 1)
                        if g + 2 < len(groups):
                            load_grp(g + 2)
                    pa = bank(2 + (ci % 2) * 2)
                    pka = f"ps{2 + (ci % 2) * 2}"
                    pbk = bank(3 + (ci % 2) * 2)
                    pkb = f"ps{3 + (ci % 2) * 2}"
                    roped = c >= perm_first
                    for k in range(8):
                        P.op("pe", lambda e, pa=pa, k=k, c=c, gb=gb, ntok=ntok: e.matmul(pa[:, 0:ntok], wfm[:, k, c * 128:(c + 1) * 128], hT[gb][:, k, 0:ntok], start=(k == 0), stop=(k == 7)),
                             reads=["wfm" + str((c * 128) // 512 * 512), f"hT{gb}"], writes=[pka])
                    sg = stg[si % 4]
                    sk = f"stg{si % 4}"
                    si += 1
                    if not roped:
                        if c % 2 == 0:
                            P.op("act", lambda e, sg=sg, pa=pa, ntok=ntok: e.copy(sg[:, 0:ntok], pa[:, 0:ntok]), reads=[pka], writes=[sk])
                        else:
                            P.op("dve", lambda e, sg=sg, pa=pa, ntok=ntok: e.tensor_copy(sg[:, 0:ntok], pa[:, 0:ntok]), reads=[pka], writes=[sk])
                    else:
                        cp = c - perm_first
                        for k in range(8):
                            P.op("pe", lambda e, pbk=pbk, k=k, cp=cp, gb=gb, ntok=ntok: e.matmul(pbk[:, 0:ntok], wfmp[:, k, cp * 128:(cp + 1) * 128], hT[gb][:, k, 0:ntok], start=(k == 0), stop=(k == 7)),
                                 reads=["wfmp" + str((cp * 128) // 512 * 512), f"hT{gb}"], writes=[pkb])
                        tsel = 0 if (l == 0 or c < 8) else 1
                        tc, ts = tabs[tsel]
                        u = ci % 2
                        a1, a2 = t1[u], t2[u]
                        P.op("dve", lambda e, a1=a1, pa=pa, tc=tc, ntok=ntok, tok0=tok0: e.tensor_tensor(a1[:, 0:ntok], pa[:, 0:ntok], tc[:, tok0:tok0 + ntok], ALU.mult),
                             reads=[pka, f"tc{tsel}"], writes=[f"t1{u}"])
                        P.op("dve", lambda e, a2=a2, pbk=pbk, ts=ts, ntok=ntok, tok0=tok0: e.tensor_tensor(a2[:, 0:ntok], pbk[:, 0:ntok], ts[:, tok0:tok0 + ntok], ALU.mult),
                             reads=[pkb, f"ts{tsel}"], writes=[f"t2{u}"])
                        if l == 0:
                            P.op("pool", lambda e, sg=sg, a1=a1, a2=a2, ntok=ntok: e.tensor_tensor(sg[:, 0:ntok], a1[:, 0:ntok], a2[:, 0:ntok], ALU.add),
                                 reads=[f"t1{u}", f"t2{u}"], writes=[sk])
                        else:
                            s_q, r_s = sq[u], rs[u]
                            pc = bank(6)
                            P.op("act", lambda e, s_q=s_q, pa=pa, ntok=ntok: e.activation(s_q[:, 0:ntok], pa[:, 0:ntok], AF.Square), reads=[pka], writes=[f"sq{u}"])
                            P.op("dve", lambda e, a1=a1, a2=a2, ntok=ntok: e.tensor_tensor(a1[:, 0:ntok], a1[:, 0:ntok], a2[:, 0:ntok], ALU.add),
                                 reads=[f"t1{u}", f"t2{u}"], writes=[f"t1{u}"])

                            def tail(pc=pc, s_q=s_q, r_s=r_s, a1=a1, sg=sg, sk=sk, u=u, ntok=ntok, c=c, tok0=tok0):
                                P.op("pe", lambda e: e.matmul(pc[:, 0:ntok], blk, s_q[:, 0:ntok], start=True, stop=True),
                                     reads=[f"sq{u}", "blk"], writes=["ps6"])
                                P.op("act", lambda e: e.activation(r_s[:, 0:ntok], pc[:, 0:ntok], AF.Ln, bias=cst[:, 1:2], scale=1.0),
                                     reads=["ps6", "cst1"], writes=[f"rs{u}"])
                                P.op("act", lambda e: e.activation(r_s[:, 0:ntok], r_s[:, 0:ntok], AF.Exp, bias=cst[:, 2:3], scale=-0.5), reads=[f"rs{u}"], writes=[f"rs{u}"])
                                P.op("pool", lambda e: e.tensor_tensor(sg[:, 0:ntok], a1[:, 0:ntok], r_s[:, 0:ntok], ALU.mult),
                                     reads=[f"t1{u}", f"rs{u}"], writes=[sk])
                                P.dma("sp", qkT[l][b, c, :, tok0:tok0 + ntok], sg[:, 0:ntok], reads=[sk])
                            if tails:
                                tails.pop(0)()
                            tails.append(tail)
                            ci += 1
                            continue
                    P.dma("sp", qkT[l][b, c, :, tok0:tok0 + ntok], sg[:, 0:ntok], reads=[sk])
                    ci += 1
                while tails:
                    tails.pop(0)()
                for ti, (src, m) in enumerate(grp):
                    row0 = tok0 + ti * 128
                    for c0 in range(0, nv, 512):
                        n = min(512, nv - c0)
                        pv = bank(7)
                        for k in range(8):
                            P.op("pe", lambda e, pv=pv, k=k, gb=gb, ti=ti, c0=c0, n=n: e.matmul(pv[:, 0:n], hT[gb][:, k, ti * 128:(ti + 1) * 128], wv[:, k, c0:c0 + n], start=(k == 0), stop=(k == 7)),
                                 reads=["wv", f"hT{gb}"], writes=["ps7"])
                        sg = stg[si % 4]
                        sk = f"stg{si % 4}"
                        si += 1
                        if vi % 2 == 0:
                            P.op("act", lambda e, sg=sg, pv=pv, n=n: e.copy(sg[:, 0:n], pv[:, 0:n]), reads=["ps7"], writes=[sk])
                        else:
                            P.op("dve", lambda e, sg=sg, pv=pv, n=n: e.tensor_copy(sg[:, 0:n], pv[:, 0:n]), reads=["ps7"], writes=[sk])
                        vi += 1
                        P.dma("sp", vsc[l][b, row0:row0 + 128, c0:c0 + n], sg[:, 0:n], reads=[sk])
            P.barrier()

        PTv = psa[:, 2560:3712].bitcast(BF16)
        O_ps = [psa[:, 3840:3968], psa[:, 3968:4096]]

        def pt_keys(c0, c1):
            ks = set()
            for c in range(c0, c1):
                ks.add("ps5" if c < 8 else ("ps6" if c < 16 else "ps7"))
            return sorted(ks)

        def softmax_pv(S_ap, s_keys, nk, scale, chunks, Pb, pkey, PT, ptkey, st, stkey, o_idx, uid):
            P.op("dve", lambda e: e.reduce_max(st[:, 0:1], S_ap, axis=AX.X), reads=s_keys, writes=[stkey + "m"])
            P.op("dve", lambda e: e.tensor_scalar(st[:, 1:2], st[:, 0:1], -scale, None, ALU.mult), reads=[stkey + "m"], writes=[stkey + "n"])
            P.op("act", lambda e: e.activation(Pb[:, 0:nk], S_ap, AF.Exp, bias=st[:, 1:2], scale=scale, accum_out=st[:, 2:3]),
                 reads=list(s_keys) + [stkey + "n"], writes=[pkey, stkey + "s"])
            nch = len(chunks)
            for j, (c0, w, v_ap, vk) in enumerate(chunks):
                P.op("pe", lambda e, j=j, c0=c0, w=w: e.transpose(PTv[0:w, j * 128:(j + 1) * 128], Pb[:, c0:c0 + w], identb),
                     reads=[pkey, "identb"], writes=pt_keys(j, j + 1))
            segs = [(0, min(nch, 8)), (8, min(nch, 16)), (16, nch)]
            for si, (a, bnd) in enumerate(segs):
                if bnd <= a:
                    continue
                eng = "act" if (uid + si) % 2 == 0 else "dve"
                if eng == "act":
                    P.op("act", lambda e, a=a, bnd=bnd: e.copy(PT[:, a * 128:bnd * 128], PTv[:, a * 128:bnd * 128]), reads=pt_keys(a, bnd), writes=[ptkey + str(si)])
                else:
                    P.op("dve", lambda e, a=a, bnd=bnd: e.tensor_copy(PT[:, a * 128:bnd * 128], PTv[:, a * 128:bnd * 128]), reads=pt_keys(a, bnd), writes=[ptkey + str(si)])
            dv = chunks[0][2].shape[-1]
            Oa = O_ps[o_idx][:, 0:dv]
            for j, (c0, w, v_ap, vk) in enumerate(chunks):
                P.op("pe", lambda e, j=j, w=w, v_ap=v_ap: e.matmul(Oa, PT[0:w, j * 128:(j + 1) * 128], v_ap, start=(j == 0), stop=(j == nch - 1)),
                     reads=[ptkey + str(j // 8), vk], writes=["ps7"])
            P.op("dve", lambda e: e.reciprocal(st[:, 2:3], st[:, 2:3]), reads=[stkey + "s"], writes=[stkey + "s"])
            return Oa

        def phase_na(b):
            A.reset()
            kctx = A.bf(4, 256)
            vctx = A.bf(2, 512)
            bias = A.f32(8, 832)
            qz = [[A.bf(4, 128), A.bf(4, 128)] for _ in range(2)]
            kb = [A.bf(4, 576), A.bf(4, 576)]
            vb = [A.bf(5, 512), A.bf(5, 512)]
            Tt = [A.f32(832) for _ in range(3)]
            Pb = [A.bf(896) for _ in range(3)]
            PT = [A.bf(7 * 128), A.bf(7 * 128)]
            sts = [A.f32(8) for _ in range(6)]
            onat = [A.bf(512), A.bf(512)]
            PTp = [bank(5).bitcast(BF16), bank(6).bitcast(BF16)]
            Obk = [(bank(7), "ps7"), (bank(4), "ps4")]
            for u in range(2):
                P.op("pool", lambda e, u=u: e.memset(qz[u][0][64:128], 0.0), writes=[f"qzz{u}0"])
                P.op("pool", lambda e, u=u: e.memset(qz[u][1][0:64], 0.0), writes=[f"qzz{u}1"])
                P.op("pool", lambda e, u=u: e.memset(vb[u][64:128, 4, :], 0.0), writes=[f"vbz{u}"])
            P.dma("sp", kctx, qkT[0][b, 4:8, :, SEQ:T].rearrange("c p t -> p c t"), writes=["kctx"])
            P.dma("sp", vctx, vsc[0][b, SEQ:T, 0:512].rearrange("(j p) c -> p j c", p=128), writes=["vctx"])

            def load_tile(i):
                u = i % 2
                P.dma("sp", qz[u][0][0:64], qkT[0][b, 0:4, 0:64, i * 128:(i + 1) * 128].rearrange("c p t -> p c t"), writes=[f"qz{u}0"])
                P.dma("sp", qz[u][1][64:128], qkT[0][b, 0:4, 64:128, i * 128:(i + 1) * 128].rearrange("c p t -> p c t"), writes=[f"qz{u}1"])
                if i < 16:
                    K0 = min(max(2 * i - 4, 0), 23)
                    r0 = K0 * 64
                    P.dma("sp", kb[u], qkT[0][b, 4:8, :, r0:r0 + 576].rearrange("c p t -> p c t"), writes=[f"kb{u}"])
                    P.dma("sp", vb[u][:, 0:4, :], vsc[0][b, r0:r0 + 512, 0:512].rearrange("(j p) c -> p j c", p=128), writes=[f"vb{u}"])
                    P.dma("sp", vb[u][0:64, 4, :], vsc[0][b, r0 + 512:r0 + 576, 0:512], writes=[f"vb4{u}"])

            units = [(i, h) for i in range(18) for h in range(8)]
            pid_of = lambda i: {0: 0, 1: 1, 14: 3, 15: 4}.get(i, 2)
            state = {"pid": -1}

            def stage1(n):
                i, h = units[n]
                u = i % 2
                cb, var = h // 2, h % 2
                if h == 2 and i + 1 < 18:
                    load_tile(i + 1)
                if h == 0:
                    if i < 16 and pid_of(i) != state["pid"]:
                        for hh in range(8):
                            P.dma("sp", bias[:, hh, :], nabias_d[pid_of(i), :, hh, :], writes=[f"bias{hh}"])
                        state["pid"] = pid_of(i)
                su = n % 2
                sbank = psa[:, su * 1024:su * 1024 + 832]
                skeys = [f"ps{2 * su}", f"ps{2 * su + 1}"]
                st_ = sts[n % 6]
                stk = f"st{n % 6}"
                qap = qz[u][var][:, cb, :]
                qk = [f"qz{u}{var}", f"qzz{u}{var}"]
                pb = Pb[n % 3]
                if i < 16:
                    for (c0, w) in ((0, 512), (512, 64)):
                        P.op("pe", lambda e, sbank=sbank, qap=qap, u=u, cb=cb, c0=c0, w=w: e.matmul(sbank[:, c0:c0 + w], qap, kb[u][:, cb, c0:c0 + w], start=True, stop=True),
                             reads=qk + [f"kb{u}"], writes=skeys)
                    P.op("pe", lambda e, sbank=sbank, qap=qap, cb=cb: e.matmul(sbank[:, 576:832], qap, kctx[:, cb, :], start=True, stop=True),
                         reads=qk + ["kctx"], writes=skeys)
                    Tb = Tt[n % 3]
                    tk = f"T{n % 3}"
                    P.op("dve", lambda e, Tb=Tb, sbank=sbank, h=h: e.scalar_tensor_tensor(Tb, sbank, 0.125, bias[:, h, :], ALU.mult, ALU.add),
                         reads=skeys + [f"bias{h}"], writes=[tk])
                    src, srck, nk, scale = Tb, [tk], 832, 1.0
                else:
                    P.op("pe", lambda e, sbank=sbank, qap=qap, cb=cb: e.matmul(sbank[:, 0:256], qap, kctx[:, cb, :], start=True, stop=True),
                         reads=qk + ["kctx"], writes=skeys)
                    src, srck, nk, scale = sbank[:, 0:256], skeys, 256, 0.125
                P.op("dve", lambda e, st_=st_, src=src: e.reduce_max(st_[:, 0:1], src, axis=AX.X), reads=srck, writes=[stk + "m"])
                P.op("dve", lambda e, st_=st_, scale=scale: e.tensor_scalar(st_[:, 1:2], st_[:, 0:1], -scale, None, ALU.mult), reads=[stk + "m"], writes=[stk + "n"])
                P.op("act", lambda e, pb=pb, src=src, nk=nk, scale=scale, st_=st_: e.activation(pb[:, 0:nk], src, AF.Exp, bias=st_[:, 1:2], scale=scale, accum_out=st_[:, 2:3]),
                     reads=list(srck) + [stk + "n"], writes=[f"P{n % 3}", stk + "s"])

            def stage2(n):
                i, h = units[n]
                u = i % 2
                st_ = sts[n % 6]
                stk = f"st{n % 6}"
                pb = Pb[n % 3]
                ptp = PTp[n % 2]
                ptk = f"ps{5 + n % 2}"
                pts = PT[n % 2]
                ob, obk = Obk[n % 2]
                if i < 16:
                    chunks = [(j * 128, vb[u][:, j, h * 64:(h + 1) * 64], [f"vb{u}"]) for j in range(4)]
                    chunks.append((512, vb[u][:, 4, h * 64:(h + 1) * 64], [f"vb4{u}", f"vbz{u}"]))
                    chunks += [(576 + j * 128, vctx[:, j, h * 64:(h + 1) * 64], ["vctx"]) for j in range(2)]
                else:
                    chunks = [(j * 128, vctx[:, j, h * 64:(h + 1) * 64], ["vctx"]) for j in range(2)]
                nch = len(chunks)
                for j, (c0, v_ap, vk) in enumerate(chunks):
                    P.op("pe", lambda e, j=j, c0=c0, ptp=ptp, pb=pb: e.transpose(ptp[:, j * 128:(j + 1) * 128], pb[:, c0:c0 + 128], identb),
                         reads=[f"P{n % 3}", "identb"], writes=[ptk])
                P.op("dve", lambda e, pts=pts, ptp=ptp, nch=nch: e.tensor_copy(pts[:, 0:nch * 128], ptp[:, 0:nch * 128]), reads=[ptk], writes=[f"PT{n % 2}"])
                Oa = ob[:, 0:64]
                for j, (c0, v_ap, vk) in enumerate(chunks):
                    P.op("pe", lambda e, j=j, Oa=Oa, pts=pts, v_ap=v_ap, nch=nch: e.matmul(Oa, pts[:, j * 128:(j + 1) * 128], v_ap, start=(j == 0), stop=(j == nch - 1)),
                         reads=[f"PT{n % 2}"] + vk, writes=[obk])
                P.op("dve", lambda e, st_=st_: e.reciprocal(st_[:, 2:3], st_[:, 2:3]), reads=[stk + "s"], writes=[stk + "s"])
                P.op("dve", lambda e, Oa=Oa, u=u, h=h, st_=st_: e.tensor_scalar(onat[u][:, h * 64:(h + 1) * 64], Oa, st_[:, 2:3], None, ALU.mult),
                     reads=[obk, stk + "s"], writes=[f"onat{u}"])
                if h == 7:
                    P.dma("sp", att[b, i * 128:(i + 1) * 128, 0:512], onat[u], reads=[f"onat{u}"])

            load_tile(0)
            N = len(units)
            stage1(0)
            stage1(1)
            for n in range(N):
                stage2(n)
                if n + 2 < N:
                    stage1(n + 2)
            P.barrier()

        def phase_full_attn(l, b):
            A.reset()
            dv = 128 if l == 0 else 64
            nsub = 2 if l == 0 else 4
            kT = [A.bf(T), A.bf(T)]
            qTa = [A.bf(2, T) if l == 1 else A.bf(1, T) for _ in range(2)]
            Vt = [A.bf(18, dv + (1 if l == 1 else 0)) for _ in range(2)]
            nqc, nqt = (2, SEQ) if l == 1 else (1, T)
            qz = [[A.bf(nqc, nqt), A.bf(nqc, nqt)] for _ in range(2)]
            Pc = [A.bf(2, 512) for _ in range(3)]
            onesb = A.bf(128)
            onesf = A.f32(128)
            rrow = [A.f32(512), A.f32(512)]
            bcs = [A.f32(512), A.f32(512)]
            outb = [A.bf(512), A.bf(512)]
            negB = [A.f32(8), A.f32(8)]
            kst = A.f32(32)
            if l == 0:
                ksq = A.bf(T)
                qsq = A.bf(T)
                sel = [A.bf(128), A.bf(128)]
                i0 = [A.f32(512), A.f32(512)]
                aa = A.f32(512)
                cc_ = A.f32(512)
                oo = [A.f32(512), A.f32(512)]
                sq = [A.f32(512), A.f32(512)]
                lnv = [A.f32(512), A.f32(512)]
                subgc = A.f32(8)
            else:
                grow = A.f32(128)
            Sb = [bank(0), bank(1), bank(2)]
            Sb2 = [psa[:, 0:1024].rearrange("p (a n) -> p a n", a=2), psa[:, 1024:2048].rearrange("p (a n) -> p a n", a=2)]
            Ob = [bank(4), bank(5)]
            Rb = [bank(6), bank(7)]
            P.op("dve", lambda e: e.memset(onesb, 1.0), writes=["onesb"])
            P.op("dve", lambda e: e.memset(onesf, 1.0), writes=["onesf"])
            for gu_ in range(2):
                P.op("pool", lambda e, gu_=gu_: e.memset(qz[gu_][0][64:128], 0.0), writes=[f"qzz{gu_}0"])
                P.op("pool", lambda e, gu_=gu_: e.memset(qz[gu_][1][0:64], 0.0), writes=[f"qzz{gu_}1"])
            if l == 0:
                for r in range(2):
                    P.op("dve", lambda e, r=r: e.memset(sel[r], 0.0), writes=[f"sel{r}"])
                    P.op("dve", lambda e, r=r: e.memset(sel[r][r * 64:(r + 1) * 64, :], 1.0), writes=[f"sel{r}"])
                P.dma("sp", subgc[:, 0:1], subgc_d, writes=["subgc"])
                P.op("dve", lambda e: e.tensor_scalar(subgc[:, 0:1], subgc[:, 0:1], 1.0 - LAM_INIT0, None, ALU.mult), reads=["subgc"], writes=["subgc"])
                P.op("dve", lambda e: e.tensor_scalar(subgc[:, 1:2], lamt[:, 0:1], -1.0, None, ALU.mult), writes=["neglam"])
            else:
                P.dma("sp", grow, grow_d.partition_broadcast(128), writes=["grow"])
                P.op("dve", lambda e: e.reduce_max(kst[:, 0:1], grow[:, 0:64], axis=AX.X, apply_absolute_value=True), reads=["grow"], writes=["kst0"])
                P.op("dve", lambda e: e.reduce_max(kst[:, 1:2], grow[:, 64:128], axis=AX.X, apply_absolute_value=True), reads=["grow"], writes=["kst1"])
                for gu in range(2):
                    P.op("dve", lambda e, gu=gu: e.scalar_tensor_tensor(negB[gu][:, 0:1], kst[:, 0:1], -8.0, kst[:, 1:2], ALU.mult, ALU.mult), reads=["kst0", "kst1"], writes=[f"negB{gu}_0"])

            def load_g(g):
                gu = g % 2
                if l == 0:
                    P.dma("sp", kT[gu], qkT[0][b, 12 + g], writes=[f"kT{gu}"])
                    P.dma("sp", qTa[gu][:, 0, :], qkT[0][b, 8 + g], writes=[f"qTa{gu}"])
                    P.dma("sp", qz[gu][0][0:64, 0, :], qkT[0][b, 8 + g, 0:64, :], writes=[f"qz{gu}0"])
                    P.dma("sp", qz[gu][1][64:128, 0, :], qkT[0][b, 8 + g, 64:128, :], writes=[f"qz{gu}1"])
                    P.dma("sp", Vt[gu], vsc[0][b, :, 512 + 128 * g:512 + 128 * (g + 1)].rearrange("(j p) c -> p j c", p=128), writes=[f"V{gu}"])
                else:
                    P.dma("sp", kT[gu], qkT[1][b, 8 + g], writes=[f"kT{gu}"])
                    P.dma("sp", qz[gu][0][0:64], qkT[1][b, 2 * g:2 * g + 2, 0:64, 0:SEQ].rearrange("c p t -> p c t"), writes=[f"qz{gu}0"])
                    P.dma("sp", qz[gu][1][64:128], qkT[1][b, 2 * g:2 * g + 2, 64:128, 0:SEQ].rearrange("c p t -> p c t"), writes=[f"qz{gu}1"])
                    P.dma("sp", Vt[gu][:, :, 0:64], vsc[1][b, :, 64 * g:64 * (g + 1)].rearrange("(j p) c -> p j c", p=128), writes=[f"V{gu}"])
                    P.op("pool", lambda e, gu=gu: e.memset(Vt[gu][:, :, 64:65], 1.0), writes=[f"V{gu}"])

            def group_prologue(g):
                gu = g % 2
                if l != 0:
                    return
                P.op("pool", lambda e, gu=gu: e.tensor_tensor(ksq, kT[gu], kT[gu], ALU.mult), reads=[f"kT{gu}"], writes=["ksq"])
                P.op("pool", lambda e, gu=gu: e.tensor_tensor(qsq, qTa[gu][:, 0, :], qTa[gu][:, 0, :], ALU.mult), reads=[f"qTa{gu}"], writes=["qsq"])
                for r in range(2):
                    for si, (src, sk_) in enumerate(((ksq, "ksq"), (qsq, "qsq"))):
                        for cc in range(5):
                            w = min(512, T - cc * 512)
                            bi = (si * 5 + cc) % 3
                            pb = Sb[bi]
                            P.op("pe", lambda e, pb=pb, r=r, cc=cc, w=w, src=src: e.matmul(pb[:, 0:w], sel[r], src[:, cc * 512:cc * 512 + w], start=True, stop=True),
                                 reads=[sk_, f"sel{r}"], writes=[f"ps{bi}"])
                            col = 8 + si * 5 + cc
                            P.op("dve", lambda e, pb=pb, col=col, w=w: e.reduce_max(kst[:, col:col + 1], pb[:, 0:w], axis=AX.X), reads=[f"ps{bi}"], writes=[f"kstc{col}"])
                    P.op("dve", lambda e, r=r: e.reduce_max(kst[:, 4:5], kst[:, 8:13], axis=AX.X), reads=[f"kstc{c}" for c in range(8, 13)], writes=["kstk"])
                    P.op("dve", lambda e, r=r: e.reduce_max(kst[:, 5:6], kst[:, 13:18], axis=AX.X), reads=[f"kstc{c}" for c in range(13, 18)], writes=["kstq"])
                    P.op("dve", lambda e: e.tensor_tensor(kst[:, 6:7], kst[:, 4:5], kst[:, 5:6], ALU.add), reads=["kstk", "kstq"], writes=["kstsum"])
                    P.op("dve", lambda e, r=r, gu=gu: e.tensor_scalar(negB[gu][:, r:r + 1], kst[:, 6:7], -0.0625, None, ALU.mult), reads=["kstsum"], writes=[f"negB{gu}_{r}"])

            units = []
            for g in range(4):
                for G in range(5 if l == 0 else 4):
                    for r in range(nsub):
                        units.append((g, G, r))
            items = []
            for ui, (g, G, r) in enumerate(units):
                kcs = list(range(16, 18)) if G == 4 else list(range(18))
                for ki in range(0, len(kcs), 2):
                    items.append((ui, ki // 2, kcs[ki], len(kcs) // 2))
            n = len(items)
            state = {"oi": 0}
            pending = []

            def qcols(G):
                return (SEQ, NCTX) if G == 4 else (G * 512, 512)

            def stage_S(k):
                ui, ki, kc, nkc = items[k]
                g, G, r = units[ui]
                gu = g % 2
                if ki == 0 and G == 0 and r == 0:
                    group_prologue(g)
                if ki == 0 and G == 1 and r == 0 and g + 1 < 4:
                    load_g(g + 1)
                q0, nq = qcols(G)
                if l == 0:
                    var = r
                    qap = qz[gu][var][:, 0, q0:q0 + nq]
                    nbc = r
                else:
                    var = (4 * g + r) % 2
                    qap = qz[gu][var][:, r // 2, q0:q0 + nq]
                    nbc = 0
                sb = Sb2[k % 2]
                skeys = [f"ps{2 * (k % 2)}", f"ps{2 * (k % 2) + 1}"]
                for a_ in range(2):
                    P.op("pe", lambda e, sb=sb, qap=qap, gu=gu, kc=kc, nq=nq, a_=a_: e.matmul(sb[:, a_, 0:nq], kT[gu][:, (kc + a_) * 128:(kc + a_ + 1) * 128], qap, start=True, stop=True),
                         reads=[f"qz{gu}{var}", f"qzz{gu}{var}", f"kT{gu}"], writes=[skeys[a_]])
                pc = Pc[k % 3]
                P.op("act", lambda e, pc=pc, sb=sb, nq=nq, gu=gu, nbc=nbc: e.activation(pc[:, :, 0:nq], sb[:, :, 0:nq], AF.Exp, bias=negB[gu][:, nbc:nbc + 1], scale=0.125),
                     reads=skeys + [f"negB{gu}_{nbc}"], writes=[f"Pc{k % 3}"])

            def stage_V(k):
                ui, ki, kc, nkc = items[k]
                g, G, r = units[ui]
                gu = g % 2
                q0, nq = qcols(G)
                pc = Pc[k % 3]
                ob = Ob[ui % 2]
                rb = Rb[ui % 2]
                okey, rkey = f"ps{4 + ui % 2}", f"ps{6 + ui % 2}"
                M = dv + (1 if l == 1 else 0)
                for a_ in range(2):
                    P.op("pe", lambda e, ob=ob, pc=pc, gu=gu, kc=kc, nq=nq, M=M, ki=ki, nkc=nkc, a_=a_: e.matmul(ob[0:M, 0:nq], Vt[gu][:, kc + a_, 0:M], pc[:, a_, 0:nq], start=(ki == 0 and a_ == 0), stop=(ki == nkc - 1 and a_ == 1)),
                         reads=[f"Pc{k % 3}", f"V{gu}"], writes=[okey])
                if l == 0:
                    for a_ in range(2):
                        P.op("pe", lambda e, rb=rb, pc=pc, nq=nq, ki=ki, nkc=nkc, a_=a_: e.matmul(rb[:, 0:nq], onesb, pc[:, a_, 0:nq], start=(ki == 0 and a_ == 0), stop=(ki == nkc - 1 and a_ == 1)),
                             reads=[f"Pc{k % 3}", "onesb"], writes=[rkey])
                if ki != nkc - 1:
                    return
                if l == 1:
                    oi = state["oi"]
                    state["oi"] += 1
                    u2 = oi % 2
                    P.op("dve", lambda e, u2=u2, ob=ob, nq=nq: e.reciprocal(rrow[u2][64:65, 0:nq], ob[64:65, 0:nq]), reads=[okey], writes=[f"rrow{u2}"])

                    def part_b(u2=u2, ob=ob, rb=rb, nq=nq, q0=q0, g=g, r=r, okey=okey, rkey=rkey):
                        P.op("pe", lambda e: e.matmul(rb[0:64, 0:nq], onesf[64:65, 0:64], rrow[u2][64:65, 0:nq], start=True, stop=True),
                             reads=[f"rrow{u2}", "onesf"], writes=[rkey])
                        P.op("dve", lambda e: e.tensor_copy(bcs[u2][0:64, 0:nq], rb[0:64, 0:nq]), reads=[rkey], writes=[f"bcs{u2}"])
                        P.op("dve", lambda e: e.tensor_tensor(outb[u2][0:64, 0:nq], ob[0:64, 0:nq], bcs[u2][0:64, 0:nq], ALU.mult), reads=[okey, f"bcs{u2}"], writes=[f"outb{u2}"])
                        hq = 4 * g + r
                        pb0 = (hq % 2) * 64
                        P.dma("sp", attT[b, hq // 2, pb0:pb0 + 64, q0:q0 + nq], outb[u2][0:64, 0:nq], reads=[f"outb{u2}"])
                    pending.append((k + 2, part_b))
                else:
                    if r == 0:
                        P.op("dve", lambda e, rb=rb, nq=nq: e.reciprocal(i0[0][:, 0:nq], rb[:, 0:nq]), reads=[rkey], writes=["i0a"])
                        P.op("dve", lambda e, ob=ob, nq=nq: e.tensor_tensor(aa[:, 0:nq], ob[:, 0:nq], i0[0][:, 0:nq], ALU.mult), reads=[okey, "i0a"], writes=["aa"])
                        return
                    oi = state["oi"]
                    state["oi"] += 1
                    u2 = oi % 2
                    P.op("dve", lambda e, rb=rb, nq=nq: e.reciprocal(i0[1][:, 0:nq], rb[:, 0:nq]), reads=[rkey], writes=["i0b"])
                    P.op("dve", lambda e, ob=ob, nq=nq: e.tensor_tensor(cc_[:, 0:nq], ob[:, 0:nq], i0[1][:, 0:nq], ALU.mult), reads=[okey, "i0b"], writes=["cc"])
                    P.op("dve", lambda e, u2=u2, nq=nq: e.scalar_tensor_tensor(oo[u2][:, 0:nq], cc_[:, 0:nq], subgc[:, 1:2], aa[:, 0:nq], ALU.mult, ALU.add), reads=["cc", "aa", "neglam"], writes=[f"oo{u2}"])
                    P.op("pool", lambda e, u2=u2, nq=nq: e.tensor_tensor(sq[u2][:, 0:nq], oo[u2][:, 0:nq], oo[u2][:, 0:nq], ALU.mult), reads=[f"oo{u2}"], writes=[f"sq{u2}"])

                    def part_b0(u2=u2, nq=nq, rb=rb, rkey=rkey, q0=q0, g=g):
                        P.op("pe", lambda e: e.matmul(rb[:, 0:nq], onesf, sq[u2][:, 0:nq], start=True, stop=True), reads=[f"sq{u2}", "onesf"], writes=[rkey])
                        P.op("act", lambda e: e.activation(lnv[u2][:, 0:nq], rb[:, 0:nq], AF.Ln, bias=cst[:, 1:2], scale=1.0 / 128.0), reads=[rkey], writes=[f"lnv{u2}"])
                        P.op("act", lambda e: e.activation(lnv[u2][:, 0:nq], lnv[u2][:, 0:nq], AF.Exp, bias=cst[:, 2:3], scale=-0.5), reads=[f"lnv{u2}"], writes=[f"lnv{u2}"])
                        P.op("dve", lambda e: e.scalar_tensor_tensor(outb[u2][:, 0:nq], oo[u2][:, 0:nq], subgc[:, 0:1], lnv[u2][:, 0:nq], ALU.mult, ALU.mult), reads=[f"oo{u2}", f"lnv{u2}", "subgc"], writes=[f"outb{u2}"])
                        P.dma("sp", attT[b, 4 + g, :, q0:q0 + nq], outb[u2][:, 0:nq], reads=[f"outb{u2}"])
                    pending.append((k + 2, part_b0))

            load_g(0)
            for k in range(-1, n):
                if k + 1 < n:
                    stage_S(k + 1)
                if k >= 0:
                    stage_V(k)
                while pending and pending[0][0] <= k:
                    pending.pop(0)[1]()
            while pending:
                pending.pop(0)[1]()
            P.barrier()

        def ln_ops(z, zk, o, okey, lng, lnb, st, stk, bn, n_t, nk):
            return [
                lambda: P.op("dve", lambda e: e.bn_stats(bn[:, 0, :], z[:, 0:512]), reads=[zk], writes=[stk + "b0"]),
                lambda: P.op("dve", lambda e: e.bn_stats(bn[:, 1, :], z[:, 512:1024]), reads=[zk], writes=[stk + "b1"]),
                lambda: P.op("dve", lambda e: e.bn_aggr(st[:, 0:2], bn.rearrange("p a b -> p (a b)")), reads=[stk + "b0", stk + "b1"], writes=[stk + "mv"]),
                lambda: P.op("act", lambda e: e.activation(st[:, 2:3], st[:, 1:2], AF.Sqrt, bias=cst[:, 0:1], scale=1.0), reads=[stk + "mv"], writes=[stk + "sd"]),
                lambda: P.op("dve", lambda e: e.reciprocal(st[:, 2:3], st[:, 2:3]), reads=[stk + "sd"], writes=[stk + "sd"]),
                lambda: P.op("dve", lambda e: e.scalar_tensor_tensor(st[:, 3:4], st[:, 0:1], -1.0, st[:, 2:3], ALU.mult, ALU.mult), reads=[stk + "mv", stk + "sd"], writes=[stk + "nb"]),
                lambda: P.op("act", lambda e: e.activation(n_t, z, AF.Identity, bias=st[:, 3:4], scale=st[:, 2:3]), reads=[zk, stk + "nb", stk + "sd"], writes=[nk]),
                lambda: P.op("pool", lambda e: e.tensor_tensor(n_t, n_t, lng, ALU.mult), reads=[nk, "lng"], writes=[nk]),
                lambda: P.op("dve", lambda e: e.tensor_tensor(o, n_t, lnb, ALU.add), reads=[nk, "lnb"], writes=[okey]),
            ]

        def zip_run(lists):
            n_ = max(len(x) for x in lists)
            for j in range(n_):
                for x in lists:
                    if j < len(x):
                        x[j]()

        def phase_outproj(l, b, Xsrc, Xdst, ntl):
            A.reset()
            wo = A.bf(8, 1024)
            gbc = [A.f32(1024), A.f32(1024)]
            lng = A.f32(1024)
            lnb = A.f32(1024)
            at = [A.bf(1024), A.bf(1024)]
            aT = [A.bf(8, 128), A.bf(8, 128)]
            xt = [A.f32(1024), A.f32(1024)]
            z = [A.f32(1024), A.f32(1024)]
            n_t = [A.f32(1024), A.f32(1024)]
            o = [A.f32(1024), A.f32(1024)]
            bn = [A.f32(2, 6), A.f32(2, 6)]
            sts = [A.f32(8), A.f32(8)]
            for c0 in (0, 512):
                P.dma("pool", wo[:, :, c0:c0 + 512], L[l]["w_out"][:, c0:c0 + 512].rearrange("(k p) n -> p k n", p=128), writes=["wo"])
            P.dma("sp", gbc[0], gates_d[l, 0, b], writes=["gbc0"])
            P.dma("sp", gbc[1], gates_d[l, 0, 2], writes=["gbc1"])
            P.dma("sp", lng, L[l]["ln"][0:1, :].partition_broadcast(128), writes=["lng"])
            P.dma("sp", lnb, L[l]["ln"][1:2, :].partition_broadcast(128), writes=["lnb"])
            tl = tiles_of(Xsrc, b, nlat=16, nctx=ntl - 16, l0=(l == 0))
            def load_t(i):
                uu = i % 2
                if l == 0:
                    P.dma("sp", at[uu][:, 0:512], att[b, i * 128:(i + 1) * 128, 0:512], writes=[f"at{uu}"])
                    P.dma("sp", aT[uu][:, 4:8, :], attT[b, 4:8, :, i * 128:(i + 1) * 128].rearrange("c p t -> p c t"), writes=[f"aTd{uu}"])
                else:
                    P.dma("sp", aT[uu], attT[b, :, :, i * 128:(i + 1) * 128].rearrange("c p t -> p c t"), writes=[f"aTd{uu}"])
                P.dma("sp", xt[uu], tl[i][0], writes=[f"xt{uu}"])

            def stage_A(i):
                u = i % 2
                src, m = tl[i]
                akeys = [f"aTd{u}"]
                if l == 0:
                    ptb = bank(u).bitcast(BF16)
                    for k in range(4):
                        P.op("pe", lambda e, ptb=ptb, k=k, u=u: e.transpose(ptb[:, k * 128:(k + 1) * 128], at[u][:, k * 128:(k + 1) * 128], identb),
                             reads=[f"at{u}", "identb"], writes=[f"ps{u}"])
                    P.op("dve", lambda e, u=u, ptb=ptb: e.tensor_copy(aT[u][:, 0:4, :].rearrange("p k t -> p (k t)"), ptb[:, 0:512]), reads=[f"ps{u}"], writes=[f"aT{u}"])
                    akeys.append(f"aT{u}")
                yb = psa[:, 1024 + u * 1024:2048 + u * 1024]
                for half in range(2):
                    for k in range(8):
                        P.op("pe", lambda e, yb=yb, half=half, k=k, u=u: e.matmul(yb[:, half * 512:(half + 1) * 512], aT[u][:, k, :], wo[:, k, half * 512:(half + 1) * 512], start=(k == 0), stop=(k == 7)),
                             reads=akeys + ["wo"], writes=[f"ps{2 + 2 * u + half}"])
                gsel = 0 if m < 2 else 1
                P.op("dve", lambda e, u=u, yb=yb, gsel=gsel: e.tensor_tensor(z[u], yb, gbc[gsel], ALU.mult), reads=[f"ps{2 + 2 * u}", f"ps{3 + 2 * u}", f"gbc{gsel}"], writes=[f"z{u}"])
                P.op("dve", lambda e, u=u: e.scalar_tensor_tensor(z[u], xt[u], ALPHA, z[u], ALU.mult, ALU.add), reads=[f"xt{u}", f"z{u}"], writes=[f"z{u}"])

            def stage_B_ops(i):
                u = i % 2
                return ln_ops(z[u], f"z{u}", o[u], f"o{u}", lng, lnb, sts[u], f"st{u}", bn[u], n_t[u], f"n{u}") + \
                    [lambda: P.dma("sp", Xdst[b, i * 128:(i + 1) * 128, :], o[u], reads=[f"o{u}"])]

            load_t(0)
            load_t(1)
            for i in range(0, len(tl), 2):
                stage_A(i)
                stage_A(i + 1)
                if i + 2 < len(tl):
                    load_t(i + 2)
                    load_t(i + 3)
                zip_run([stage_B_ops(i), stage_B_ops(i + 1)])
            P.barrier()


        def phase_ffn(l, tl, dsts, final):
            A.reset()
            NT = len(tl)
            moe = (l == 1)
            FD = EDIM if moe else FFN
            nexp = NEXP if moe else 1
            hT = A.bf(8, NT * 128)
            yacc = A.f32(NT, 1024)
            G = A.f32(NT, 8)
            wa = [A.bf(8, 512), A.bf(8, 512)]
            wg = [A.bf(8, 512), A.bf(8, 512)]
            w2 = [A.bf(4, 1024), A.bf(4, 1024)]
            mark = A.off
            fchunks = []
            f0 = 0
            while f0 < FD:
                fw = min(512, FD - f0)
                fchunks.append((f0, fw))
                f0 += fw
            wlist = [(ex_i, f0, fw) for ex_i in range(nexp) for (f0, fw) in fchunks]
            loaded = set()

            def load_w(wi_):
                if wi_ in loaded or wi_ >= len(wlist):
                    return
                loaded.add(wi_)
                ex_i, f0, fw = wlist[wi_]
                wu = wi_ % 2
                if moe:
                    w13s, w2s = w13_1[ex_i], w2_1[ex_i]
                else:
                    w13s, w2s = w13_0, w2_0
                P.dma("pool", wa[wu][:, :, 0:fw], w13s[:, f0:f0 + fw].rearrange("(k p) n -> p k n", p=128), writes=[f"wa{wu}"])
                P.dma("pool", wg[wu][:, :, 0:fw], w13s[:, FD + f0:FD + f0 + fw].rearrange("(k p) n -> p k n", p=128), writes=[f"wg{wu}"])
                P.dma("pool", w2[wu][:, 0:fw // 128, :], w2s[f0:f0 + fw, :].rearrange("(j p) n -> p j n", p=128), writes=[f"w2{wu}"])

            load_w(0)
            load_w(1)
            xt = [A.f32(1024), A.f32(1024)]
            hTf = [A.f32(8, 128), A.f32(8, 128)]
            rw = A.f32(8, 8)
            lg = [A.f32(8), A.f32(8)]
            ex = [A.f32(8), A.f32(8)]
            sts = [A.f32(16), A.f32(16)]
            if moe:
                P.dma("sp", rw, router_d.rearrange("(k p) n -> p k n", p=128), writes=["rw"])
            cnt = [0]
            P.dma("sp", xt[0], tl[0][0], writes=["xt0"])
            for i, (src, m) in enumerate(tl):
                u = i % 2
                if i + 1 < NT:
                    P.dma("sp", xt[(i + 1) % 2], tl[i + 1][0], writes=[f"xt{(i + 1) % 2}"])
                if moe:
                    emit_hT(l, 1, xt[u], f"xt{u}", m, hT, "hT", i * 128, [bank(0), bank(1)], ["ps0", "ps1"], cnt, hTf=hTf[u], hfk=f"hTf{u}")
                    pl = bank(2 + u, 8)
                    for k in range(8):
                        P.op("pe", lambda e, pl=pl, k=k, u=u: e.matmul(pl, hTf[u][:, k, :], rw[:, k, :], start=(k == 0), stop=(k == 7)),
                             reads=[f"hTf{u}", "rw"], writes=[f"ps{2 + u}"])
                    s_ = sts[u]
                    sk = f"rst{u}"
                    P.op("dve", lambda e, u=u, pl=pl: e.tensor_copy(lg[u], pl), reads=[f"ps{2 + u}"], writes=[f"lg{u}"])
                    P.op("dve", lambda e, u=u, s_=s_: e.max(s_[:, 0:8], lg[u]), reads=[f"lg{u}"], writes=[sk + "a"])
                    P.op("dve", lambda e, s_=s_: e.tensor_scalar(s_[:, 8:9], s_[:, 0:1], -1.0, None, ALU.mult), reads=[sk + "a"], writes=[sk + "b"])
                    P.op("act", lambda e, u=u, s_=s_: e.activation(ex[u], lg[u], AF.Exp, bias=s_[:, 8:9], scale=1.0), reads=[f"lg{u}", sk + "b"], writes=[f"ex{u}"])
                    P.op("dve", lambda e, u=u, s_=s_: e.tensor_scalar(lg[u], lg[u], s_[:, 1:2], None, ALU.is_ge), reads=[f"lg{u}", sk + "a", f"ex{u}"], writes=[f"lg{u}"])
                    P.op("dve", lambda e, u=u: e.tensor_tensor(ex[u], ex[u], lg[u], ALU.mult), reads=[f"lg{u}", f"ex{u}"], writes=[f"ex{u}"])
                    P.op("dve", lambda e, u=u, s_=s_: e.reduce_sum(s_[:, 9:10], ex[u], axis=AX.X), reads=[f"ex{u}"], writes=[sk + "c"])
                    P.op("dve", lambda e, s_=s_: e.reciprocal(s_[:, 9:10], s_[:, 9:10]), reads=[sk + "c"], writes=[sk + "c"])
                    P.op("dve", lambda e, u=u, i=i, s_=s_: e.tensor_scalar(G[:, i, :], ex[u], s_[:, 9:10], None, ALU.mult), reads=[f"ex{u}", sk + "c"], writes=["G"])
                else:
                    emit_hT(l, 1, xt[u], f"xt{u}", m, hT, "hT", i * 128, [bank(0), bank(1)], ["ps0", "ps1"], cnt)
            P.barrier()
            A.off = mark
            uT = [A.bf(4, 512), A.bf(4, 512)]
            sa = [A.f32(512), A.f32(512)]
            ev = [A.f32(1024), A.f32(1024)]
            tgs = [(t0, min(4, NT - t0)) for t0 in range(0, NT, 4)]
            items = []
            for wi, (ex_i, f0, fw) in enumerate(wlist):
                for (t0, nt) in tgs:
                    items.append((ex_i, f0, fw, t0, nt, wi))

            upc = [0]

            def emit_up(it, n):
                ex_i, f0, fw, t0, nt, wi_ = it
                wu = wi_ % 2
                uu = n % 2
                ntok = nt * 128
                for fs in range(fw // 128):
                    q = upc[0] % 2
                    upc[0] += 1
                    pa, pg = bank(q), bank(2 + q)
                    for k in range(8):
                        P.op("pe", lambda e, pa=pa, k=k, fs=fs, wu=wu, t0=t0, ntok=ntok: e.matmul(pa[:, 0:ntok], wa[wu][:, k, fs * 128:(fs + 1) * 128], hT[:, k, t0 * 128:t0 * 128 + ntok], start=(k == 0), stop=(k == 7)),
                             reads=[f"wa{wu}"], writes=[f"ps{q}"])
                    for k in range(8):
                        P.op("pe", lambda e, pg=pg, k=k, fs=fs, wu=wu, t0=t0, ntok=ntok: e.matmul(pg[:, 0:ntok], wg[wu][:, k, fs * 128:(fs + 1) * 128], hT[:, k, t0 * 128:t0 * 128 + ntok], start=(k == 0), stop=(k == 7)),
                             reads=[f"wg{wu}"], writes=[f"ps{2 + q}"])
                    P.op("act", lambda e, q=q, pa=pa, ntok=ntok: e.activation(sa[q][:, 0:ntok], pa[:, 0:ntok], AF.Silu), reads=[f"ps{q}"], writes=[f"sa{q}"])
                    P.op("dve", lambda e, q=q, pg=pg, uu=uu, fs=fs, ntok=ntok: e.tensor_tensor(uT[uu][:, fs, 0:ntok], sa[q][:, 0:ntok], pg[:, 0:ntok], ALU.mult),
                         reads=[f"sa{q}", f"ps{2 + q}"], writes=[f"uT{uu}"])

            dnc = [0]
            first_done = set()

            def emit_down(it, n):
                ex_i, f0, fw, t0, nt, wi_ = it
                wu = wi_ % 2
                uu = n % 2
                nfs = fw // 128
                for tt in range(nt):
                    ti = t0 + tt
                    q = dnc[0] % 2
                    dnc[0] += 1
                    pd = psa[:, 2048 + q * 1024:3072 + q * 1024]
                    for oh in range(2):
                        for fs in range(nfs):
                            P.op("pe", lambda e, pd=pd, oh=oh, fs=fs, uu=uu, tt=tt, wu=wu, nfs=nfs: e.matmul(pd[:, oh * 512:(oh + 1) * 512], uT[uu][:, fs, tt * 128:(tt + 1) * 128], w2[wu][:, fs, oh * 512:(oh + 1) * 512], start=(fs == 0), stop=(fs == nfs - 1)),
                                 reads=[f"uT{uu}", f"w2{wu}"], writes=[f"ps{4 + 2 * q + oh}"])
                    yk = f"y{ti}"
                    first = ti not in first_done
                    first_done.add(ti)
                    if moe:
                        gsc = G[:, ti, ex_i:ex_i + 1]
                        if first:
                            P.op("act", lambda e, pd=pd, ti=ti, gsc=gsc: e.activation(yacc[:, ti, :], pd, AF.Identity, bias=cst[:, 2:3], scale=gsc), reads=[f"ps{4 + 2 * q}", f"ps{5 + 2 * q}"], writes=[yk])
                        else:
                            P.op("act", lambda e, pd=pd, q=q, gsc=gsc: e.activation(ev[q], pd, AF.Identity, bias=cst[:, 2:3], scale=gsc), reads=[f"ps{4 + 2 * q}", f"ps{5 + 2 * q}"], writes=[f"ev{q}"])
                            eng = "pool" if ti % 2 == 0 else "dve"
                            P.op(eng, lambda e, ti=ti, q=q: e.tensor_tensor(yacc[:, ti, :], yacc[:, ti, :], ev[q], ALU.add), reads=[f"ev{q}", yk], writes=[yk])
                    else:
                        if first:
                            P.op("act", lambda e, pd=pd, ti=ti: e.copy(yacc[:, ti, :], pd), reads=[f"ps{4 + 2 * q}", f"ps{5 + 2 * q}"], writes=[yk])
                        else:
                            P.op("dve", lambda e, pd=pd, ti=ti: e.tensor_tensor(yacc[:, ti, :], yacc[:, ti, :], pd, ALU.add), reads=[f"ps{4 + 2 * q}", f"ps{5 + 2 * q}", yk], writes=[yk])

            load_w(0)
            for n, it in enumerate(items):
                if n == 0 or items[n - 1][5] != it[5]:
                    load_w(it[5] + 1)
                if n == 0:
                    emit_up(it, 0)
                if n + 1 < len(items):
                    emit_up(items[n + 1], n + 1)
                emit_down(it, n)
            P.barrier()
            A.off = mark
            xt = [A.f32(1024), A.f32(1024)]
            gbm = [A.f32(1024), A.f32(1024), A.f32(1024)]
            lng = A.f32(1024)
            lnb = A.f32(1024)
            z = [A.f32(1024), A.f32(1024)]
            n_t = [A.f32(1024), A.f32(1024)]
            o = [A.f32(1024), A.f32(1024)]
            bn = [A.f32(2, 6), A.f32(2, 6)]
            st2 = [A.f32(8), A.f32(8)]
            ms = sorted(set(m for _, m in tl))
            for m in ms:
                P.dma("sp", gbm[m], gates_d[l, 1, m], writes=[f"gbm{m}"])
            P.dma("sp", lng, L[l]["ln"][2:3, :].partition_broadcast(128), writes=["lng"])
            P.dma("sp", lnb, L[l]["ln"][3:4, :].partition_broadcast(128), writes=["lnb"])
            def ld3(i):
                P.dma("sp", xt[i % 2], tl[i][0], writes=[f"xt{i % 2}"])

            def stA(i):
                u = i % 2
                m = tl[i][1]
                P.op("dve", lambda e, u=u, i=i, m=m: e.tensor_tensor(z[u], yacc[:, i, :], gbm[m], ALU.mult), reads=[f"gbm{m}"], writes=[f"z{u}"])
                P.op("dve", lambda e, u=u: e.scalar_tensor_tensor(z[u], xt[u], ALPHA, z[u], ALU.mult, ALU.add), reads=[f"xt{u}", f"z{u}"], writes=[f"z{u}"])

            def stB_ops(i):
                u = i % 2
                return ln_ops(z[u], f"z{u}", o[u], f"o{u}", lng, lnb, st2[u], f"lst{u}", bn[u], n_t[u], f"n{u}") + \
                    [lambda: P.dma("sp", dsts[i], o[u], reads=[f"o{u}"])]

            ld3(0)
            ld3(1)
            for i in range(0, NT, 2):
                stA(i)
                stA(i + 1)
                if i + 2 < NT:
                    ld3(i + 2)
                    ld3(i + 3)
                zip_run([stB_ops(i), stB_ops(i + 1)])
            P.barrier()

        plist = []
        plist.append(lambda: phase_mods(0))
        for b in range(2):
            plist.append(lambda b=b: phase_proj(0, b, None))
            plist.append(lambda b=b: phase_na(b))
            plist.append(lambda b=b: phase_full_attn(0, b))
            plist.append(lambda b=b: phase_outproj(0, b, None, X1, 18))
        for b in range(2):
            tl = [(X1[b, t * 128:(t + 1) * 128, :], b) for t in range(16)]
            ds = [X2[b, t * 128:(t + 1) * 128, :] for t in range(16)]
            plist.append(lambda tl=tl, ds=ds: phase_ffn(0, tl, ds, False))
        tl = [(X1[b, SEQ + t * 128:SEQ + (t + 1) * 128, :], 2) for b in range(2) for t in range(2)]
        ds = [X2[b, SEQ + t * 128:SEQ + (t + 1) * 128, :] for b in range(2) for t in range(2)]
        plist.append(lambda tl=tl, ds=ds: phase_ffn(0, tl, ds, False))
        plist.append(lambda: phase_mods(1))
        for b in range(2):
            plist.append(lambda b=b: phase_proj(1, b, X2))
            plist.append(lambda b=b: phase_full_attn(1, b))
            plist.append(lambda b=b: phase_outproj(1, b, X2, X3, 16))
            tl = [(X3[b, t * 128:(t + 1) * 128, :], b) for t in range(16)]
            ds = [out_d[b, t * 128:(t + 1) * 128, :] for t in range(16)]
            plist.append(lambda tl=tl, ds=ds: phase_ffn(1, tl, ds, True))
        for pf in plist[:nphases]:
            pf()
        P.emit()
    return nc


def _perm64(ncols):
    idx = np.arange(ncols)
    blk = idx // 64
    r = idx % 64
    return blk * 64 + (r + 32) % 64


def _na_bias_tables(rpb):
    out = np.zeros((5, 128, 8, 832), np.float32)
    reps = [0, 1, 2, 14, 15]
    qi = np.arange(128)
    rr, c = qi // 64, qi % 64
    kk = np.arange(576)
    kr, kc = kk // 64, kk % 64
    for pid, i in enumerate(reps):
        K0 = min(max(2 * i - 4, 0), 23)
        r = 2 * i + rr
        r0 = np.clip(r - 4, 0, 24)
        ws = np.clip(c - 8, 0, 48)
        krow = K0 + kr
        vrow = (krow[None, :] >= r0[:, None]) & (krow[None, :] < r0[:, None] + 8)
        vcol = (kc[None, :] >= ws[:, None]) & (kc[None, :] < ws[:, None] + 16)
        valid = vrow & vcol
        dr = np.clip(krow[None, :] - r[:, None] + 7, 0, 14)
        dc = np.clip(kc[None, :] - c[:, None] + 15, 0, 30)
        g = rpb[:, dr, dc]
        g = np.where(valid[None], g, np.float32(NEG))
        out[pid, :, :, 0:576] = g.transpose(1, 0, 2)
    return out


def _rope_tables():
    t = np.arange(SEQ)
    inv = (np.float32(10000.0) ** (-np.arange(16, dtype=np.float32) / np.float32(16))).astype(np.float32)
    ang = np.concatenate([(t // 64).astype(np.float32)[:, None] * inv, (t % 64).astype(np.float32)[:, None] * inv], axis=-1)
    cos = np.cos(ang).astype(np.float32).T
    sin = np.sin(ang).astype(np.float32).T
    C = np.ones((128, T), np.float32)
    S = np.zeros((128, T), np.float32)
    for p in range(128):
        C[p, :SEQ] = cos[p % 32]
        S[p, :SEQ] = sin[p % 32] if (p % 64) >= 32 else -sin[p % 32]
    return C, S


_CACHE = {}


def _prep(inp):
    f = lambda a: np.ascontiguousarray(np.asarray(a, dtype=np.float32))
    C, S = _rope_tables()
    blk = np.zeros((128, 128), np.float32)
    blk[:64, :64] = 1.0 / 64
    blk[64:, 64:] = 1.0 / 64
    shared = {"ident": np.eye(128, dtype=np.float32), "blk": blk, "ropeC": C, "ropeS": S}
    for l in range(2):
        shared[f"l{l}_ada_w"] = f(inp[f"l{l}_ada_w"])
        ab = f(inp[f"l{l}_ada_b"])
        shared[f"l{l}_ada_bT"] = np.ascontiguousarray(ab.reshape(48, 128).T)
        shared[f"l{l}_ada_b"] = ab.reshape(1, 6 * D)
        shared[f"l{l}_w_out"] = f(inp[f"l{l}_w_out"])
        shared[f"l{l}_ln"] = np.stack([f(inp[f"l{l}_ln1_g"]), f(inp[f"l{l}_ln1_b"]), f(inp[f"l{l}_ln2_g"]), f(inp[f"l{l}_ln2_b"])])
    w0 = f(inp["l0_w_in"])
    qa, ka, va, qb, kb, vb = w0[:, 0:512], w0[:, 512:1024], w0[:, 1024:1536], w0[:, 1536:2048], w0[:, 2048:2560], w0[:, 2560:3072]
    shared["l0_wfm"] = np.ascontiguousarray(np.concatenate([qa, ka, qb, kb], axis=1))
    p512 = _perm64(512)
    shared["l0_wfmp"] = np.ascontiguousarray(np.concatenate([qb[:, p512], kb[:, p512]], axis=1))
    shared["l0_wv"] = np.ascontiguousarray(np.concatenate([va, vb], axis=1))
    shared["l0_nabias"] = _na_bias_tables(f(inp["l0_rpb"]))
    shared["l0_lq"] = f(inp["l0_lambda_qk"]).reshape(1, 256)
    shared["l0_subg"] = f(inp["l0_subln_g"]).reshape(1, 128)
    shared["l0_subgc"] = f(inp["l0_subln_g"]).reshape(128, 1)
    shared["l0_w13"] = f(inp["l0_ffn_w13"])
    shared["l0_w2"] = f(inp["l0_ffn_w2"])
    w1 = f(inp["l1_w_in"])
    q1, k1, v1 = w1[:, 0:1024], w1[:, 1024:1280], w1[:, 1280:1536]
    kdup = np.concatenate([np.concatenate([k1[:, 64 * g:64 * (g + 1)]] * 2, axis=1) for g in range(4)], axis=1)
    wfm1 = np.concatenate([q1, kdup], axis=1)
    shared["l1_wfm"] = np.ascontiguousarray(wfm1)
    shared["l1_wfmp"] = np.ascontiguousarray(wfm1[:, _perm64(1536)])
    shared["l1_wv"] = np.ascontiguousarray(v1)
    gq, gk = f(inp["l1_q_norm_g"]), f(inp["l1_k_norm_g"])
    p64 = _perm64(64)
    shared["l1_gcols"] = np.ascontiguousarray(np.stack([np.tile(gq, 2), np.tile(gq[p64], 2), np.tile(gk, 2), np.tile(gk[p64], 2)], axis=1))
    shared["l1_grow"] = np.ascontiguousarray(np.concatenate([gq, gk]).reshape(1, 128))
    shared["l1_router"] = f(inp["l1_router_w"])
    shared["l1_w13"] = f(inp["l1_moe_w13"])
    shared["l1_w2"] = f(inp["l1_moe_w2"])
    x, ctx, c, c_ctx = f(inp["x"]), f(inp["ctx"]), f(inp["c"]), f(inp["c_ctx"])
    in_maps = []
    for i in range(NCORES):
        d = dict(shared)
        d["x"] = x[2 * i:2 * i + 2]
        d["ctx"] = ctx[2 * i:2 * i + 2]
        c3 = np.stack([c[2 * i], c[2 * i + 1], c_ctx])
        d["cT"] = np.ascontiguousarray(c3.reshape(3, 8, 128).transpose(2, 1, 0))
        in_maps.append(d)
    return in_maps


def kernel(**inp):
    if "nc" not in _CACHE:
        _CACHE["nc"] = build_program()
    nc = _CACHE["nc"]
    in_maps = _prep(inp)
    res = run_bass_kernel_spmd(nc, in_maps, core_ids=list(range(NCORES)))
    return np.concatenate([r["out"] for r in res.results], axis=0).astype(np.float32)
```

```python
import math
from contextlib import ExitStack
import numpy as np
import concourse.bass as bass
import concourse.mybir as mybir
from concourse.bass_utils import run_bass_kernel_spmd

F32 = mybir.dt.float32
BF16 = mybir.dt.bfloat16
AF = mybir.ActivationFunctionType
ALU = mybir.AluOpType
AX = mybir.AxisListType

NCORES = 8
D = 1024
SEQ = 2048
NCTX = 256
T = SEQ + NCTX
ALPHA = 4.0 ** 0.25
LN_EPS = 1e-5
RMS_EPS = 1e-6
FFN = 2816
EDIM = 3584
NEXP = 8
NEG = -30000.0
LAM_INIT0 = 0.8 - 0.6 * math.exp(-0.3 * 0)

ENGS = ("pe", "act", "dve", "pool", "sp")
SELF_SYNC = ("act", "dve", "pool")


class Op:
    __slots__ = ("eng", "fn", "deps", "idx", "is_dma", "waits", "signal", "sem", "target", "dma_prev")

    def __init__(self, eng, fn, is_dma):
        self.eng = eng
        self.fn = fn
        self.deps = []
        self.is_dma = is_dma
        self.waits = []
        self.signal = False
        self.sem = None
        self.target = None
        self.dma_prev = None


class Prog:
    def __init__(self, nc, n_rot=8, n_dma_sems=12):
        self.nc = nc
        self.ops = {e: [] for e in ENGS}
        self.last_writer = {}
        self.readers = {}
        self.n_rot = n_rot
        self.n_dma_sems = n_dma_sems
        self.dmas_live = []

    def _add(self, eng, fn, reads, writes, is_dma, extra=()):
        psr = tuple(k for k in reads if isinstance(k, str) and k.startswith("ps") and k[2:].isdigit())
        if psr:
            reads = tuple(k for k in reads if k not in psr)
            writes = tuple(writes) + tuple(k for k in psr if k not in writes)
        op = Op(eng, fn, is_dma)
        op.idx = len(self.ops[eng])
        deps = list(extra)
        for k in reads:
            w = self.last_writer.get(k)
            if w is not None:
                deps.append(w)
        for k in writes:
            w = self.last_writer.get(k)
            if w is not None:
                deps.append(w)
            deps.extend(self.readers.get(k, ()))
        op.deps = deps
        for k in writes:
            self.last_writer[k] = op
            self.readers[k] = []
        for k in reads:
            if k not in writes:
                self.readers.setdefault(k, []).append(op)
        if is_dma:
            self.dmas_live.append(op)
        self.ops[eng].append(op)
        return op

    def op(self, eng, fn, reads=(), writes=()):
        return self._add(eng, fn, tuple(reads), tuple(writes), False)

    def dma(self, eng, out, in_, reads=(), writes=()):
        return self._add(eng, lambda e: e.dma_start(out=out, in_=in_), tuple(reads), tuple(writes), True)

    def barrier(self):
        lasts = [self.ops[e][-1] for e in ENGS if self.ops[e]]
        deps = lasts + self.dmas_live
        for e in ENGS:
            self._add(e, lambda eng: eng.nop(), (), (), False, extra=deps)
        self.last_writer.clear()
        self.readers.clear()
        self.dmas_live = []

    def resolve(self):
        R, K = self.n_rot, self.n_dma_sems
        for e in ENGS:
            dmas = [o for o in self.ops[e] if o.is_dma]
            for i, o in enumerate(dmas):
                o.sem = ("dma", e, i % K)
                o.target = 16 * (i // K + 1)
                o.signal = True
                o.dma_prev = dmas[i - K] if i >= K else None
        for e in ENGS:
            seen_eng = {x: -1 for x in ENGS}
            seen_dma = {}
            for o in self.ops[e]:
                deps = o.deps
                if o.is_dma and o.dma_prev is not None:
                    deps = deps + [o.dma_prev]
                best = {}
                for d in deps:
                    if d.is_dma:
                        if seen_dma.get(d.sem, 0) >= d.target:
                            continue
                        seen_dma[d.sem] = d.target
                        best[d.sem] = d
                    else:
                        if d.eng == e and e not in SELF_SYNC:
                            continue
                        if seen_eng[d.eng] >= d.idx:
                            continue
                        seen_eng[d.eng] = d.idx
                        best[("e", d.eng)] = d
                o.waits = list(best.values())
                for d in o.waits:
                    d.signal = True
                o.deps = None
        for e in ENGS:
            s = 0
            for o in self.ops[e]:
                if o.is_dma:
                    continue
                if o.signal:
                    o.sem = ("cmp", e, s % R)
                    o.target = s // R + 1
                    s += 1

    def emit(self):
        nc = self.nc
        self.resolve()
        names = set()
        for e in ENGS:
            for o in self.ops[e]:
                if o.signal:
                    names.add(o.sem)
        with ExitStack() as st:
            sems = {n: st.enter_context(nc.semaphore("_".join(map(str, n)))) for n in sorted(names)}
            block = st.enter_context(nc.Block())

            def run(e):
                def body(eng):
                    for o in self.ops[e]:
                        ws = o.waits
                        attach = None
                        if ws and not o.is_dma:
                            attach = ws[-1]
                            ws = ws[:-1]
                        for d in ws:
                            eng.wait_ge(sems[d.sem], d.target)
                        ins = o.fn(eng)
                        if attach is not None:
                            ins._wait_ge(sems[attach.sem], attach.target)
                        if o.signal:
                            ins.then_inc(sems[o.sem], 16 if o.is_dma else 1)
                return body

            block.tensor(run("pe"))
            block.scalar(run("act"))
            block.vector(run("dve"))
            block.gpsimd(run("pool"))
            block.sync(run("sp"))


class Arena:
    def __init__(self, ap, nwords):
        self.ap = ap
        self.n = nwords
        self.off = 0
        self.base = 0

    def _take(self, words):
        words = (words + 7) // 8 * 8
        a = self.off
        self.off += words
        assert self.off <= self.n, f"arena overflow {self.off} > {self.n}"
        return self.ap[:, a:a + words], words

    @staticmethod
    def _shape(v, free):
        if len(free) == 1:
            return v
        if len(free) == 2:
            return v.rearrange("p (a b) -> p a b", a=free[0])
        if len(free) == 3:
            return v.rearrange("p (a b c) -> p a b c", a=free[0], b=free[1])
        raise ValueError

    def f32(self, *free):
        n = int(np.prod(free))
        v, w = self._take(n)
        return self._shape(v[:, 0:n], free)

    def bf(self, *free):
        n = int(np.prod(free))
        v, w = self._take((n + 1) // 2)
        return self._shape(v.bitcast(BF16)[:, 0:n], free)

    def mark(self):
        self.base = self.off

    def reset(self):
        self.off = self.base


def build_program(debug=False, nphases=10**9):
    nc = bass.Bass("TRN2", target_bir_lowering=False)

    def din(name, shape, dt=F32):
        return nc.dram_tensor(name, list(shape), dt, kind="ExternalInput").ap()

    def dscr(name, shape, dt):
        if debug:
            return nc.dram_tensor(name, list(shape), dt, kind="ExternalOutput").ap()
        return nc.dram_tensor(name, list(shape), dt).ap()

    x_in = din("x", [2, SEQ, D])
    ctx_in = din("ctx", [2, NCTX, D])
    cT_d = din("cT", [128, 8, 3])
    ident_d = din("ident", [128, 128])
    blk_d = din("blk", [128, 128])
    ropeC_d = din("ropeC", [128, T])
    ropeS_d = din("ropeS", [128, T])
    L = [dict(), dict()]
    for l in range(2):
        L[l]["ada_w"] = din(f"l{l}_ada_w", [D, 6 * D])
        L[l]["ada_bT"] = din(f"l{l}_ada_bT", [128, 48])
        L[l]["ada_b"] = din(f"l{l}_ada_b", [1, 6 * D])
        L[l]["w_out"] = din(f"l{l}_w_out", [D, D])
        L[l]["ln"] = din(f"l{l}_ln", [4, D])
    L[0]["wfm"] = din("l0_wfm", [D, 2048])
    L[0]["wfmp"] = din("l0_wfmp", [D, 1024])
    L[0]["wv"] = din("l0_wv", [D, 1024])
    nabias_d = din("l0_nabias", [5, 128, 8, 832])
    lq_d = din("l0_lq", [1, 256])
    subg_d = din("l0_subg", [1, 128])
    subgc_d = din("l0_subgc", [128, 1])
    w13_0 = din("l0_w13", [D, 2 * FFN])
    w2_0 = din("l0_w2", [FFN, D])
    L[1]["wfm"] = din("l1_wfm", [D, 1536])
    L[1]["wfmp"] = din("l1_wfmp", [D, 1536])
    L[1]["wv"] = din("l1_wv", [D, 256])
    gcols_d = din("l1_gcols", [128, 4])
    grow_d = din("l1_grow", [1, 128])
    router_d = din("l1_router", [D, NEXP])
    w13_1 = din("l1_w13", [NEXP, D, 2 * EDIM])
    w2_1 = din("l1_w2", [NEXP, EDIM, D])
    out_d = nc.dram_tensor("out", [2, SEQ, D], F32, kind="ExternalOutput").ap()

    qkT = [dscr("qkT0", [2, 16, 128, T], BF16), dscr("qkT1", [2, 12, 128, T], BF16)]
    vsc = [dscr("v0", [2, T, 1024], BF16), dscr("v1", [2, T, 256], BF16)]
    att = dscr("att", [2, T, D], BF16)
    attT = dscr("attT", [2, 8, 128, T], BF16)
    X1 = dscr("X1", [2, T, D], F32)
    X2 = dscr("X2", [2, T, D], F32)
    X3 = dscr("X3", [2, SEQ, D], F32)
    gates_d = dscr("gates", [2, 2, 3, 128, D], F32)

    P = Prog(nc)
    out_keys = []

    with ExitStack() as st:
        NW = 52000
        arena_t = st.enter_context(nc.sbuf_tensor("arena", [128, NW], F32))
        psa = st.enter_context(nc.psum_tensor("psa", [128, 4096], F32))
        A = Arena(arena_t, NW)

        def bank(i, n=512):
            return psa[:, 512 * i:512 * i + n]

        ident = A.f32(128)
        identb = A.bf(128)
        blk = A.f32(128)
        ones = A.f32(128)
        cst = A.f32(8)
        modsT = [A.f32(48, 3), A.f32(48, 3)]
        opsc = [[A.f32(8, 3), A.f32(8, 3)], [A.f32(8, 3), A.f32(8, 3)]]
        lamt = A.f32(8)
        subg = A.f32(128)
        gcols = A.f32(4)
        A.mark()

        P.dma("sp", ident, ident_d, writes=["ident"])
        P.dma("sp", blk, blk_d, writes=["blk"])
        P.dma("sp", gcols, gcols_d, writes=["gcols"])
        P.op("dve", lambda e: e.tensor_copy(identb, ident), reads=["ident"], writes=["identb"])
        P.op("dve", lambda e: e.memset(ones, 1.0), writes=["ones"])
        P.op("dve", lambda e: e.memset(cst[:, 0:1], LN_EPS), writes=["cst0"])
        P.op("dve", lambda e: e.memset(cst[:, 1:2], RMS_EPS), writes=["cst1"])
        P.op("dve", lambda e: e.memset(cst[:, 2:3], 0.0), writes=["cst2"])
        lqb = A.f32(256)
        lqp = A.f32(128)
        P.dma("sp", lqb, lq_d.partition_broadcast(128), writes=["lqb"])
        P.dma("sp", subg, subg_d.partition_broadcast(128), writes=["subg"])
        P.op("dve", lambda e: e.tensor_scalar(subg, subg, 1.0 - LAM_INIT0, None, ALU.mult), reads=["subg"], writes=["subg"])
        P.op("dve", lambda e: e.tensor_tensor(lqp[:, 0:64], lqb[:, 0:64], lqb[:, 64:128], ALU.mult), reads=["lqb"], writes=["lqp0"])
        P.op("dve", lambda e: e.tensor_tensor(lqp[:, 64:128], lqb[:, 128:192], lqb[:, 192:256], ALU.mult), reads=["lqb"], writes=["lqp1"])
        P.op("dve", lambda e: e.reduce_sum(lamt[:, 2:3], lqp[:, 0:64], axis=AX.X), reads=["lqp0"], writes=["lam2"])
        P.op("dve", lambda e: e.reduce_sum(lamt[:, 3:4], lqp[:, 64:128], axis=AX.X), reads=["lqp1"], writes=["lam3"])
        P.op("act", lambda e: e.activation(lamt[:, 4:6], lamt[:, 2:4], AF.Exp), reads=["lam2", "lam3"], writes=["lam45"])
        P.op("dve", lambda e: e.tensor_tensor(lamt[:, 0:1], lamt[:, 4:5], lamt[:, 5:6], ALU.subtract), reads=["lam45"], writes=["lam0a"])
        P.op("dve", lambda e: e.tensor_scalar(lamt[:, 0:1], lamt[:, 0:1], LAM_INIT0, None, ALU.add), reads=["lam0a"], writes=["lam0"])
        P.barrier()
        A.reset()

        def phase_mods(l):
            A.reset()
            cT = A.f32(8, 3)
            scT = A.f32(8, 3)
            abT = A.f32(48)
            wch = [A.f32(8, 1024), A.f32(8, 1024)]
            bbc = [A.f32(512), A.f32(512)]
            gst = [A.f32(512), A.f32(512)]
            pm = bank(0, 144)
            P.dma("sp", cT, cT_d, writes=["cT"])
            P.dma("sp", abT, L[l]["ada_bT"], writes=["abT"])
            P.op("act", lambda e: e.activation(scT, cT, AF.Silu), reads=["cT"], writes=["scT"])
            gi = 0
            for ty in range(6):
                wb = wch[ty % 2]
                wk = f"wch{ty % 2}"
                P.dma("sp", wb, L[l]["ada_w"][:, ty * 1024:(ty + 1) * 1024].rearrange("(k p) n -> p k n", p=128), writes=[wk])
                for jc in range(8):
                    j = ty * 8 + jc
                    for k in range(8):
                        P.op("pe", lambda e, wb=wb, jc=jc, k=k, j=j: e.matmul(pm[:, 3 * j:3 * j + 3], wb[:, k, jc * 128:(jc + 1) * 128], scT[:, k, :],
                                                                       start=(k == 0), stop=(k == 7)),
                             reads=[wk, "scT"], writes=["ps0"])
            pm3 = pm.rearrange("p (j m) -> p j m", m=3)
            for m in range(3):
                P.op("dve", lambda e, m=m: e.tensor_tensor(modsT[l][:, :, m], pm3[:, :, m], abT, ALU.add), reads=["ps0", "abT"], writes=[f"modsT{m}"])
            Rg = [A.f32(1024), A.f32(1024)]
            gi = 0
            for gsel, ty in ((0, 2), (1, 5)):
                for m in range(3):
                    R = Rg[gi % 2]
                    for k in range(8):
                        P.op("dve", lambda e, R=R, k=k, ty=ty, m=m: e.tensor_scalar(R[:, k * 128:(k + 1) * 128], ident, modsT[l][:, ty * 8 + k, m:m + 1], None, ALU.mult),
                             reads=[f"modsT{m}", "ident"], writes=[f"Rg{gi % 2}"])
                    for half in range(2):
                        pb = bank(1 + half)
                        P.op("pe", lambda e, pb=pb, R=R, half=half: e.matmul(pb, ones, R[:, half * 512:(half + 1) * 512], start=True, stop=True),
                             reads=[f"Rg{gi % 2}", "ones"], writes=[f"ps{1 + half}"])
                        gs = gst[half]
                        P.op("act", lambda e, gs=gs, pb=pb: e.copy(gs, pb), reads=[f"ps{1 + half}"], writes=[f"gst{half}"])
                        P.dma("sp", gates_d[l, gsel, m, :, half * 512:(half + 1) * 512], gs, reads=[f"gst{half}"])
                    gi += 1
            P.op("dve", lambda e: e.tensor_scalar(opsc[l][0], modsT[l][:, 8:16, :], 1.0, None, ALU.add), reads=["modsT0", "modsT1", "modsT2"], writes=["opsc0"])
            P.op("dve", lambda e: e.tensor_scalar(opsc[l][1], modsT[l][:, 32:40, :], 1.0, None, ALU.add), reads=["modsT0", "modsT1", "modsT2"], writes=["opsc1"])
            P.barrier()

        def tiles_of(Xsrc, b, nlat=16, nctx=2, l0=False):
            tl = []
            for t in range(nlat):
                if l0:
                    tl.append((x_in[b, t * 128:(t + 1) * 128, :], b))
                else:
                    tl.append((Xsrc[b, t * 128:(t + 1) * 128, :], b))
            for t in range(nctx):
                if l0:
                    tl.append((ctx_in[b, t * 128:(t + 1) * 128, :], 2))
                else:
                    tl.append((Xsrc[b, SEQ + t * 128:SEQ + (t + 1) * 128, :], 2))
            return tl

        def emit_hT(l, sub, xt, xk, m, hT, hk, col0, pbanks, pkeys, cnt, hTf=None, hfk=None):
            shift_base = 0 if sub == 0 else 24
            for half in range(2):
                pb = pbanks[cnt[0] % 2]
                pk = pkeys[cnt[0] % 2]
                cnt[0] += 1
                for kk in range(4):
                    k = half * 4 + kk
                    P.op("pe", lambda e, pb=pb, kk=kk, k=k: e.transpose(pb[:, kk * 128:(kk + 1) * 128], xt[:, k * 128:(k + 1) * 128], ident),
                         reads=[xk, "ident"], writes=[pk])
                for kk in range(4):
                    k = half * 4 + kk
                    sc = opsc[l][sub][:, k, m:m + 1]
                    sh = modsT[l][:, shift_base + k, m:m + 1]
                    if hTf is not None:
                        P.op("act", lambda e, pb=pb, kk=kk, k=k, sc=sc, sh=sh: e.activation(hTf[:, k, :], pb[:, kk * 128:(kk + 1) * 128], AF.Identity, bias=sh, scale=sc),
                             reads=[pk], writes=[hfk])
                    elif pk == pkeys[0]:
                        P.op("act", lambda e, pb=pb, kk=kk, k=k, sc=sc, sh=sh: e.activation(hT[:, k, col0:col0 + 128], pb[:, kk * 128:(kk + 1) * 128], AF.Identity, bias=sh, scale=sc),
                             reads=[pk], writes=[hk])
                    else:
                        P.op("dve", lambda e, pb=pb, kk=kk, k=k, sc=sc, sh=sh: e.tensor_scalar(hT[:, k, col0:col0 + 128], pb[:, kk * 128:(kk + 1) * 128], sc, sh, ALU.mult, ALU.add),
                             reads=[pk], writes=[hk])
            if hTf is not None:
                P.op("dve", lambda e: e.tensor_copy(hT[:, :, col0:col0 + 128], hTf), reads=[hfk], writes=[hk])

        def phase_proj(l, b, Xsrc):
            A.reset()
            nfm = 16 if l == 0 else 12
            nperm = 8 if l == 0 else 12
            perm_first = 8 if l == 0 else 0
            nv = 1024 if l == 0 else 256
            wfm = A.bf(8, nfm * 128)
            wfmp = A.bf(8, nperm * 128)
            wv = A.bf(8, nv)
            xt = [A.f32(4, 1024), A.f32(4, 1024)]
            hT = [A.bf(8, 512), A.bf(8, 512)]
            if l == 0:
                tabs = [(A.f32(T), A.f32(T))]
            else:
                tabs = [(A.f32(T), A.f32(T)), (A.f32(T), A.f32(T))]
            stg = [A.bf(512) for _ in range(4)]
            t1 = [A.f32(512), A.f32(512)]
            t2 = [A.f32(512), A.f32(512)]
            sq = [A.f32(512), A.f32(512)]
            rs = [A.f32(512), A.f32(512)]
            wl = [("wfm", wfm, c0) for c0 in range(0, nfm * 128, 512)]
            pl_ = [("wfmp", wfmp, c0) for c0 in range(0, nperm * 128, 512)]
            if l == 1:
                order = [x for pair in zip(wl, pl_) for x in pair]
            else:
                order = wl + pl_
            for nm, wt, c0 in order:
                P.dma("pool", wt[:, :, c0:c0 + 512], L[l][nm][:, c0:c0 + 512].rearrange("(k p) n -> p k n", p=128), writes=[nm + str(c0)])
            for c0 in range(0, nv, 512):
                n = min(512, nv - c0)
                P.dma("pool", wv[:, :, c0:c0 + n], L[l]["wv"][:, c0:c0 + n].rearrange("(k p) n -> p k n", p=128), writes=["wv"])
            for ti, (tc, ts) in enumerate(tabs):
                P.dma("sp", tc, ropeC_d, writes=[f"tc{ti}"])
                P.dma("sp", ts, ropeS_d, writes=[f"ts{ti}"])
                if l == 1:
                    P.op("dve", lambda e, tc=tc, ti=ti: e.tensor_scalar(tc, tc, gcols[:, 2 * ti:2 * ti + 1], None, ALU.mult), reads=[f"tc{ti}", "gcols"], writes=[f"tc{ti}"])
                    P.op("dve", lambda e, ts=ts, ti=ti: e.tensor_scalar(ts, ts, gcols[:, 2 * ti + 1:2 * ti + 2], None, ALU.mult), reads=[f"ts{ti}", "gcols"], writes=[f"ts{ti}"])
            tl = tiles_of(Xsrc, b, l0=(l == 0))
            groups = [tl[0:4], tl[4:8], tl[8:12], tl[12:16], tl[16:18]]
            cnt = [0]
            si = 0
            ci = 0
            vi = 0
            tails = []
            def load_grp(g):
                for ti, (src, m) in enumerate(groups[g]):
                    P.dma("sp", xt[g % 2][:, ti, :], src, writes=[f"xt{g % 2}_{ti}"])
            def build_hT(g):
                gb_ = g % 2
                for ti, (src, m) in enumerate(groups[g]):
                    emit_hT(l, 0, xt[gb_][:, ti, :], f"xt{gb_}_{ti}", m, hT[gb_], f"hT{gb_}", ti * 128, [bank(0), bank(1)], ["ps0", "ps1"], cnt)

            load_grp(0)
            build_hT(0)
            load_grp(1)
            for g, grp in enumerate(groups):
                gb = g % 2
                ntok = 128 * len(grp)
                tok0 = g * 512
                for c in range(nfm):
                    if c == nfm // 2:
                        if g + 1 < len(groups):
                            build_hT(g + 1)
                        if g + 2 < len(groups):
                            load_grp(g + 2)
                    pa = bank(2 + (ci % 2) * 2)
                    pka = f"ps{2 + (ci % 2) * 2}"
                    pbk = bank(3 + (ci % 2) * 2)
                    pkb = f"ps{3 + (ci % 2) * 2}"
                    roped = c >= perm_first
                    for k in range(8):
                        P.op("pe", lambda e, pa=pa, k=k, c=c, gb=gb, ntok=ntok: e.matmul(pa[:, 0:ntok], wfm[:, k, c * 128:(c + 1) * 128], hT[gb][:, k, 0:ntok], start=(k == 0), stop=(k == 7)),
                             reads=["wfm" + str((c * 128) // 512 * 512), f"hT{gb}"], writes=[pka])
                    sg = stg[si % 4]
                    sk = f"stg{si % 4}"
                    si += 1
                    if not roped:
                        if c % 2 == 0:
                            P.op("act", lambda e, sg=sg, pa=pa, ntok=ntok: e.copy(sg[:, 0:ntok], pa[:, 0:ntok]), reads=[pka], writes=[sk])
                        else:
                            P.op("dve", lambda e, sg=sg, pa=pa, ntok=ntok: e.tensor_copy(sg[:, 0:ntok], pa[:, 0:ntok]), reads=[pka], writes=[sk])
                    else:
                        cp = c - perm_first
                        for k in range(8):
                            P.op("pe", lambda e, pbk=pbk, k=k, cp=cp, gb=gb, ntok=ntok: e.matmul(pbk[:, 0:ntok], wfmp[:, k, cp * 128:(cp + 1) * 128], hT[gb][:, k, 0:ntok], start=(k == 0), stop=(k == 7)),
                                 reads=["wfmp" + str((cp * 128) // 512 * 512), f"hT{gb}"], writes=[pkb])
                        tsel = 0 if (l == 0 or c < 8) else 1
                        tc, ts = tabs[tsel]
                        u = ci % 2
                        a1, a2 = t1[u], t2[u]
                        P.op("dve", lambda e, a1=a1, pa=pa, tc=tc, ntok=ntok, tok0=tok0: e.tensor_tensor(a1[:, 0:ntok], pa[:, 0:ntok], tc[:, tok0:tok0 + ntok], ALU.mult),
                             reads=[pka, f"tc{tsel}"], writes=[f"t1{u}"])
                        P.op("dve", lambda e, a2=a2, pbk=pbk, ts=ts, ntok=ntok, tok0=tok0: e.tensor_tensor(a2[:, 0:ntok], pbk[:, 0:ntok], ts[:, tok0:tok0 + ntok], ALU.mult),
                             reads=[pkb, f"ts{tsel}"], writes=[f"t2{u}"])
                        if l == 0:
                            P.op("pool", lambda e, sg=sg, a1=a1, a2=a2, ntok=ntok: e.tensor_tensor(sg[:, 0:ntok], a1[:, 0:ntok], a2[:, 0:ntok], ALU.add),
                                 reads=[f"t1{u}", f"t2{u}"], writes=[sk])
                        else:
                            s_q, r_s = sq[u], rs[u]
                            pc = bank(6)
                            P.op("act", lambda e, s_q=s_q, pa=pa, ntok=ntok: e.activation(s_q[:, 0:ntok], pa[:, 0:ntok], AF.Square), reads=[pka], writes=[f"sq{u}"])
                            P.op("dve", lambda e, a1=a1, a2=a2, ntok=ntok: e.tensor_tensor(a1[:, 0:ntok], a1[:, 0:ntok], a2[:, 0:ntok], ALU.add),
                                 reads=[f"t1{u}", f"t2{u}"], writes=[f"t1{u}"])

                            def tail(pc=pc, s_q=s_q, r_s=r_s, a1=a1, sg=sg, sk=sk, u=u, ntok=ntok, c=c, tok0=tok0):
                                P.op("pe", lambda e: e.matmul(pc[:, 0:ntok], blk, s_q[:, 0:ntok], start=True, stop=True),
                                     reads=[f"sq{u}", "blk"], writes=["ps6"])
                                P.op("act", lambda e: e.activation(r_s[:, 0:ntok], pc[:, 0:ntok], AF.Ln, bias=cst[:, 1:2], scale=1.0),
                                     reads=["ps6", "cst1"], writes=[f"rs{u}"])
                                P.op("act", lambda e: e.activation(r_s[:, 0:ntok], r_s[:, 0:ntok], AF.Exp, bias=cst[:, 2:3], scale=-0.5), reads=[f"rs{u}"], writes=[f"rs{u}"])
                                P.op("pool", lambda e: e.tensor_tensor(sg[:, 0:ntok], a1[:, 0:ntok], r_s[:, 0:ntok], ALU.mult),
                                     reads=[f"t1{u}", f"rs{u}"], writes=[sk])
                                P.dma("sp", qkT[l][b, c, :, tok0:tok0 + ntok], sg[:, 0:ntok], reads=[sk])
                            if tails:
                                tails.pop(0)()
                            tails.append(tail)
                            ci += 1
                            continue
                    P.dma("sp", qkT[l][b, c, :, tok0:tok0 + ntok], sg[:, 0:ntok], reads=[sk])
                    ci += 1
                while tails:
                    tails.pop(0)()
                for ti, (src, m) in enumerate(grp):
                    row0 = tok0 + ti * 128
                    for c0 in range(0, nv, 512):
                        n = min(512, nv - c0)
                        pv = bank(7)
                        for k in range(8):
                            P.op("pe", lambda e, pv=pv, k=k, gb=gb, ti=ti, c0=c0, n=n: e.matmul(pv[:, 0:n], hT[gb][:, k, ti * 128:(ti + 1) * 128], wv[:, k, c0:c0 + n], start=(k == 0), stop=(k == 7)),
                                 reads=["wv", f"hT{gb}"], writes=["ps7"])
                        sg = stg[si % 4]
                        sk = f"stg{si % 4}"
                        si += 1
                        if vi % 2 == 0:
                            P.op("act", lambda e, sg=sg, pv=pv, n=n: e.copy(sg[:, 0:n], pv[:, 0:n]), reads=["ps7"], writes=[sk])
                        else:
                            P.op("dve", lambda e, sg=sg, pv=pv, n=n: e.tensor_copy(sg[:, 0:n], pv[:, 0:n]), reads=["ps7"], writes=[sk])
                        vi += 1
                        P.dma("sp", vsc[l][b, row0:row0 + 128, c0:c0 + n], sg[:, 0:n], reads=[sk])
            P.barrier()

        PTv = psa[:, 2560:3712].bitcast(BF16)
        O_ps = [psa[:, 3840:3968], psa[:, 3968:4096]]

        def pt_keys(c0, c1):
            ks = set()
            for c in range(c0, c1):
                ks.add("ps5" if c < 8 else ("ps6" if c < 16 else "ps7"))
            return sorted(ks)

        def softmax_pv(S_ap, s_keys, nk, scale, chunks, Pb, pkey, PT, ptkey, st, stkey, o_idx, uid):
            P.op("dve", lambda e: e.reduce_max(st[:, 0:1], S_ap, axis=AX.X), reads=s_keys, writes=[stkey + "m"])
            P.op("dve", lambda e: e.tensor_scalar(st[:, 1:2], st[:, 0:1], -scale, None, ALU.mult), reads=[stkey + "m"], writes=[stkey + "n"])
            P.op("act", lambda e: e.activation(Pb[:, 0:nk], S_ap, AF.Exp, bias=st[:, 1:2], scale=scale, accum_out=st[:, 2:3]),
                 reads=list(s_keys) + [stkey + "n"], writes=[pkey, stkey + "s"])
            nch = len(chunks)
            for j, (c0, w, v_ap, vk) in enumerate(chunks):
                P.op("pe", lambda e, j=j, c0=c0, w=w: e.transpose(PTv[0:w, j * 128:(j + 1) * 128], Pb[:, c0:c0 + w], identb),
                     reads=[pkey, "identb"], writes=pt_keys(j, j + 1))
            segs = [(0, min(nch, 8)), (8, min(nch, 16)), (16, nch)]
            for si, (a, bnd) in enumerate(segs):
                if bnd <= a:
                    continue
                eng = "act" if (uid + si) % 2 == 0 else "dve"
                if eng == "act":
                    P.op("act", lambda e, a=a, bnd=bnd: e.copy(PT[:, a * 128:bnd * 128], PTv[:, a * 128:bnd * 128]), reads=pt_keys(a, bnd), writes=[ptkey + str(si)])
                else:
                    P.op("dve", lambda e, a=a, bnd=bnd: e.tensor_copy(PT[:, a * 128:bnd * 128], PTv[:, a * 128:bnd * 128]), reads=pt_keys(a, bnd), writes=[ptkey + str(si)])
            dv = chunks[0][2].shape[-1]
            Oa = O_ps[o_idx][:, 0:dv]
            for j, (c0, w, v_ap, vk) in enumerate(chunks):
                P.op("pe", lambda e, j=j, w=w, v_ap=v_ap: e.matmul(Oa, PT[0:w, j * 128:(j + 1) * 128], v_ap, start=(j == 0), stop=(j == nch - 1)),
                     reads=[ptkey + str(j // 8), vk], writes=["ps7"])
            P.op("dve", lambda e: e.reciprocal(st[:, 2:3], st[:, 2:3]), reads=[stkey + "s"], writes=[stkey + "s"])
            return Oa

        def phase_na(b):
            A.reset()
            kctx = A.bf(4, 256)
            vctx = A.bf(2, 512)
            bias = A.f32(8, 832)
            qz = [[A.bf(4, 128), A.bf(4, 128)] for _ in range(2)]
            kb = [A.bf(4, 576), A.bf(4, 576)]
            vb = [A.bf(5, 512), A.bf(5, 512)]
            Tt = [A.f32(832) for _ in range(3)]
            Pb = [A.bf(896) for _ in range(3)]
            PT = [A.bf(7 * 128), A.bf(7 * 128)]
            sts = [A.f32(8) for _ in range(6)]
            onat = [A.bf(512), A.bf(512)]
            PTp = [bank(5).bitcast(BF16), bank(6).bitcast(BF16)]
            Obk = [(bank(7), "ps7"), (bank(4), "ps4")]
            for u in range(2):
                P.op("pool", lambda e, u=u: e.memset(qz[u][0][64:128], 0.0), writes=[f"qzz{u}0"])
                P.op("pool", lambda e, u=u: e.memset(qz[u][1][0:64], 0.0), writes=[f"qzz{u}1"])
                P.op("pool", lambda e, u=u: e.memset(vb[u][64:128, 4, :], 0.0), writes=[f"vbz{u}"])
            P.dma("sp", kctx, qkT[0][b, 4:8, :, SEQ:T].rearrange("c p t -> p c t"), writes=["kctx"])
            P.dma("sp", vctx, vsc[0][b, SEQ:T, 0:512].rearrange("(j p) c -> p j c", p=128), writes=["vctx"])

            def load_tile(i):
                u = i % 2
                P.dma("sp", qz[u][0][0:64], qkT[0][b, 0:4, 0:64, i * 128:(i + 1) * 128].rearrange("c p t -> p c t"), writes=[f"qz{u}0"])
                P.dma("sp", qz[u][1][64:128], qkT[0][b, 0:4, 64:128, i * 128:(i + 1) * 128].rearrange("c p t -> p c t"), writes=[f"qz{u}1"])
                if i < 16:
                    K0 = min(max(2 * i - 4, 0), 23)
                    r0 = K0 * 64
                    P.dma("sp", kb[u], qkT[0][b, 4:8, :, r0:r0 + 576].rearrange("c p t -> p c t"), writes=[f"kb{u}"])
                    P.dma("sp", vb[u][:, 0:4, :], vsc[0][b, r0:r0 + 512, 0:512].rearrange("(j p) c -> p j c", p=128), writes=[f"vb{u}"])
                    P.dma("sp", vb[u][0:64, 4, :], vsc[0][b, r0 + 512:r0 + 576, 0:512], writes=[f"vb4{u}"])

            units = [(i, h) for i in range(18) for h in range(8)]
            pid_of = lambda i: {0: 0, 1: 1, 14: 3, 15: 4}.get(i, 2)
            state = {"pid": -1}

            def stage1(n):
                i, h = units[n]
                u = i % 2
                cb, var = h // 2, h % 2
                if h == 2 and i + 1 < 18:
                    load_tile(i + 1)
                if h == 0:
                    if i < 16 and pid_of(i) != state["pid"]:
                        for hh in range(8):
                            P.dma("sp", bias[:, hh, :], nabias_d[pid_of(i), :, hh, :], writes=[f"bias{hh}"])
                        state["pid"] = pid_of(i)
                su = n % 2
                sbank = psa[:, su * 1024:su * 1024 + 832]
                skeys = [f"ps{2 * su}", f"ps{2 * su + 1}"]
                st_ = sts[n % 6]
                stk = f"st{n % 6}"
                qap = qz[u][var][:, cb, :]
                qk = [f"qz{u}{var}", f"qzz{u}{var}"]
                pb = Pb[n % 3]
                if i < 16:
                    for (c0, w) in ((0, 512), (512, 64)):
                        P.op("pe", lambda e, sbank=sbank, qap=qap, u=u, cb=cb, c0=c0, w=w: e.matmul(sbank[:, c0:c0 + w], qap, kb[u][:, cb, c0:c0 + w], start=True, stop=True),
                             reads=qk + [f"kb{u}"], writes=skeys)
                    P.op("pe", lambda e, sbank=sbank, qap=qap, cb=cb: e.matmul(sbank[:, 576:832], qap, kctx[:, cb, :], start=True, stop=True),
                         reads=qk + ["kctx"], writes=skeys)
                    Tb = Tt[n % 3]
                    tk = f"T{n % 3}"
                    P.op("dve", lambda e, Tb=Tb, sbank=sbank, h=h: e.scalar_tensor_tensor(Tb, sbank, 0.125, bias[:, h, :], ALU.mult, ALU.add),
                         reads=skeys + [f"bias{h}"], writes=[tk])
                    src, srck, nk, scale = Tb, [tk], 832, 1.0
                else:
                    P.op("pe", lambda e, sbank=sbank, qap=qap, cb=cb: e.matmul(sbank[:, 0:256], qap, kctx[:, cb, :], start=True, stop=True),
                         reads=qk + ["kctx"], writes=skeys)
                    src, srck, nk, scale = sbank[:, 0:256], skeys, 256, 0.125
                P.op("dve", lambda e, st_=st_, src=src: e.reduce_max(st_[:, 0:1], src, axis=AX.X), reads=srck, writes=[stk + "m"])
                P.op("dve", lambda e, st_=st_, scale=scale: e.tensor_scalar(st_[:, 1:2], st_[:, 0:1], -scale, None, ALU.mult), reads=[stk + "m"], writes=[stk + "n"])
                P.op("act", lambda e, pb=pb, src=src, nk=nk, scale=scale, st_=st_: e.activation(pb[:, 0:nk], src, AF.Exp, bias=st_[:, 1:2], scale=scale, accum_out=st_[:, 2:3]),
                     reads=list(srck) + [stk + "n"], writes=[f"P{n % 3}", stk + "s"])

            def stage2(n):
                i, h = units[n]
                u = i % 2
                st_ = sts[n % 6]
                stk = f"st{n % 6}"
                pb = Pb[n % 3]
                ptp = PTp[n % 2]
                ptk = f"ps{5 + n % 2}"
                pts = PT[n % 2]
                ob, obk = Obk[n % 2]
                if i < 16:
                    chunks = [(j * 128, vb[u][:, j, h * 64:(h + 1) * 64], [f"vb{u}"]) for j in range(4)]
                    chunks.append((512, vb[u][:, 4, h * 64:(h + 1) * 64], [f"vb4{u}", f"vbz{u}"]))
                    chunks += [(576 + j * 128, vctx[:, j, h * 64:(h + 1) * 64], ["vctx"]) for j in range(2)]
                else:
                    chunks = [(j * 128, vctx[:, j, h * 64:(h + 1) * 64], ["vctx"]) for j in range(2)]
                nch = len(chunks)
                for j, (c0, v_ap, vk) in enumerate(chunks):
                    P.op("pe", lambda e, j=j, c0=c0, ptp=ptp, pb=pb: e.transpose(ptp[:, j * 128:(j + 1) * 128], pb[:, c0:c0 + 128], identb),
                         reads=[f"P{n % 3}", "identb"], writes=[ptk])
                P.op("dve", lambda e, pts=pts, ptp=ptp, nch=nch: e.tensor_copy(pts[:, 0:nch * 128], ptp[:, 0:nch * 128]), reads=[ptk], writes=[f"PT{n % 2}"])
                Oa = ob[:, 0:64]
                for j, (c0, v_ap, vk) in enumerate(chunks):
                    P.op("pe", lambda e, j=j, Oa=Oa, pts=pts, v_ap=v_ap, nch=nch: e.matmul(Oa, pts[:, j * 128:(j + 1) * 128], v_ap, start=(j == 0), stop=(j == nch - 1)),
                         reads=[f"PT{n % 2}"] + vk, writes=[obk])
                P.op("dve", lambda e, st_=st_: e.reciprocal(st_[:, 2:3], st_[:, 2:3]), reads=[stk + "s"], writes=[stk + "s"])
                P.op("dve", lambda e, Oa=Oa, u=u, h=h, st_=st_: e.tensor_scalar(onat[u][:, h * 64:(h + 1) * 64], Oa, st_[:, 2:3], None, ALU.mult),
                     reads=[obk, stk + "s"], writes=[f"onat{u}"])
                if h == 7:
                    P.dma("sp", att[b, i * 128:(i + 1) * 128, 0:512], onat[u], reads=[f"onat{u}"])

            load_tile(0)
            N = len(units)
            stage1(0)
            stage1(1)
            for n in range(N):
                stage2(n)
                if n + 2 < N:
                    stage1(n + 2)
            P.barrier()

        def phase_full_attn(l, b):
            A.reset()
            dv = 128 if l == 0 else 64
            nsub = 2 if l == 0 else 4
            kT = [A.bf(T), A.bf(T)]
            qTa = [A.bf(2, T) if l == 1 else A.bf(1, T) for _ in range(2)]
            Vt = [A.bf(18, dv + (1 if l == 1 else 0)) for _ in range(2)]
            nqc, nqt = (2, SEQ) if l == 1 else (1, T)
            qz = [[A.bf(nqc, nqt), A.bf(nqc, nqt)] for _ in range(2)]
            Pc = [A.bf(2, 512) for _ in range(3)]
            onesb = A.bf(128)
            onesf = A.f32(128)
            rrow = [A.f32(512), A.f32(512)]
            bcs = [A.f32(512), A.f32(512)]
            outb = [A.bf(512), A.bf(512)]
            negB = [A.f32(8), A.f32(8)]
            kst = A.f32(32)
            if l == 0:
                ksq = A.bf(T)
                qsq = A.bf(T)
                sel = [A.bf(128), A.bf(128)]
                i0 = [A.f32(512), A.f32(512)]
                aa = A.f32(512)
                cc_ = A.f32(512)
                oo = [A.f32(512), A.f32(512)]
                sq = [A.f32(512), A.f32(512)]
                lnv = [A.f32(512), A.f32(512)]
                subgc = A.f32(8)
            else:
                grow = A.f32(128)
            Sb = [bank(0), bank(1), bank(2)]
            Sb2 = [psa[:, 0:1024].rearrange("p (a n) -> p a n", a=2), psa[:, 1024:2048].rearrange("p (a n) -> p a n", a=2)]
            Ob = [bank(4), bank(5)]
            Rb = [bank(6), bank(7)]
            P.op("dve", lambda e: e.memset(onesb, 1.0), writes=["onesb"])
            P.op("dve", lambda e: e.memset(onesf, 1.0), writes=["onesf"])
            for gu_ in range(2):
                P.op("pool", lambda e, gu_=gu_: e.memset(qz[gu_][0][64:128], 0.0), writes=[f"qzz{gu_}0"])
                P.op("pool", lambda e, gu_=gu_: e.memset(qz[gu_][1][0:64], 0.0), writes=[f"qzz{gu_}1"])
            if l == 0:
                for r in range(2):
                    P.op("dve", lambda e, r=r: e.memset(sel[r], 0.0), writes=[f"sel{r}"])
                    P.op("dve", lambda e, r=r: e.memset(sel[r][r * 64:(r + 1) * 64, :], 1.0), writes=[f"sel{r}"])
                P.dma("sp", subgc[:, 0:1], subgc_d, writes=["subgc"])
                P.op("dve", lambda e: e.tensor_scalar(subgc[:, 0:1], subgc[:, 0:1], 1.0 - LAM_INIT0, None, ALU.mult), reads=["subgc"], writes=["subgc"])
                P.op("dve", lambda e: e.tensor_scalar(subgc[:, 1:2], lamt[:, 0:1], -1.0, None, ALU.mult), writes=["neglam"])
            else:
                P.dma("sp", grow, grow_d.partition_broadcast(128), writes=["grow"])
                P.op("dve", lambda e: e.reduce_max(kst[:, 0:1], grow[:, 0:64], axis=AX.X, apply_absolute_value=True), reads=["grow"], writes=["kst0"])
                P.op("dve", lambda e: e.reduce_max(kst[:, 1:2], grow[:, 64:128], axis=AX.X, apply_absolute_value=True), reads=["grow"], writes=["kst1"])
                for gu in range(2):
                    P.op("dve", lambda e, gu=gu: e.scalar_tensor_tensor(negB[gu][:, 0:1], kst[:, 0:1], -8.0, kst[:, 1:2], ALU.mult, ALU.mult), reads=["kst0", "kst1"], writes=[f"negB{gu}_0"])

            def load_g(g):
                gu = g % 2
                if l == 0:
                    P.dma("sp", kT[gu], qkT[0][b, 12 + g], writes=[f"kT{gu}"])
                    P.dma("sp", qTa[gu][:, 0, :], qkT[0][b, 8 + g], writes=[f"qTa{gu}"])
                    P.dma("sp", qz[gu][0][0:64, 0, :], qkT[0][b, 8 + g, 0:64, :], writes=[f"qz{gu}0"])
                    P.dma("sp", qz[gu][1][64:128, 0, :], qkT[0][b, 8 + g, 64:128, :], writes=[f"qz{gu}1"])
                    P.dma("sp", Vt[gu], vsc[0][b, :, 512 + 128 * g:512 + 128 * (g + 1)].rearrange("(j p) c -> p j c", p=128), writes=[f"V{gu}"])
                else:
                    P.dma("sp", kT[gu], qkT[1][b, 8 + g], writes=[f"kT{gu}"])
                    P.dma("sp", qz[gu][0][0:64], qkT[1][b, 2 * g:2 * g + 2, 0:64, 0:SEQ].rearrange("c p t -> p c t"), writes=[f"qz{gu}0"])
                    P.dma("sp", qz[gu][1][64:128], qkT[1][b, 2 * g:2 * g + 2, 64:128, 0:SEQ].rearrange("c p t -> p c t"), writes=[f"qz{gu}1"])
                    P.dma("sp", Vt[gu][:, :, 0:64], vsc[1][b, :, 64 * g:64 * (g + 1)].rearrange("(j p) c -> p j c", p=128), writes=[f"V{gu}"])
                    P.op("pool", lambda e, gu=gu: e.memset(Vt[gu][:, :, 64:65], 1.0), writes=[f"V{gu}"])

            def group_prologue(g):
                gu = g % 2
                if l != 0:
                    return
                P.op("pool", lambda e, gu=gu: e.tensor_tensor(ksq, kT[gu], kT[gu], ALU.mult), reads=[f"kT{gu}"], writes=["ksq"])
                P.op("pool", lambda e, gu=gu: e.tensor_tensor(qsq, qTa[gu][:, 0, :], qTa[gu][:, 0, :], ALU.mult), reads=[f"qTa{gu}"], writes=["qsq"])
                for r in range(2):
                    for si, (src, sk_) in enumerate(((ksq, "ksq"), (qsq, "qsq"))):
                        for cc in range(5):
                            w = min(512, T - cc * 512)
                            bi = (si * 5 + cc) % 3
                            pb = Sb[bi]
                            P.op("pe", lambda e, pb=pb, r=r, cc=cc, w=w, src=src: e.matmul(pb[:, 0:w], sel[r], src[:, cc * 512:cc * 512 + w], start=True, stop=True),
                                 reads=[sk_, f"sel{r}"], writes=[f"ps{bi}"])
                            col = 8 + si * 5 + cc
                            P.op("dve", lambda e, pb=pb, col=col, w=w: e.reduce_max(kst[:, col:col + 1], pb[:, 0:w], axis=AX.X), reads=[f"ps{bi}"], writes=[f"kstc{col}"])
                    P.op("dve", lambda e, r=r: e.reduce_max(kst[:, 4:5], kst[:, 8:13], axis=AX.X), reads=[f"kstc{c}" for c in range(8, 13)], writes=["kstk"])
                    P.op("dve", lambda e, r=r: e.reduce_max(kst[:, 5:6], kst[:, 13:18], axis=AX.X), reads=[f"kstc{c}" for c in range(13, 18)], writes=["kstq"])
                    P.op("dve", lambda e: e.tensor_tensor(kst[:, 6:7], kst[:, 4:5], kst[:, 5:6], ALU.add), reads=["kstk", "kstq"], writes=["kstsum"])
                    P.op("dve", lambda e, r=r, gu=gu: e.tensor_scalar(negB[gu][:, r:r + 1], kst[:, 6:7], -0.0625, None, ALU.mult), reads=["kstsum"], writes=[f"negB{gu}_{r}"])

            units = []
            for g in range(4):
                for G in range(5 if l == 0 else 4):
                    for r in range(nsub):
                        units.append((g, G, r))
            items = []
            for ui, (g, G, r) in enumerate(units):
                kcs = list(range(16, 18)) if G == 4 else list(range(18))
                for ki in range(0, len(kcs), 2):
                    items.append((ui, ki // 2, kcs[ki], len(kcs) // 2))
            n = len(items)
            state = {"oi": 0}
            pending = []

            def qcols(G):
                return (SEQ, NCTX) if G == 4 else (G * 512, 512)

            def stage_S(k):
                ui, ki, kc, nkc = items[k]
                g, G, r = units[ui]
                gu = g % 2
                if ki == 0 and G == 0 and r == 0 and g == 0:
                    group_prologue(0)
                if ki == 0 and G == 1 and r == 0 and g + 1 < 4:
                    load_g(g + 1)
                if ki == 0 and G == 2 and r == 0 and g + 1 < 4:
                    group_prologue(g + 1)
                q0, nq = qcols(G)
                if l == 0:
                    var = r
                    qap = qz[gu][var][:, 0, q0:q0 + nq]
                    nbc = r
                else:
                    var = (4 * g + r) % 2
                    qap = qz[gu][var][:, r // 2, q0:q0 + nq]
                    nbc = 0
                sb = Sb2[k % 2]
                skeys = [f"ps{2 * (k % 2)}", f"ps{2 * (k % 2) + 1}"]
                for a_ in range(2):
                    P.op("pe", lambda e, sb=sb, qap=qap, gu=gu, kc=kc, nq=nq, a_=a_: e.matmul(sb[:, a_, 0:nq], kT[gu][:, (kc + a_) * 128:(kc + a_ + 1) * 128], qap, start=True, stop=True),
                         reads=[f"qz{gu}{var}", f"qzz{gu}{var}", f"kT{gu}"], writes=[skeys[a_]])
                pc = Pc[k % 3]
                P.op("act", lambda e, pc=pc, sb=sb, nq=nq, gu=gu, nbc=nbc: e.activation(pc[:, :, 0:nq], sb[:, :, 0:nq], AF.Exp, bias=negB[gu][:, nbc:nbc + 1], scale=0.125),
                     reads=skeys + [f"negB{gu}_{nbc}"], writes=[f"Pc{k % 3}"])

            def stage_V(k):
                ui, ki, kc, nkc = items[k]
                g, G, r = units[ui]
                gu = g % 2
                q0, nq = qcols(G)
                pc = Pc[k % 3]
                ob = Ob[ui % 2]
                rb = Rb[ui % 2]
                okey, rkey = f"ps{4 + ui % 2}", f"ps{6 + ui % 2}"
                M = dv + (1 if l == 1 else 0)
                for a_ in range(2):
                    P.op("pe", lambda e, ob=ob, pc=pc, gu=gu, kc=kc, nq=nq, M=M, ki=ki, nkc=nkc, a_=a_: e.matmul(ob[0:M, 0:nq], Vt[gu][:, kc + a_, 0:M], pc[:, a_, 0:nq], start=(ki == 0 and a_ == 0), stop=(ki == nkc - 1 and a_ == 1)),
                         reads=[f"Pc{k % 3}", f"V{gu}"], writes=[okey])
                if l == 0:
                    for a_ in range(2):
                        P.op("pe", lambda e, rb=rb, pc=pc, nq=nq, ki=ki, nkc=nkc, a_=a_: e.matmul(rb[:, 0:nq], onesb, pc[:, a_, 0:nq], start=(ki == 0 and a_ == 0), stop=(ki == nkc - 1 and a_ == 1)),
                             reads=[f"Pc{k % 3}", "onesb"], writes=[rkey])
                if ki != nkc - 1:
                    return
                if l == 1:
                    oi = state["oi"]
                    state["oi"] += 1
                    u2 = oi % 2
                    P.op("dve", lambda e, u2=u2, ob=ob, nq=nq: e.reciprocal(rrow[u2][64:65, 0:nq], ob[64:65, 0:nq]), reads=[okey], writes=[f"rrow{u2}"])

                    def part_b(u2=u2, ob=ob, rb=rb, nq=nq, q0=q0, g=g, r=r, okey=okey, rkey=rkey):
                        P.op("pe", lambda e: e.matmul(rb[0:64, 0:nq], onesf[64:65, 0:64], rrow[u2][64:65, 0:nq], start=True, stop=True),
                             reads=[f"rrow{u2}", "onesf"], writes=[rkey])
                        P.op("dve", lambda e: e.tensor_copy(bcs[u2][0:64, 0:nq], rb[0:64, 0:nq]), reads=[rkey], writes=[f"bcs{u2}"])
                        P.op("dve", lambda e: e.tensor_tensor(outb[u2][0:64, 0:nq], ob[0:64, 0:nq], bcs[u2][0:64, 0:nq], ALU.mult), reads=[okey, f"bcs{u2}"], writes=[f"outb{u2}"])
                        hq = 4 * g + r
                        pb0 = (hq % 2) * 64
                        P.dma("sp", attT[b, hq // 2, pb0:pb0 + 64, q0:q0 + nq], outb[u2][0:64, 0:nq], reads=[f"outb{u2}"])
                    pending.append((k + 2, part_b))
                else:
                    if r == 0:
                        P.op("dve", lambda e, rb=rb, nq=nq: e.reciprocal(i0[0][:, 0:nq], rb[:, 0:nq]), reads=[rkey], writes=["i0a"])
                        P.op("dve", lambda e, ob=ob, nq=nq: e.tensor_tensor(aa[:, 0:nq], ob[:, 0:nq], i0[0][:, 0:nq], ALU.mult), reads=[okey, "i0a"], writes=["aa"])
                        return
                    oi = state["oi"]
                    state["oi"] += 1
                    u2 = oi % 2
                    P.op("dve", lambda e, rb=rb, nq=nq: e.reciprocal(i0[1][:, 0:nq], rb[:, 0:nq]), reads=[rkey], writes=["i0b"])
                    P.op("dve", lambda e, ob=ob, nq=nq: e.tensor_tensor(cc_[:, 0:nq], ob[:, 0:nq], i0[1][:, 0:nq], ALU.mult), reads=[okey, "i0b"], writes=["cc"])
                    P.op("dve", lambda e, u2=u2, nq=nq: e.scalar_tensor_tensor(oo[u2][:, 0:nq], cc_[:, 0:nq], subgc[:, 1:2], aa[:, 0:nq], ALU.mult, ALU.add), reads=["cc", "aa", "neglam"], writes=[f"oo{u2}"])
                    P.op("pool", lambda e, u2=u2, nq=nq: e.tensor_tensor(sq[u2][:, 0:nq], oo[u2][:, 0:nq], oo[u2][:, 0:nq], ALU.mult), reads=[f"oo{u2}"], writes=[f"sq{u2}"])

                    def part_b0(u2=u2, nq=nq, rb=rb, rkey=rkey, q0=q0, g=g):
                        P.op("pe", lambda e: e.matmul(rb[:, 0:nq], onesf, sq[u2][:, 0:nq], start=True, stop=True), reads=[f"sq{u2}", "onesf"], writes=[rkey])
                        P.op("act", lambda e: e.activation(lnv[u2][:, 0:nq], rb[:, 0:nq], AF.Ln, bias=cst[:, 1:2], scale=1.0 / 128.0), reads=[rkey], writes=[f"lnv{u2}"])
                        P.op("act", lambda e: e.activation(lnv[u2][:, 0:nq], lnv[u2][:, 0:nq], AF.Exp, bias=cst[:, 2:3], scale=-0.5), reads=[f"lnv{u2}"], writes=[f"lnv{u2}"])
                        P.op("dve", lambda e: e.scalar_tensor_tensor(outb[u2][:, 0:nq], oo[u2][:, 0:nq], subgc[:, 0:1], lnv[u2][:, 0:nq], ALU.mult, ALU.mult), reads=[f"oo{u2}", f"lnv{u2}", "subgc"], writes=[f"outb{u2}"])
                        P.dma("sp", attT[b, 4 + g, :, q0:q0 + nq], outb[u2][:, 0:nq], reads=[f"outb{u2}"])
                    pending.append((k + 2, part_b0))

            load_g(0)
            for k in range(-1, n):
                if k + 1 < n:
                    stage_S(k + 1)
                if k >= 0:
                    stage_V(k)
                while pending and pending[0][0] <= k:
                    pending.pop(0)[1]()
            while pending:
                pending.pop(0)[1]()
            P.barrier()

        def ln_ops(z, zk, o, okey, lng, lnb, st, stk, bn, n_t, nk):
            return [
                lambda: P.op("dve", lambda e: e.bn_stats(bn[:, 0, :], z[:, 0:512]), reads=[zk], writes=[stk + "b0"]),
                lambda: P.op("dve", lambda e: e.bn_stats(bn[:, 1, :], z[:, 512:1024]), reads=[zk], writes=[stk + "b1"]),
                lambda: P.op("dve", lambda e: e.bn_aggr(st[:, 0:2], bn.rearrange("p a b -> p (a b)")), reads=[stk + "b0", stk + "b1"], writes=[stk + "mv"]),
                lambda: P.op("act", lambda e: e.activation(st[:, 2:3], st[:, 1:2], AF.Sqrt, bias=cst[:, 0:1], scale=1.0), reads=[stk + "mv"], writes=[stk + "sd"]),
                lambda: P.op("dve", lambda e: e.reciprocal(st[:, 2:3], st[:, 2:3]), reads=[stk + "sd"], writes=[stk + "sd"]),
                lambda: P.op("dve", lambda e: e.scalar_tensor_tensor(st[:, 3:4], st[:, 0:1], -1.0, st[:, 2:3], ALU.mult, ALU.mult), reads=[stk + "mv", stk + "sd"], writes=[stk + "nb"]),
                lambda: P.op("act", lambda e: e.activation(n_t, z, AF.Identity, bias=st[:, 3:4], scale=st[:, 2:3]), reads=[zk, stk + "nb", stk + "sd"], writes=[nk]),
                lambda: P.op("pool", lambda e: e.tensor_tensor(n_t, n_t, lng, ALU.mult), reads=[nk, "lng"], writes=[nk]),
                lambda: P.op("dve", lambda e: e.tensor_tensor(o, n_t, lnb, ALU.add), reads=[nk, "lnb"], writes=[okey]),
            ]

        def zip_run(lists):
            n_ = max(len(x) for x in lists)
            for j in range(n_):
                for x in lists:
                    if j < len(x):
                        x[j]()

        def phase_outproj(l, b, Xsrc, Xdst, ntl):
            A.reset()
            wo = A.bf(8, 1024)
            gbc = [A.f32(1024), A.f32(1024)]
            lng = A.f32(1024)
            lnb = A.f32(1024)
            at = [A.bf(1024), A.bf(1024)]
            aT = [A.bf(8, 128), A.bf(8, 128)]
            xt = [A.f32(1024), A.f32(1024)]
            z = [A.f32(1024), A.f32(1024)]
            n_t = [A.f32(1024), A.f32(1024)]
            o = [A.f32(1024), A.f32(1024)]
            bn = [A.f32(2, 6), A.f32(2, 6)]
            sts = [A.f32(8), A.f32(8)]
            for c0 in (0, 512):
                P.dma("pool", wo[:, :, c0:c0 + 512], L[l]["w_out"][:, c0:c0 + 512].rearrange("(k p) n -> p k n", p=128), writes=["wo"])
            P.dma("sp", gbc[0], gates_d[l, 0, b], writes=["gbc0"])
            P.dma("sp", gbc[1], gates_d[l, 0, 2], writes=["gbc1"])
            P.dma("sp", lng, L[l]["ln"][0:1, :].partition_broadcast(128), writes=["lng"])
            P.dma("sp", lnb, L[l]["ln"][1:2, :].partition_broadcast(128), writes=["lnb"])
            tl = tiles_of(Xsrc, b, nlat=16, nctx=ntl - 16, l0=(l == 0))
            def load_t(i):
                uu = i % 2
                if l == 0:
                    P.dma("sp", at[uu][:, 0:512], att[b, i * 128:(i + 1) * 128, 0:512], writes=[f"at{uu}"])
                    P.dma("sp", aT[uu][:, 4:8, :], attT[b, 4:8, :, i * 128:(i + 1) * 128].rearrange("c p t -> p c t"), writes=[f"aTd{uu}"])
                else:
                    P.dma("sp", aT[uu], attT[b, :, :, i * 128:(i + 1) * 128].rearrange("c p t -> p c t"), writes=[f"aTd{uu}"])
                P.dma("sp", xt[uu], tl[i][0], writes=[f"xt{uu}"])

            def stage_A(i):
                u = i % 2
                src, m = tl[i]
                akeys = [f"aTd{u}"]
                if l == 0:
                    ptb = bank(u).bitcast(BF16)
                    for k in range(4):
                        P.op("pe", lambda e, ptb=ptb, k=k, u=u: e.transpose(ptb[:, k * 128:(k + 1) * 128], at[u][:, k * 128:(k + 1) * 128], identb),
                             reads=[f"at{u}", "identb"], writes=[f"ps{u}"])
                    P.op("dve", lambda e, u=u, ptb=ptb: e.tensor_copy(aT[u][:, 0:4, :].rearrange("p k t -> p (k t)"), ptb[:, 0:512]), reads=[f"ps{u}"], writes=[f"aT{u}"])
                    akeys.append(f"aT{u}")
                yb = psa[:, 1024 + u * 1024:2048 + u * 1024]
                for half in range(2):
                    for k in range(8):
                        P.op("pe", lambda e, yb=yb, half=half, k=k, u=u: e.matmul(yb[:, half * 512:(half + 1) * 512], aT[u][:, k, :], wo[:, k, half * 512:(half + 1) * 512], start=(k == 0), stop=(k == 7)),
                             reads=akeys + ["wo"], writes=[f"ps{2 + 2 * u + half}"])
                gsel = 0 if m < 2 else 1
                P.op("dve", lambda e, u=u, yb=yb, gsel=gsel: e.tensor_tensor(z[u], yb, gbc[gsel], ALU.mult), reads=[f"ps{2 + 2 * u}", f"ps{3 + 2 * u}", f"gbc{gsel}"], writes=[f"z{u}"])
                P.op("dve", lambda e, u=u: e.scalar_tensor_tensor(z[u], xt[u], ALPHA, z[u], ALU.mult, ALU.add), reads=[f"xt{u}", f"z{u}"], writes=[f"z{u}"])

            def stage_B_ops(i):
                u = i % 2
                return ln_ops(z[u], f"z{u}", o[u], f"o{u}", lng, lnb, sts[u], f"st{u}", bn[u], n_t[u], f"n{u}") + \
                    [lambda: P.dma("sp", Xdst[b, i * 128:(i + 1) * 128, :], o[u], reads=[f"o{u}"])]

            load_t(0)
            load_t(1)
            for i in range(0, len(tl), 2):
                stage_A(i)
                stage_A(i + 1)
                if i + 2 < len(tl):
                    load_t(i + 2)
                    load_t(i + 3)
                zip_run([stage_B_ops(i), stage_B_ops(i + 1)])
            P.barrier()


        def phase_ffn(l, tl, dsts, final):
            A.reset()
            NT = len(tl)
            moe = (l == 1)
            FD = EDIM if moe else FFN
            nexp = NEXP if moe else 1
            hT = A.bf(8, NT * 128)
            yacc = A.f32(NT, 1024)
            G = A.f32(NT, 8)
            wa = [A.bf(8, 512), A.bf(8, 512)]
            wg = [A.bf(8, 512), A.bf(8, 512)]
            w2 = [A.bf(4, 1024), A.bf(4, 1024)]
            mark = A.off
            fchunks = []
            f0 = 0
            while f0 < FD:
                fw = min(512, FD - f0)
                fchunks.append((f0, fw))
                f0 += fw
            wlist = [(ex_i, f0, fw) for ex_i in range(nexp) for (f0, fw) in fchunks]
            loaded = set()

            def load_w(wi_):
                if wi_ in loaded or wi_ >= len(wlist):
                    return
                loaded.add(wi_)
                ex_i, f0, fw = wlist[wi_]
                wu = wi_ % 2
                if moe:
                    w13s, w2s = w13_1[ex_i], w2_1[ex_i]
                else:
                    w13s, w2s = w13_0, w2_0
                P.dma("pool", wa[wu][:, :, 0:fw], w13s[:, f0:f0 + fw].rearrange("(k p) n -> p k n", p=128), writes=[f"wa{wu}"])
                P.dma("pool", wg[wu][:, :, 0:fw], w13s[:, FD + f0:FD + f0 + fw].rearrange("(k p) n -> p k n", p=128), writes=[f"wg{wu}"])
                P.dma("pool", w2[wu][:, 0:fw // 128, :], w2s[f0:f0 + fw, :].rearrange("(j p) n -> p j n", p=128), writes=[f"w2{wu}"])

            load_w(0)
            load_w(1)
            xt = [A.f32(1024), A.f32(1024)]
            hTf = [A.f32(8, 128), A.f32(8, 128)]
            rw = A.f32(8, 8)
            lg = [A.f32(8), A.f32(8)]
            ex = [A.f32(8), A.f32(8)]
            sts = [A.f32(16), A.f32(16)]
            if moe:
                P.dma("sp", rw, router_d.rearrange("(k p) n -> p k n", p=128), writes=["rw"])
            cnt = [0]
            P.dma("sp", xt[0], tl[0][0], writes=["xt0"])
            for i, (src, m) in enumerate(tl):
                u = i % 2
                if i + 1 < NT:
                    P.dma("sp", xt[(i + 1) % 2], tl[i + 1][0], writes=[f"xt{(i + 1) % 2}"])
                if moe:
                    emit_hT(l, 1, xt[u], f"xt{u}", m, hT, "hT", i * 128, [bank(0), bank(1)], ["ps0", "ps1"], cnt, hTf=hTf[u], hfk=f"hTf{u}")
                    pl = bank(2 + u, 8)
                    for k in range(8):
                        P.op("pe", lambda e, pl=pl, k=k, u=u: e.matmul(pl, hTf[u][:, k, :], rw[:, k, :], start=(k == 0), stop=(k == 7)),
                             reads=[f"hTf{u}", "rw"], writes=[f"ps{2 + u}"])
                    s_ = sts[u]
                    sk = f"rst{u}"
                    P.op("dve", lambda e, u=u, pl=pl: e.tensor_copy(lg[u], pl), reads=[f"ps{2 + u}"], writes=[f"lg{u}"])
                    P.op("dve", lambda e, u=u, s_=s_: e.max(s_[:, 0:8], lg[u]), reads=[f"lg{u}"], writes=[sk + "a"])
                    P.op("dve", lambda e, s_=s_: e.tensor_scalar(s_[:, 8:9], s_[:, 0:1], -1.0, None, ALU.mult), reads=[sk + "a"], writes=[sk + "b"])
                    P.op("act", lambda e, u=u, s_=s_: e.activation(ex[u], lg[u], AF.Exp, bias=s_[:, 8:9], scale=1.0), reads=[f"lg{u}", sk + "b"], writes=[f"ex{u}"])
                    P.op("dve", lambda e, u=u, s_=s_: e.tensor_scalar(lg[u], lg[u], s_[:, 1:2], None, ALU.is_ge), reads=[f"lg{u}", sk + "a", f"ex{u}"], writes=[f"lg{u}"])
                    P.op("dve", lambda e, u=u: e.tensor_tensor(ex[u], ex[u], lg[u], ALU.mult), reads=[f"lg{u}", f"ex{u}"], writes=[f"ex{u}"])
                    P.op("dve", lambda e, u=u, s_=s_: e.reduce_sum(s_[:, 9:10], ex[u], axis=AX.X), reads=[f"ex{u}"], writes=[sk + "c"])
                    P.op("dve", lambda e, s_=s_: e.reciprocal(s_[:, 9:10], s_[:, 9:10]), reads=[sk + "c"], writes=[sk + "c"])
                    P.op("dve", lambda e, u=u, i=i, s_=s_: e.tensor_scalar(G[:, i, :], ex[u], s_[:, 9:10], None, ALU.mult), reads=[f"ex{u}", sk + "c"], writes=["G"])
                else:
                    emit_hT(l, 1, xt[u], f"xt{u}", m, hT, "hT", i * 128, [bank(0), bank(1)], ["ps0", "ps1"], cnt)
            P.barrier()
            A.off = mark
            uT = [A.bf(4, 512), A.bf(4, 512)]
            sa = [A.f32(512), A.f32(512)]
            ev = [A.f32(1024), A.f32(1024)]
            tgs = [(t0, min(4, NT - t0)) for t0 in range(0, NT, 4)]
            items = []
            for wi, (ex_i, f0, fw) in enumerate(wlist):
                for (t0, nt) in tgs:
                    items.append((ex_i, f0, fw, t0, nt, wi))

            upc = [0]

            def emit_up(it, n):
                ex_i, f0, fw, t0, nt, wi_ = it
                wu = wi_ % 2
                uu = n % 2
                ntok = nt * 128
                for fs in range(fw // 128):
                    q = upc[0] % 2
                    upc[0] += 1
                    pa, pg = bank(q), bank(2 + q)
                    for k in range(8):
                        P.op("pe", lambda e, pa=pa, k=k, fs=fs, wu=wu, t0=t0, ntok=ntok: e.matmul(pa[:, 0:ntok], wa[wu][:, k, fs * 128:(fs + 1) * 128], hT[:, k, t0 * 128:t0 * 128 + ntok], start=(k == 0), stop=(k == 7)),
                             reads=[f"wa{wu}"], writes=[f"ps{q}"])
                    for k in range(8):
                        P.op("pe", lambda e, pg=pg, k=k, fs=fs, wu=wu, t0=t0, ntok=ntok: e.matmul(pg[:, 0:ntok], wg[wu][:, k, fs * 128:(fs + 1) * 128], hT[:, k, t0 * 128:t0 * 128 + ntok], start=(k == 0), stop=(k == 7)),
                             reads=[f"wg{wu}"], writes=[f"ps{2 + q}"])
                    P.op("act", lambda e, q=q, pa=pa, ntok=ntok: e.activation(sa[q][:, 0:ntok], pa[:, 0:ntok], AF.Silu), reads=[f"ps{q}"], writes=[f"sa{q}"])
                    P.op("dve", lambda e, q=q, pg=pg, uu=uu, fs=fs, ntok=ntok: e.tensor_tensor(uT[uu][:, fs, 0:ntok], sa[q][:, 0:ntok], pg[:, 0:ntok], ALU.mult),
                         reads=[f"sa{q}", f"ps{2 + q}"], writes=[f"uT{uu}"])

            dnc = [0]
            first_done = set()

            def emit_down(it, n):
                ex_i, f0, fw, t0, nt, wi_ = it
                wu = wi_ % 2
                uu = n % 2
                nfs = fw // 128
                for tt in range(nt):
                    ti = t0 + tt
                    q = dnc[0] % 2
                    dnc[0] += 1
                    pd = psa[:, 2048 + q * 1024:3072 + q * 1024]
                    for oh in range(2):
                        for fs in range(nfs):
                            P.op("pe", lambda e, pd=pd, oh=oh, fs=fs, uu=uu, tt=tt, wu=wu, nfs=nfs: e.matmul(pd[:, oh * 512:(oh + 1) * 512], uT[uu][:, fs, tt * 128:(tt + 1) * 128], w2[wu][:, fs, oh * 512:(oh + 1) * 512], start=(fs == 0), stop=(fs == nfs - 1)),
                                 reads=[f"uT{uu}", f"w2{wu}"], writes=[f"ps{4 + 2 * q + oh}"])
                    yk = f"y{ti}"
                    first = ti not in first_done
                    first_done.add(ti)
                    if moe:
                        gsc = G[:, ti, ex_i:ex_i + 1]
                        if first:
                            P.op("act", lambda e, pd=pd, ti=ti, gsc=gsc: e.activation(yacc[:, ti, :], pd, AF.Identity, bias=cst[:, 2:3], scale=gsc), reads=[f"ps{4 + 2 * q}", f"ps{5 + 2 * q}"], writes=[yk])
                        else:
                            P.op("act", lambda e, pd=pd, q=q, gsc=gsc: e.activation(ev[q], pd, AF.Identity, bias=cst[:, 2:3], scale=gsc), reads=[f"ps{4 + 2 * q}", f"ps{5 + 2 * q}"], writes=[f"ev{q}"])
                            eng = "pool" if ti % 2 == 0 else "dve"
                            P.op(eng, lambda e, ti=ti, q=q: e.tensor_tensor(yacc[:, ti, :], yacc[:, ti, :], ev[q], ALU.add), reads=[f"ev{q}", yk], writes=[yk])
                    else:
                        if first:
                            P.op("act", lambda e, pd=pd, ti=ti: e.copy(yacc[:, ti, :], pd), reads=[f"ps{4 + 2 * q}", f"ps{5 + 2 * q}"], writes=[yk])
                        else:
                            P.op("dve", lambda e, pd=pd, ti=ti: e.tensor_tensor(yacc[:, ti, :], yacc[:, ti, :], pd, ALU.add), reads=[f"ps{4 + 2 * q}", f"ps{5 + 2 * q}", yk], writes=[yk])

            load_w(0)
            for n, it in enumerate(items):
                if n == 0 or items[n - 1][5] != it[5]:
                    load_w(it[5] + 1)
                if n == 0:
                    emit_up(it, 0)
                if n + 1 < len(items):
                    emit_up(items[n + 1], n + 1)
                emit_down(it, n)
            P.barrier()
            A.off = mark
            xt = [A.f32(1024), A.f32(1024)]
            gbm = [A.f32(1024), A.f32(1024), A.f32(1024)]
            lng = A.f32(1024)
            lnb = A.f32(1024)
            z = [A.f32(1024), A.f32(1024)]
            n_t = [A.f32(1024), A.f32(1024)]
            o = [A.f32(1024), A.f32(1024)]
            bn = [A.f32(2, 6), A.f32(2, 6)]
            st2 = [A.f32(8), A.f32(8)]
            ms = sorted(set(m for _, m in tl))
            for m in ms:
                P.dma("sp", gbm[m], gates_d[l, 1, m], writes=[f"gbm{m}"])
            P.dma("sp", lng, L[l]["ln"][2:3, :].partition_broadcast(128), writes=["lng"])
            P.dma("sp", lnb, L[l]["ln"][3:4, :].partition_broadcast(128), writes=["lnb"])
            def ld3(i):
                P.dma("sp", xt[i % 2], tl[i][0], writes=[f"xt{i % 2}"])

            def stA(i):
                u = i % 2
                m = tl[i][1]
                P.op("dve", lambda e, u=u, i=i, m=m: e.tensor_tensor(z[u], yacc[:, i, :], gbm[m], ALU.mult), reads=[f"gbm{m}"], writes=[f"z{u}"])
                P.op("dve", lambda e, u=u: e.scalar_tensor_tensor(z[u], xt[u], ALPHA, z[u], ALU.mult, ALU.add), reads=[f"xt{u}", f"z{u}"], writes=[f"z{u}"])

            def stB_ops(i):
                u = i % 2
                return ln_ops(z[u], f"z{u}", o[u], f"o{u}", lng, lnb, st2[u], f"lst{u}", bn[u], n_t[u], f"n{u}") + \
                    [lambda: P.dma("sp", dsts[i], o[u], reads=[f"o{u}"])]

            ld3(0)
            ld3(1)
            for i in range(0, NT, 2):
                stA(i)
                stA(i + 1)
                if i + 2 < NT:
                    ld3(i + 2)
                    ld3(i + 3)
                zip_run([stB_ops(i), stB_ops(i + 1)])
            P.barrier()

        plist = []
        plist.append(lambda: phase_mods(0))
        for b in range(2):
            plist.append(lambda b=b: phase_proj(0, b, None))
            plist.append(lambda b=b: phase_na(b))
            plist.append(lambda b=b: phase_full_attn(0, b))
            plist.append(lambda b=b: phase_outproj(0, b, None, X1, 18))
        for b in range(2):
            tl = [(X1[b, t * 128:(t + 1) * 128, :], b) for t in range(16)]
            ds = [X2[b, t * 128:(t + 1) * 128, :] for t in range(16)]
            plist.append(lambda tl=tl, ds=ds: phase_ffn(0, tl, ds, False))
        tl = [(X1[b, SEQ + t * 128:SEQ + (t + 1) * 128, :], 2) for b in range(2) for t in range(2)]
        ds = [X2[b, SEQ + t * 128:SEQ + (t + 1) * 128, :] for b in range(2) for t in range(2)]
        plist.append(lambda tl=tl, ds=ds: phase_ffn(0, tl, ds, False))
        plist.append(lambda: phase_mods(1))
        for b in range(2):
            plist.append(lambda b=b: phase_proj(1, b, X2))
            plist.append(lambda b=b: phase_full_attn(1, b))
            plist.append(lambda b=b: phase_outproj(1, b, X2, X3, 16))
            tl = [(X3[b, t * 128:(t + 1) * 128, :], b) for t in range(16)]
            ds = [out_d[b, t * 128:(t + 1) * 128, :] for t in range(16)]
            plist.append(lambda tl=tl, ds=ds: phase_ffn(1, tl, ds, True))
        for pf in plist[:nphases]:
            pf()
        P.emit()
    return nc


def _perm64(ncols):
    idx = np.arange(ncols)
    blk = idx // 64
    r = idx % 64
    return blk * 64 + (r + 32) % 64


def _na_bias_tables(rpb):
    out = np.zeros((5, 128, 8, 832), np.float32)
    reps = [0, 1, 2, 14, 15]
    qi = np.arange(128)
    rr, c = qi // 64, qi % 64
    kk = np.arange(576)
    kr, kc = kk // 64, kk % 64
    for pid, i in enumerate(reps):
        K0 = min(max(2 * i - 4, 0), 23)
        r = 2 * i + rr
        r0 = np.clip(r - 4, 0, 24)
        ws = np.clip(c - 8, 0, 48)
        krow = K0 + kr
        vrow = (krow[None, :] >= r0[:, None]) & (krow[None, :] < r0[:, None] + 8)
        vcol = (kc[None, :] >= ws[:, None]) & (kc[None, :] < ws[:, None] + 16)
        valid = vrow & vcol
        dr = np.clip(krow[None, :] - r[:, None] + 7, 0, 14)
        dc = np.clip(kc[None, :] - c[:, None] + 15, 0, 30)
        g = rpb[:, dr, dc]
        g = np.where(valid[None], g, np.float32(NEG))
        out[pid, :, :, 0:576] = g.transpose(1, 0, 2)
    return out


def _rope_tables():
    t = np.arange(SEQ)
    inv = (np.float32(10000.0) ** (-np.arange(16, dtype=np.float32) / np.float32(16))).astype(np.float32)
    ang = np.concatenate([(t // 64).astype(np.float32)[:, None] * inv, (t % 64).astype(np.float32)[:, None] * inv], axis=-1)
    cos = np.cos(ang).astype(np.float32).T
    sin = np.sin(ang).astype(np.float32).T
    C = np.ones((128, T), np.float32)
    S = np.zeros((128, T), np.float32)
    for p in range(128):
        C[p, :SEQ] = cos[p % 32]
        S[p, :SEQ] = sin[p % 32] if (p % 64) >= 32 else -sin[p % 32]
    return C, S


_CACHE = {}


def _prep(inp):
    f = lambda a: np.ascontiguousarray(np.asarray(a, dtype=np.float32))
    C, S = _rope_tables()
    blk = np.zeros((128, 128), np.float32)
    blk[:64, :64] = 1.0 / 64
    blk[64:, 64:] = 1.0 / 64
    shared = {"ident": np.eye(128, dtype=np.float32), "blk": blk, "ropeC": C, "ropeS": S}
    for l in range(2):
        shared[f"l{l}_ada_w"] = f(inp[f"l{l}_ada_w"])
        ab = f(inp[f"l{l}_ada_b"])
        shared[f"l{l}_ada_bT"] = np.ascontiguousarray(ab.reshape(48, 128).T)
        shared[f"l{l}_ada_b"] = ab.reshape(1, 6 * D)
        shared[f"l{l}_w_out"] = f(inp[f"l{l}_w_out"])
        shared[f"l{l}_ln"] = np.stack([f(inp[f"l{l}_ln1_g"]), f(inp[f"l{l}_ln1_b"]), f(inp[f"l{l}_ln2_g"]), f(inp[f"l{l}_ln2_b"])])
    w0 = f(inp["l0_w_in"])
    qa, ka, va, qb, kb, vb = w0[:, 0:512], w0[:, 512:1024], w0[:, 1024:1536], w0[:, 1536:2048], w0[:, 2048:2560], w0[:, 2560:3072]
    shared["l0_wfm"] = np.ascontiguousarray(np.concatenate([qa, ka, qb, kb], axis=1))
    p512 = _perm64(512)
    shared["l0_wfmp"] = np.ascontiguousarray(np.concatenate([qb[:, p512], kb[:, p512]], axis=1))
    shared["l0_wv"] = np.ascontiguousarray(np.concatenate([va, vb], axis=1))
    shared["l0_nabias"] = _na_bias_tables(f(inp["l0_rpb"]))
    shared["l0_lq"] = f(inp["l0_lambda_qk"]).reshape(1, 256)
    shared["l0_subg"] = f(inp["l0_subln_g"]).reshape(1, 128)
    shared["l0_subgc"] = f(inp["l0_subln_g"]).reshape(128, 1)
    shared["l0_w13"] = f(inp["l0_ffn_w13"])
    shared["l0_w2"] = f(inp["l0_ffn_w2"])
    w1 = f(inp["l1_w_in"])
    q1, k1, v1 = w1[:, 0:1024], w1[:, 1024:1280], w1[:, 1280:1536]
    kdup = np.concatenate([np.concatenate([k1[:, 64 * g:64 * (g + 1)]] * 2, axis=1) for g in range(4)], axis=1)
    wfm1 = np.concatenate([q1, kdup], axis=1)
    shared["l1_wfm"] = np.ascontiguousarray(wfm1)
    shared["l1_wfmp"] = np.ascontiguousarray(wfm1[:, _perm64(1536)])
    shared["l1_wv"] = np.ascontiguousarray(v1)
    gq, gk = f(inp["l1_q_norm_g"]), f(inp["l1_k_norm_g"])
    p64 = _perm64(64)
    shared["l1_gcols"] = np.ascontiguousarray(np.stack([np.tile(gq, 2), np.tile(gq[p64], 2), np.tile(gk, 2), np.tile(gk[p64], 2)], axis=1))
    shared["l1_grow"] = np.ascontiguousarray(np.concatenate([gq, gk]).reshape(1, 128))
    shared["l1_router"] = f(inp["l1_router_w"])
    shared["l1_w13"] = f(inp["l1_moe_w13"])
    shared["l1_w2"] = f(inp["l1_moe_w2"])
    x, ctx, c, c_ctx = f(inp["x"]), f(inp["ctx"]), f(inp["c"]), f(inp["c_ctx"])
    in_maps = []
    for i in range(NCORES):
        d = dict(shared)
        d["x"] = x[2 * i:2 * i + 2]
        d["ctx"] = ctx[2 * i:2 * i + 2]
        c3 = np.stack([c[2 * i], c[2 * i + 1], c_ctx])
        d["cT"] = np.ascontiguousarray(c3.reshape(3, 8, 128).transpose(2, 1, 0))
        in_maps.append(d)
    return in_maps


def kernel(**inp):
    if "nc" not in _CACHE:
        _CACHE["nc"] = build_program()
    nc = _CACHE["nc"]
    in_maps = _prep(inp)
    res = run_bass_kernel_spmd(nc, in_maps, core_ids=list(range(NCORES)))
    return np.concatenate([r["out"] for r in res.results], axis=0).astype(np.float32)
```
